# Optimizing a Trainium2 kernel written in Bass

```python
import jax, jax.numpy as jnp
from jax import lax
import numpy as np

D_MODEL = 1024
BATCH = 8
SEQ = 2048
DEPTH = 2

GRID_W = 64
CTX_LEN = 256
N_MOD = 6
HEAD_DIM = 64
ROPE_BASE = 10000.0
Q_BLOCK = 128
EPS = 1e-6

MLA_HEADS = 4
MLA_Q_LORA = 256
MLA_KV_LORA = 128
MLA_NOPE = 64
MLA_ROPE = 32
MLA_V = 64
MLA_QK = MLA_NOPE + MLA_ROPE
MLA_COLS = MLA_Q_LORA + MLA_KV_LORA + MLA_ROPE
MLA_WIDTH = MLA_HEADS * MLA_V

RW_HEADS = 6
RW_N = 64
RW_WIDTH = RW_HEADS * RW_N
RW_DECAY_LORA = 64
RW_A_LORA = 64
RW_GATE_LORA = 160
RW_COLS = 3 * RW_WIDTH + 2 * RW_DECAY_LORA + 2 * RW_A_LORA + RW_GATE_LORA
RW_GN_EPS = 64e-5

GQA_HEADS = 6
GQA_KV_HEADS = 2
GQA_GROUP = GQA_HEADS // GQA_KV_HEADS
GQA_COLS = (GQA_HEADS + 2 * GQA_KV_HEADS) * HEAD_DIM
GQA_WIDTH = GQA_HEADS * HEAD_DIM

D_MIX = MLA_WIDTH + RW_WIDTH + GQA_WIDTH
IN_COLS = MLA_COLS + RW_COLS + GQA_COLS

PEER_HEADS = 8
PEER_NKEYS = 128
PEER_N = PEER_NKEYS * PEER_NKEYS
PEER_QDIM = 256
PEER_HALF = PEER_QDIM // 2
PEER_TOPK = 16
PEER_CHUNK = 128

kernel_name = "hybrid_mla_rwkv7_gqa_peer_dit"

F32 = jnp.float32


def rmsnorm(x, g, eps=EPS):
    x32 = x.astype(F32)
    y = x32 * lax.rsqrt(jnp.mean(x32 * x32, axis=-1, keepdims=True) + eps)
    return (y * g.astype(F32)).astype(x.dtype)


def modulate(h, shift, scale):
    return h * (1 + scale) + shift


def grid_positions(n_tokens):
    rows = n_tokens // GRID_W
    row = jnp.repeat(jnp.arange(rows, dtype=jnp.int32), GRID_W)
    col = jnp.tile(jnp.arange(GRID_W, dtype=jnp.int32), rows)
    return row, col


def rope_2d(x, row, col):
    rdim = x.shape[-1]
    quarter = rdim // 4
    inv_freq = ROPE_BASE ** (-jnp.arange(quarter, dtype=F32) / quarter)
    ang = jnp.concatenate([row.astype(F32)[:, None] * inv_freq,
                           col.astype(F32)[:, None] * inv_freq], axis=-1)
    cos = jnp.cos(ang)[:, None, :]
    sin = jnp.sin(ang)[:, None, :]
    x32 = x.astype(F32)
    xa, xb = x32[..., :rdim // 2], x32[..., rdim // 2:]
    return jnp.concatenate([xa * cos - xb * sin, xa * sin + xb * cos], axis=-1).astype(x.dtype)


def attend(q, k, v):
    B, T, Hkv, G, dq = q.shape
    scale = dq ** -0.5
    nb = T // Q_BLOCK
    qb = jnp.moveaxis(q.reshape(B, nb, Q_BLOCK, Hkv, G, dq), 1, 0)

    def one_block(qblk):
        s = jnp.einsum('bqhgd,bkhd->bhgqk', qblk, k).astype(F32) * scale
        p = jax.nn.softmax(s, axis=-1).astype(v.dtype)
        return jnp.einsum('bhgqk,bkhd->bqhgd', p, v)

    o = lax.map(one_block, qb)
    return jnp.moveaxis(o, 0, 1).reshape(B, T, Hkv * G * v.shape[-1])


def mla_project(p, q_norm_g, w_qb, kv_norm_g, w_kvb, qk_g):
    B, T, _ = p.shape
    cq, ckv, k_rope = jnp.split(p, [MLA_Q_LORA, MLA_Q_LORA + MLA_KV_LORA], axis=-1)
    q = (rmsnorm(cq, q_norm_g) @ w_qb).reshape(B, T, MLA_HEADS, MLA_QK)
    kv = (rmsnorm(ckv, kv_norm_g) @ w_kvb).reshape(B, T, MLA_HEADS, MLA_NOPE + MLA_V)
    k_nope, v = kv[..., :MLA_NOPE], kv[..., MLA_NOPE:]
    k = jnp.concatenate([k_nope, jnp.broadcast_to(k_rope[:, :, None, :], (B, T, MLA_HEADS, MLA_ROPE))], axis=-1)
    return rmsnorm(q, qk_g[0]), rmsnorm(k, qk_g[1]), v


def mla_mixer(p_lat, p_ctx, row, col, q_norm_g, w_qb, kv_norm_g, w_kvb, qk_g, need_ctx):
    ql, kl, vl = mla_project(p_lat, q_norm_g, w_qb, kv_norm_g, w_kvb, qk_g)
    qc, kc, vc = mla_project(p_ctx, q_norm_g, w_qb, kv_norm_g, w_kvb, qk_g)
    ql = jnp.concatenate([ql[..., :MLA_NOPE], rope_2d(ql[..., MLA_NOPE:], row, col)], axis=-1)
    kl = jnp.concatenate([kl[..., :MLA_NOPE], rope_2d(kl[..., MLA_NOPE:], row, col)], axis=-1)
    k_all = jnp.concatenate([kc, kl], axis=1)
    v_all = jnp.concatenate([vc, vl], axis=1)
    o_lat = attend(ql[:, :, :, None, :], k_all, v_all)
    o_ctx = attend(qc[:, :, :, None, :], kc, vc) if need_ctx else None
    return o_lat, o_ctx


def token_shift(p, mu):
    prev = jnp.pad(p, ((0, 0), (1, 0), (0, 0)))[:, :-1]
    nxt = jnp.pad(p, ((0, 0), (0, 1), (0, 0)))[:, 1:]
    return p + mu[0] * (prev - p) + mu[1] * (nxt - p)


def rwkv_prep(p, mu, w0, w2, a0, a2, g2, k_k, k_a):
    B, T, _ = p.shape
    xs = token_shift(p, mu)
    W, DL, AL = RW_WIDTH, RW_DECAY_LORA, RW_A_LORA
    r, k, v, wl, al, gl = jnp.split(xs, [W, 2 * W, 3 * W, 3 * W + 2 * DL, 3 * W + 2 * DL + 2 * AL], axis=-1)
    wl = wl.reshape(B, T, 2, DL)
    al = al.reshape(B, T, 2, AL)
    w_raw = (w0 + jnp.einsum('btdr,drc->btdc', jnp.tanh(wl), w2)).astype(F32)
    decay = jnp.exp(-jnp.exp(-jax.nn.softplus(-w_raw) - 0.5))
    a = jax.nn.sigmoid((a0 + jnp.einsum('btdr,drc->btdc', al, a2)).astype(F32))
    heads = lambda t: t.reshape(*t.shape[:-1], RW_HEADS, RW_N)
    kk = heads((k * k_k).astype(F32))
    kk = kk * lax.rsqrt(jnp.sum(kk * kk, axis=-1, keepdims=True) + 1e-12)
    a_h = heads(a)
    k_dir = heads(k.astype(F32))[:, :, None] * (1 + (a_h - 1) * heads(k_a.astype(F32)))
    b_dir = kk[:, :, None] * a_h
    g = jax.nn.sigmoid(gl) @ g2
    return heads(r), heads(decay), k_dir, heads(v), kk, b_dir, g


def rwkv_scan(s0, r, decay, k, v, kk, b, reverse, with_outputs):
    xs = tuple(jnp.moveaxis(t.astype(F32), 1, 0) for t in (r, decay, k, v, kk, b))

    def step(S, inp):
        r_t, w_t, k_t, v_t, kk_t, b_t = inp
        s_kk = jnp.einsum('bhij,bhj->bhi', S, kk_t)
        S = S * w_t[:, :, None, :] - s_kk[..., :, None] * b_t[:, :, None, :] + v_t[..., :, None] * k_t[:, :, None, :]
        y = jnp.einsum('bhij,bhj->bhi', S, r_t) if with_outputs else None
        return S, y

    s_final, ys = lax.scan(step, s0, xs, reverse=reverse)
    return s_final, (jnp.moveaxis(ys, 0, 1) if with_outputs else None)


def rwkv_readout(y, r, k_dir, v, g, r_k, ln_g, ln_b, dtype):
    B, T, H, N = y.shape
    mean = jnp.mean(y, axis=-1, keepdims=True)
    var = jnp.mean((y - mean) ** 2, axis=-1, keepdims=True)
    yn = ((y - mean) * lax.rsqrt(var + RW_GN_EPS)).reshape(B, T, H * N) * ln_g + ln_b
    coef = jnp.sum(r[:, :, None].astype(F32) * k_dir * r_k.astype(F32), axis=(2, 4))[..., None]
    bonus = (coef * v.astype(F32)).reshape(B, T, H * N)
    return ((yn + bonus) * g).astype(dtype)


def rwkv_mixer(p_lat, p_ctx, mu, w0, w2, a0, a2, g2, k_k, k_a, r_k, ln_g, ln_b, need_ctx):
    lr, ldec, lk, lv, lkk, lb, lg = rwkv_prep(p_lat, mu, w0, w2, a0, a2, g2, k_k, k_a)
    cr, cdec, ck, cv, ckk, cb, cg = rwkv_prep(p_ctx, mu, w0, w2, a0, a2, g2, k_k, k_a)
    B = p_lat.shape[0]
    s0 = jnp.zeros((B, RW_HEADS, RW_N, RW_N), F32)
    ys_lat, ys_ctx = [], []
    for d in range(2):
        rev = d == 1
        s_ctx, y_c = rwkv_scan(s0, cr, cdec[:, :, d], ck[:, :, d], cv, ckk, cb[:, :, d], rev, need_ctx)
        _, y_l = rwkv_scan(s_ctx, lr, ldec[:, :, d], lk[:, :, d], lv, lkk, lb[:, :, d], rev, True)
        ys_lat.append(y_l)
        ys_ctx.append(y_c)
    o_lat = rwkv_readout(ys_lat[0] + ys_lat[1], lr, lk, lv, lg, r_k, ln_g, ln_b, p_lat.dtype)
    o_ctx = rwkv_readout(ys_ctx[0] + ys_ctx[1], cr, ck, cv, cg, r_k, ln_g, ln_b, p_ctx.dtype) if need_ctx else None
    return o_lat, o_ctx


def gqa_project(p, qk_g):
    B, T, _ = p.shape
    q, k, v = jnp.split(p, [GQA_HEADS * HEAD_DIM, (GQA_HEADS + GQA_KV_HEADS) * HEAD_DIM], axis=-1)
    q = rmsnorm(q.reshape(B, T, GQA_HEADS, HEAD_DIM), qk_g[0])
    k = rmsnorm(k.reshape(B, T, GQA_KV_HEADS, HEAD_DIM), qk_g[1])
    return q, k, v.reshape(B, T, GQA_KV_HEADS, HEAD_DIM)


def gqa_mixer(p_lat, p_ctx, row, col, qk_g, need_ctx):
    ql, kl, vl = gqa_project(p_lat, qk_g)
    qc, kc, vc = gqa_project(p_ctx, qk_g)
    ql = rope_2d(ql, row, col)
    kl = rope_2d(kl, row, col)
    B, T = ql.shape[:2]
    k_all = jnp.concatenate([kc, kl], axis=1)
    v_all = jnp.concatenate([vc, vl], axis=1)
    o_lat = attend(ql.reshape(B, T, GQA_KV_HEADS, GQA_GROUP, HEAD_DIM), k_all, v_all)
    o_ctx = attend(qc.reshape(B, qc.shape[1], GQA_KV_HEADS, GQA_GROUP, HEAD_DIM), kc, vc) if need_ctx else None
    return o_lat, o_ctx


def peer_ffn(h, w_q, sub_keys, u_tab, v_tab):
    B, T, D = h.shape
    chunks = h.reshape(-1, PEER_CHUNK, D)

    def one_chunk(xc):
        q = (xc @ w_q).reshape(PEER_CHUNK, PEER_HEADS, 2, PEER_HALF)
        s = jnp.einsum('cpsd,pskd->cpsk', q, sub_keys).astype(F32)
        s1, i1 = lax.top_k(s[:, :, 0], PEER_TOPK)
        s2, i2 = lax.top_k(s[:, :, 1], PEER_TOPK)
        cand_s = (s1[..., :, None] + s2[..., None, :]).reshape(PEER_CHUNK, PEER_HEADS, PEER_TOPK * PEER_TOPK)
        cand_i = (i1[..., :, None] * PEER_NKEYS + i2[..., None, :]).reshape(PEER_CHUNK, PEER_HEADS, PEER_TOPK * PEER_TOPK)
        top_s, pos = lax.top_k(cand_s, PEER_TOPK)
        e_idx = jnp.take_along_axis(cand_i, pos, axis=-1)
        gate = jax.nn.softmax(top_s, axis=-1)
        act = jax.nn.gelu(jnp.einsum('cd,cpkd->cpk', xc, u_tab[e_idx]).astype(F32))
        w = (gate * act).astype(xc.dtype)
        return jnp.einsum('cpk,cpkd->cd', w, v_tab[e_idx])

    return lax.map(one_chunk, chunks).reshape(B, T, D)


def setup_inputs(seed: int = 0) -> dict:
    key = jax.random.key(seed)
    ks = iter(jax.random.split(key, 40))
    nrm = lambda shape, scale: scale * jax.random.normal(next(ks), shape, F32)
    gain = lambda shape: 1.0 + 0.02 * jax.random.normal(next(ks), shape, F32)
    return {
        "x": nrm((BATCH, SEQ, D_MODEL), 1.0),
        "c": nrm((BATCH, D_MODEL), 1.0),
        "ctx": nrm((BATCH, CTX_LEN, D_MODEL), 1.0),
        "c_ctx": nrm((D_MODEL,), 1.0),
        "ada_w": nrm((DEPTH, D_MODEL, N_MOD * D_MODEL), 0.5 * D_MODEL ** -0.5),
        "ada_b": nrm((DEPTH, N_MOD * D_MODEL), 0.02),
        "norm_mix_g": gain((DEPTH, D_MODEL)),
        "norm_ffn_g": gain((DEPTH, D_MODEL)),
        "w_in": nrm((DEPTH, D_MODEL, IN_COLS), D_MODEL ** -0.5),
        "w_out": nrm((DEPTH, D_MIX, D_MODEL), D_MIX ** -0.5),
        "mla_q_norm_g": gain((DEPTH, MLA_Q_LORA)),
        "mla_w_qb": nrm((DEPTH, MLA_Q_LORA, MLA_HEADS * MLA_QK), MLA_Q_LORA ** -0.5),
        "mla_kv_norm_g": gain((DEPTH, MLA_KV_LORA)),
        "mla_w_kvb": nrm((DEPTH, MLA_KV_LORA, MLA_HEADS * (MLA_NOPE + MLA_V)), MLA_KV_LORA ** -0.5),
        "mla_qk_g": gain((DEPTH, 2, MLA_QK)),
        "rw_mu": jax.random.uniform(next(ks), (DEPTH, 2, RW_COLS), F32, 0.0, 0.5),
        "rw_w0": jax.random.uniform(next(ks), (DEPTH, 2, RW_WIDTH), F32, -6.0, 0.0),
        "rw_w2": nrm((DEPTH, 2, RW_DECAY_LORA, RW_WIDTH), 0.1),
        "rw_a0": nrm((DEPTH, 2, RW_WIDTH), 0.1),
        "rw_a2": nrm((DEPTH, 2, RW_A_LORA, RW_WIDTH), 0.1),
        "rw_g2": nrm((DEPTH, RW_GATE_LORA, RW_WIDTH), RW_GATE_LORA ** -0.5),
        "rw_k_k": 0.85 + nrm((DEPTH, RW_WIDTH), 0.02),
        "rw_k_a": gain((DEPTH, RW_WIDTH)),
        "rw_r_k": nrm((DEPTH, RW_HEADS, RW_N), 0.1),
        "rw_ln_g": gain((DEPTH, RW_WIDTH)),
        "rw_ln_b": nrm((DEPTH, RW_WIDTH), 0.02),
        "gqa_qk_g": gain((DEPTH, 2, HEAD_DIM)),
        "pe_wq": nrm((DEPTH, D_MODEL, PEER_HEADS * PEER_QDIM), D_MODEL ** -0.5),
        "pe_keys": nrm((DEPTH, PEER_HEADS, 2, PEER_NKEYS, PEER_HALF), PEER_HALF ** -0.5),
        "pe_u": nrm((DEPTH, PEER_N, D_MODEL), D_MODEL ** -0.5),
        "pe_v": nrm((DEPTH, PEER_N, D_MODEL), 0.3),
    }


def reference(x, c, ctx, c_ctx, ada_w, ada_b, norm_mix_g, norm_ffn_g, w_in, w_out,
              mla_q_norm_g, mla_w_qb, mla_kv_norm_g, mla_w_kvb, mla_qk_g,
              rw_mu, rw_w0, rw_w2, rw_a0, rw_a2, rw_g2, rw_k_k, rw_k_a, rw_r_k, rw_ln_g, rw_ln_b,
              gqa_qk_g, pe_wq, pe_keys, pe_u, pe_v):
    B = x.shape[0]
    row, col = grid_positions(x.shape[1])
    for l in range(DEPTH):
        need_ctx = l < DEPTH - 1
        m = (jax.nn.silu(c) @ ada_w[l] + ada_b[l]).reshape(B, N_MOD, 1, D_MODEL)
        mc = (jax.nn.silu(c_ctx) @ ada_w[l] + ada_b[l]).reshape(N_MOD, D_MODEL)
        p = modulate(rmsnorm(x, norm_mix_g[l]), m[:, 0], m[:, 1]) @ w_in[l]
        pc = modulate(rmsnorm(ctx, norm_mix_g[l]), mc[0], mc[1]) @ w_in[l]
        p_mla, p_rw, p_gqa = jnp.split(p, [MLA_COLS, MLA_COLS + RW_COLS], axis=-1)
        pc_mla, pc_rw, pc_gqa = jnp.split(pc, [MLA_COLS, MLA_COLS + RW_COLS], axis=-1)
        a_lat, a_ctx = mla_mixer(p_mla, pc_mla, row, col, mla_q_norm_g[l], mla_w_qb[l],
                                 mla_kv_norm_g[l], mla_w_kvb[l], mla_qk_g[l], need_ctx)
        r_lat, r_ctx = rwkv_mixer(p_rw, pc_rw, rw_mu[l], rw_w0[l], rw_w2[l], rw_a0[l], rw_a2[l],
                                  rw_g2[l], rw_k_k[l], rw_k_a[l], rw_r_k[l], rw_ln_g[l], rw_ln_b[l], need_ctx)
        g_lat, g_ctx = gqa_mixer(p_gqa, pc_gqa, row, col, gqa_qk_g[l], need_ctx)
        x = x + m[:, 2] * (jnp.concatenate([a_lat, r_lat, g_lat], axis=-1) @ w_out[l])
        x = x + m[:, 5] * peer_ffn(modulate(rmsnorm(x, norm_ffn_g[l]), m[:, 3], m[:, 4]),
                                   pe_wq[l], pe_keys[l], pe_u[l], pe_v[l])
        if need_ctx:
            ctx = ctx + mc[2] * (jnp.concatenate([a_ctx, r_ctx, g_ctx], axis=-1) @ w_out[l])
            ctx = ctx + mc[5] * peer_ffn(modulate(rmsnorm(ctx, norm_ffn_g[l]), mc[3], mc[4]),
                                         pe_wq[l], pe_keys[l], pe_u[l], pe_v[l])
    return x
```

```python
from contextlib import ExitStack
import numpy as np
import concourse.bass as bass
import concourse.mybir as mybir

F32 = mybir.dt.float32
BF16 = mybir.dt.bfloat16
U32 = mybir.dt.uint32
AF = mybir.ActivationFunctionType
ALU = mybir.AluOpType
AX = mybir.AxisListType

COMPUTE = ("pe", "dve", "act", "pool")
NDS = 24


class Trk:
    __slots__ = ("w", "r")

    def __init__(self):
        self.w = None
        self.r = {}


class Kb:
    def __init__(self, nc, es, same_eng_sync=True, scopes=False):
        self.scopes = scopes
        self.nc = nc
        self.es = es
        self.same = ("dve", "act", "pool") if same_eng_sync is True else tuple(same_eng_sync or ())
        self.eng = {"pe": nc.tensor, "dve": nc.vector, "act": nc.scalar, "pool": nc.gpsimd, "sp": nc.sync}
        self.q = {e: [] for e in self.eng}
        self.cnt = {e: 0 for e in COMPUTE}
        self.sem = {e: es.enter_context(nc.semaphore("sem_" + e)) for e in COMPUTE}
        self.dsem = [es.enter_context(nc.semaphore("dsem%d" % i)) for i in range(NDS)]
        self.dcount = 0
        self.waited = {}
        self.ntens = 0
        self.pend = {e: [] for e in self.eng}
        self.stacks = []
        self.cur = "main"

    def push(self):
        st = ExitStack()
        st.__enter__()
        self.stacks.append(self.es)
        self.es = st

    def pop(self):
        self.barrier()
        self.es.__exit__(None, None, None)
        self.es = self.stacks.pop()

    def run_lanes(self, lanes):
        self.push()
        active = [[nm, g, w, 0.0] for nm, g, w in lanes]
        while active:
            active.sort(key=lambda a: a[3])
            a = active[0]
            self.cur = a[0]
            try:
                nm = next(a[1])
                if isinstance(nm, str):
                    a[0] = nm
                a[3] += 1.0 / a[2]
            except StopIteration:
                active.remove(a)
        self.pop()

    def barrier(self):
        for eng in self.eng:
            deps = [("e", E, self.cnt[E]) for E in COMPUTE if self.cnt[E] > 0 and E != eng]
            for k in range(NDS):
                if self.dcount > k:
                    deps.append(("d", k, 16 * ((self.dcount - 1 - k) // NDS + 1)))
            self.pend[eng].extend(self._filter(eng, deps))

    def sb(self, shape, dtype=F32, name=None):
        self.ntens += 1
        t = self.es.enter_context(self.nc.sbuf_tensor(name or ("sb%d" % self.ntens), list(shape), dtype))
        return t

    def ps(self, shape, dtype=F32, name=None):
        self.ntens += 1
        t = self.es.enter_context(self.nc.psum_tensor(name or ("ps%d" % self.ntens), list(shape), dtype))
        return t

    def _deps(self, reads, writes):
        deps = []
        for t in reads:
            if t.w is not None:
                deps.append(t.w)
        for t in writes:
            if t.w is not None:
                deps.append(t.w)
            deps.extend(t.r.values())
        return deps

    def _filter(self, eng, deps):
        waits = []
        for d in deps:
            if d[0] == "e":
                _, E, n = d
                if E == eng and E not in self.same:
                    continue
                key = (eng, E)
                if self.waited.get(key, 0) >= n:
                    continue
                self.waited[key] = n
                waits.append((self.sem[E], n))
            else:
                _, k, v = d
                key = (eng, "d%d" % k)
                if self.waited.get(key, 0) >= v:
                    continue
                self.waited[key] = v
                waits.append((self.dsem[k], v))
        return waits

    def _mark(self, tok, reads, writes):
        for t in reads:
            t.r[tok[1] if tok[0] == "e" else ("d%d" % tok[1])] = tok
        for t in writes:
            t.w = tok
            t.r = {}

    def op(self, eng, fn, reads=(), writes=()):
        deps = self._deps(reads, writes)
        waits = self.pend[eng] + self._filter(eng, deps)
        self.pend[eng] = []
        n = self.cnt[eng] + 1
        self.cnt[eng] = n
        self.q[eng].append((waits, fn, self.sem[eng], 1, self.cur))
        self._mark(("e", eng, n), reads, writes)

    def dma(self, out, in_, reads=(), writes=(), queue="sp", **kw):
        deps = self._deps(reads, writes)
        k = self.dcount % NDS
        v = 16 * (self.dcount // NDS + 1)
        self.dcount += 1
        if v > 16:
            deps.append(("d", k, v - 16))
        waits = self.pend[queue] + self._filter(queue, deps)
        self.pend[queue] = []
        self.q[queue].append((waits, lambda e: e.dma_start(out=out, in_=in_, **kw), self.dsem[k], 16, self.cur))
        self._mark(("d", k, v), reads, writes)

    def emit(self, final_tracks=()):
        deps = []
        for t in final_tracks:
            if t.w is not None:
                deps.append(t.w)
        fw = self._filter("sp", deps)
        nc = self.nc
        q = self.q
        with nc.Block() as block:
            def run(e, recs, extra=()):
                i = 0
                while i < len(recs):
                    sc = recs[i][4]
                    j = i
                    while j < len(recs) and recs[j][4] == sc:
                        j += 1
                    if self.scopes:
                        with nc.named_scope(sc):
                            for waits, fn, sem, inc, _ in recs[i:j]:
                                for s, v in waits:
                                    e.wait_ge(s, v)
                                fn(e).then_inc(sem, inc)
                    else:
                        for waits, fn, sem, inc, _ in recs[i:j]:
                            for s, v in waits:
                                e.wait_ge(s, v)
                            fn(e).then_inc(sem, inc)
                    i = j
                for s, v in extra:
                    e.wait_ge(s, v)

            pend = self.pend

            @block.sync
            def _(e):
                run(e, q["sp"], pend["sp"] + fw)

            @block.tensor
            def _(e):
                run(e, q["pe"], pend["pe"])

            @block.vector
            def _(e):
                run(e, q["dve"], pend["dve"])

            @block.scalar
            def _(e):
                run(e, q["act"], pend["act"])

            @block.gpsimd
            def _(e):
                run(e, q["pool"], pend["pool"])


D = 1024
NTOK = 2304
NT = 18
NCTX_T = 2
IN_COLS = 2624
C_MLA, C_RW, C_GQA = 0, 416, 1984
EPS = 1e-6


def make_ident(K, dtype):
    idf = K.sb([128, 128], F32)
    t = Trk()
    K.op("pool", lambda e: e.memset(idf[:], 0.0), writes=[t])
    K.op("pool", lambda e: e.affine_select(out=idf[:], in_=idf[:], pattern=[[-1, 128]], compare_op=ALU.not_equal,
                                           fill=1.0, base=0, channel_multiplier=1), reads=[t], writes=[t])
    if dtype == F32:
        return idf, t
    idb = K.sb([128, 128], dtype)
    t2 = Trk()
    K.op("dve", lambda e: e.tensor_copy(out=idb[:], in_=idf[:]), reads=[t], writes=[t2])
    return idb, t2


def bcast_load(K, dst, src_row, tr_src, tr_dst, parts=128):
    K.dma(dst, src_row.partition_broadcast(parts), reads=tr_src, writes=[tr_dst])


def phase_ada(K, A, l, modscr, t_mod):
    K.push()
    cv = K.sb([128, 16]); t_cv = Trk()
    K.dma(cv[:], A["cvec"], writes=[t_cv])
    sc = K.sb([128, 16]); t_sc = Trk()
    K.op("act", lambda e: e.activation(out=sc[:], in_=cv[:], func=AF.Silu), reads=[t_cv], writes=[t_sc])
    bias = K.sb([2, 6144]); t_b = Trk()
    K.dma(bias[:], A["ada_b"][l, :].partition_broadcast(2), writes=[t_b])
    res = K.sb([2, 6144]); t_res = Trk()
    wt = [K.sb([128, 8, 512]) for _ in range(2)]; t_w = [Trk(), Trk()]
    ps = [K.ps([2, 512]) for _ in range(2)]; t_ps = [Trk(), Trk()]
    for cb in range(12):
        w = wt[cb % 2]; tw = t_w[cb % 2]; p = ps[cb % 2]; tp = t_ps[cb % 2]
        K.dma(w[:], A["ada_w"][l, :, cb * 512:(cb + 1) * 512].rearrange("(k p) n -> p k n", p=128), writes=[tw])
        for k in range(8):
            K.op("pe", lambda e, k=k, w=w, p=p: e.matmul(p[:], lhsT=sc[:, 2 * k:2 * k + 2], rhs=w[:, k, :],
                                                       start=(k == 0), stop=(k == 7)), reads=[t_sc, tw], writes=[tp])
        K.op("dve", lambda e, p=p, cb=cb: e.tensor_tensor(out=res[:, cb * 512:(cb + 1) * 512], in0=p[:],
                                                          in1=bias[:, cb * 512:(cb + 1) * 512], op=ALU.add),
             reads=[tp, t_b], writes=[t_res])
    K.dma(modscr, res[:], reads=[t_res], writes=[t_mod])
    K.pop()


def load_mod(K, modscr, t_mod, g_row, i_shift, i_scale):
    out = []
    g = K.sb([128, D]); t_g = Trk()
    K.dma(g[:], g_row.partition_broadcast(128), writes=[t_g])
    for which in range(2):
        gm = K.sb([128, D]); sh = K.sb([128, D]); t1 = Trk(); t2 = Trk()
        K.dma(gm[:], modscr[which, i_scale * D:(i_scale + 1) * D].partition_broadcast(128), reads=[t_mod], writes=[t1])
        K.dma(sh[:], modscr[which, i_shift * D:(i_shift + 1) * D].partition_broadcast(128), reads=[t_mod], writes=[t2])
        K.op("pool", lambda e, gm=gm: e.scalar_tensor_tensor(out=gm[:], in0=gm[:], scalar=1.0, in1=g[:], op0=ALU.add, op1=ALU.mult),
             reads=[t1, t_g], writes=[t1]) if False else \
            K.op("dve", lambda e, gm=gm: e.scalar_tensor_tensor(out=gm[:], in0=gm[:], scalar=1.0, in1=g[:], op0=ALU.add, op1=ALU.mult),
                 reads=[t1, t_g], writes=[t1])
        out.append((gm, t1, sh, t2))
    return out


def norm_mod_tile(K, xt, t_x, mods, h, t_h, scr, t_scr, ss, t_ss):
    gm, t_gm, sh, t_sh = mods
    K.op("act", lambda e: e.activation(out=scr[:], in_=xt, func=AF.Square, accum_out=ss[:]), reads=[t_x], writes=[t_scr, t_ss])
    K.op("act", lambda e: e.activation(out=ss[:], in_=ss[:], func=AF.Sqrt, scale=1.0 / D, bias=EPS), reads=[t_ss], writes=[t_ss])
    K.op("dve", lambda e: e.reciprocal(out=ss[:], in_=ss[:]), reads=[t_ss], writes=[t_ss])
    K.op("dve", lambda e: e.scalar_tensor_tensor(out=scr[:], in0=xt, scalar=ss[:, 0:1], in1=gm[:], op0=ALU.mult, op1=ALU.mult),
         reads=[t_x, t_ss, t_gm], writes=[t_scr])
    K.op("dve", lambda e: e.tensor_tensor(out=h[:], in0=scr[:], in1=sh[:], op=ALU.add), reads=[t_scr, t_sh], writes=[t_h])


def transpose_to(K, src, t_src, nchunks, ident, t_id, pst, t_pst, dst, t_dst, eng="act"):
    for c in range(nchunks):
        K.op("pe", lambda e, c=c: e.transpose(out=pst[:, c, :], in_=src[:, c * 128:(c + 1) * 128], identity=ident[:]),
             reads=[t_src, t_id], writes=[t_pst])
    if eng == "act":
        K.op("act", lambda e: e.copy(out=dst, in_=pst[:, 0:nchunks, :]), reads=[t_pst], writes=[t_dst])
    else:
        K.op("dve", lambda e: e.tensor_copy(out=dst, in_=pst[:, 0:nchunks, :]), reads=[t_pst], writes=[t_dst])


def gen_proj_in(K, A, l, xs, t_xs, modscr, t_mod, pscr, t_p):
    ident, t_id = make_ident(K, BF16)
    mods = load_mod(K, modscr, t_mod, A["norm_mix_g"][l, :], 0, 1)
    wb = K.sb([128, 8, IN_COLS], BF16); t_wb = Trk()
    stg = [K.sb([128, 8, 512]) for _ in range(2)]; t_stg = [Trk(), Trk()]
    blocks = [(c0, min(512, IN_COLS - c0)) for c0 in range(0, IN_COLS, 512)]
    for i, (c0, n) in enumerate(blocks):
        s = stg[i % 2]; ts = t_stg[i % 2]
        K.dma(s[:, :, 0:n], A["w_in"][l, :, c0:c0 + n].rearrange("(k p) n -> p k n", p=128), writes=[ts])
        K.op("pool", lambda e, s=s, c0=c0, n=n: e.tensor_copy(out=wb[:, :, c0:c0 + n], in_=s[:, :, 0:n]), reads=[ts], writes=[t_wb])
    xt = [K.sb([128, D]) for _ in range(2)]; t_xt = [Trk(), Trk()]
    scr = K.sb([128, D]); t_scr = Trk()
    ss = K.sb([128, 1]); t_ss = Trk()
    h = K.sb([128, D], BF16); t_h = Trk()
    hT = [K.sb([128, 8, 128], BF16) for _ in range(2)]; t_hT = [Trk(), Trk()]
    pst = K.ps([128, 8, 128], BF16); t_pst = Trk()
    pp = [K.ps([128, 512]) for _ in range(3)]; t_pp = [Trk() for _ in range(3)]
    po = [K.sb([128, IN_COLS]) for _ in range(2)]; t_po = [Trk(), Trk()]
    nmm = [0]

    def body(tt):
        x_ = xt[tt % 2]; tx = t_xt[tt % 2]
        K.dma(x_[:], xs[tt * 128:(tt + 1) * 128, :], reads=[t_xs], writes=[tx])
        norm_mod_tile(K, x_[:], tx, mods[1 if tt < NCTX_T else 0], h, t_h, scr, t_scr, ss, t_ss)
        hT_ = hT[tt % 2]; thT = t_hT[tt % 2]
        transpose_to(K, h, t_h, 8, ident, t_id, pst, t_pst, hT_[:], thT)
        o = po[tt % 2]; to = t_po[tt % 2]
        for i, (c0, n) in enumerate(blocks):
            p_ = pp[nmm[0] % 3]; tp = t_pp[nmm[0] % 3]; nmm[0] += 1
            for k in range(8):
                K.op("pe", lambda e, k=k, p_=p_, c0=c0, n=n: e.matmul(p_[:, 0:n], lhsT=hT_[:, k, :], rhs=wb[:, k, c0:c0 + n], start=(k == 0), stop=(k == 7)),
                     reads=[thT, t_wb], writes=[tp])
            if i % 2 == 0:
                K.op("act", lambda e, p_=p_, c0=c0, n=n: e.copy(out=o[:, c0:c0 + n], in_=p_[:, 0:n]), reads=[tp], writes=[to])
            else:
                K.op("dve", lambda e, p_=p_, c0=c0, n=n: e.tensor_copy(out=o[:, c0:c0 + n], in_=p_[:, 0:n]), reads=[tp], writes=[to])
        K.dma(pscr[tt * 128:(tt + 1) * 128, :], o[:], reads=[to], writes=[t_p])

    yield "proj_in_%d" % l
    for tt in range(NT):
        body(tt)
        yield


def norm_rope_heads(K, src, t_src, H, dh, g, t_g, rope, t_rope, rdim, roff, out_bf, t_out, W):
    sq3 = W["sq"][:, 0:H * dh].rearrange("p (h d) -> p h d", h=H)
    qn = W["qn"][:, 0:H * dh].rearrange("p (h d) -> p h d", h=H)
    ssq = W["ssq"][:, 0:H]
    K.op("dve", lambda e: e.tensor_tensor(out=sq3, in0=src, in1=src, op=ALU.mult), reads=[t_src], writes=[W["t_sq"]])
    K.op("dve", lambda e: e.tensor_reduce(out=ssq, in_=sq3, axis=AX.X, op=ALU.add), reads=[W["t_sq"]], writes=[W["t_ssq"]])
    K.op("act", lambda e: e.activation(out=ssq, in_=ssq, func=AF.Sqrt, scale=1.0 / dh, bias=EPS), reads=[W["t_ssq"]], writes=[W["t_ssq"]])
    K.op("dve", lambda e: e.reciprocal(out=ssq, in_=ssq), reads=[W["t_ssq"]], writes=[W["t_ssq"]])
    K.op("dve", lambda e: e.tensor_tensor(out=qn, in0=src, in1=ssq.unsqueeze(2).broadcast_to([128, H, dh]), op=ALU.mult),
         reads=[t_src, W["t_ssq"]], writes=[W["t_qn"]])
    gb = g.unsqueeze(1).broadcast_to([128, H, dh])
    if rope is None:
        K.op("dve", lambda e: e.tensor_tensor(out=out_bf, in0=qn, in1=gb, op=ALU.mult), reads=[W["t_qn"], t_g], writes=[t_out])
        return
    K.op("dve", lambda e: e.tensor_tensor(out=qn, in0=qn, in1=gb, op=ALU.mult), reads=[W["t_qn"], t_g], writes=[W["t_qn"]])
    hf = rdim // 2
    xa = qn[:, :, roff:roff + hf]; xb = qn[:, :, roff + hf:roff + rdim]
    cos = rope[:, 0:hf].unsqueeze(1).broadcast_to([128, H, hf]); sin = rope[:, hf:rdim].unsqueeze(1).broadcast_to([128, H, hf])
    tm = [W["rt%d" % i][:, 0:H * hf].rearrange("p (h d) -> p h d", h=H) for i in range(4)]
    for i, (a_, b_) in enumerate([(xa, cos), (xb, sin), (xa, sin), (xb, cos)]):
        K.op("pool", lambda e, i=i, a_=a_, b_=b_: e.tensor_tensor(out=tm[i], in0=a_, in1=b_, op=ALU.mult),
             reads=[W["t_qn"], t_rope], writes=[W["t_rt%d" % i]])
    if roff > 0:
        K.op("act", lambda e: e.copy(out=out_bf[:, :, 0:roff], in_=qn[:, :, 0:roff]), reads=[W["t_qn"]], writes=[t_out])
    K.op("dve", lambda e: e.tensor_tensor(out=out_bf[:, :, roff:roff + hf], in0=tm[0], in1=tm[1], op=ALU.subtract),
         reads=[W["t_rt0"], W["t_rt1"]], writes=[t_out])
    K.op("dve", lambda e: e.tensor_tensor(out=out_bf[:, :, roff + hf:roff + rdim], in0=tm[2], in1=tm[3], op=ALU.add),
         reads=[W["t_rt2"], W["t_rt3"]], writes=[t_out])


def make_work(K, n):
    W = {}
    for nm, sh in [("sq", [128, n]), ("qn", [128, n]), ("ssq", [128, 8]), ("rt0", [128, n]), ("rt1", [128, n]), ("rt2", [128, n]), ("rt3", [128, n])]:
        W[nm] = K.sb(sh); W["t_" + nm] = Trk()
    return W


def attn_core(K, QT, t_QT, KT, t_KT, Vd, t_Vd, dq, heads, mixT, t_mix, need_ctx, R):
    ones, t_ones = R["ones"]
    ps_s, t_ps_s = R["ps_s"]
    PT, t_PT = R["PT"]; rden, t_rden = R["rden"]; osb, t_osb = R["osb"]
    blocks = [(256 + i * 512, 512, list(range(NT))) for i in range(4)]
    if need_ctx:
        blocks.append((0, 256, [0, 1]))
    st = R["cnt"]

    def unit(qh, kh, row0, q0, nq, kts):
        half = (row0 % 128) // 64
        po, tpo = R["ps_o"][st[1] % len(R["ps_o"])]; pd, tpd = R["ps_d"][st[1] % len(R["ps_d"])]
        for j, kt in enumerate(kts):
            ns = st[0]; st[0] += 1
            pS = ps_s[ns % 2]; tS = t_ps_s[ns % 2]; pt_ = PT[ns % 3]; tpt = t_PT[ns % 3]
            K.op("pe", lambda e, pS=pS, kt=kt: e.matmul(pS[:, 0:nq], lhsT=KT[0:dq, kh, kt * 128:(kt + 1) * 128], rhs=QT[0:dq, qh, q0:q0 + nq], start=True, stop=True),
                 reads=[t_KT, t_QT], writes=[tS])
            K.op("act", lambda e, pS=pS, pt_=pt_: e.activation(out=pt_[:, 0:nq], in_=pS[:, 0:nq], func=AF.Exp), reads=[tS], writes=[tpt])
            K.op("pe", lambda e, kt=kt, pt_=pt_, j=j: e.matmul(po[:, 0:nq], lhsT=Vd[:, kt, kh, :], rhs=pt_[:, 0:nq], start=(j == 0), stop=(j == len(kts) - 1)),
                 reads=[t_Vd, tpt], writes=[tpo])
            K.op("pe", lambda e, pt_=pt_, j=j: e.matmul(pd[:, 0:nq], lhsT=ones[:], rhs=pt_[:, 0:nq], start=(j == 0), stop=(j == len(kts) - 1)),
                 reads=[t_ones, tpt], writes=[tpd])
            if j % 2 == 1:
                yield
        nb = st[1]; st[1] += 1
        rd = rden[nb % 2]; trd = t_rden[nb % 2]; ob = osb[nb % 2]; tob = t_osb[nb % 2]
        K.op("dve", lambda e: e.reciprocal(out=rd[:, 0:nq], in_=pd[:, 0:nq]), reads=[tpd], writes=[trd])
        K.op("dve", lambda e: e.tensor_tensor(out=ob[:, 0:nq], in0=po[:, 0:nq], in1=rd[:, 0:nq], op=ALU.mult), reads=[tpo, trd], writes=[tob])
        K.dma(mixT[row0:row0 + 64, q0:q0 + nq], ob[half * 64:half * 64 + 64, 0:nq], reads=[tob], writes=[t_mix])

    for (qh, kh, row0) in heads:
        for (q0, nq, kts) in blocks:
            yield from unit(qh, kh, row0, q0, nq, kts)
            yield


def gen_attn(K, A, l, DS, mixT, t_mix, need_ctx, which):
    dq, nqh, nkh = (96, 4, 4) if which == "m" else (64, 6, 2)
    QT = K.sb([dq, nqh, NTOK], BF16); t_QT = Trk()
    KT = K.sb([dq, nkh, NTOK], BF16); t_KT = Trk()
    Vd = K.sb([128, NT, nkh, 128], BF16); t_Vd = Trk()
    ones = K.sb([128, 128], BF16); t_ones = Trk()
    K.op("pool", lambda e: e.memset(ones[:], 1.0), writes=[t_ones])
    R = {"ones": (ones, t_ones),
         "ps_s": ([K.ps([128, 512]) for _ in range(2)], [Trk(), Trk()]),
         "ps_o": [(K.ps([128, 512]), Trk())], "ps_d": [(K.ps([128, 512]), Trk())],
         "PT": ([K.sb([128, 512], BF16) for _ in range(3)], [Trk() for _ in range(3)]),
         "rden": ([K.sb([128, 512]) for _ in range(2)], [Trk(), Trk()]),
         "osb": ([K.sb([128, 512], BF16) for _ in range(2)], [Trk(), Trk()]), "cnt": [0, 0]}
    sfx = "m" if which == "m" else "g"
    yield ("attn_mla_%d" if which == "m" else "attn_gqa_%d") % l
    K.dma(QT[:], DS["qt" + sfx], reads=[DS["t_qt" + sfx]], writes=[t_QT])
    K.dma(KT[:], DS["kt" + sfx], reads=[DS["t_kt" + sfx]], writes=[t_KT])
    K.dma(Vd[:], DS["vd" + sfx].rearrange("(t p) h d -> p t h d", p=128), reads=[DS["t_vd" + sfx]], writes=[t_Vd])
    yield
    if which == "m":
        heads = [(h, h, h * 64) for h in range(4)]
    else:
        heads = [(g, g // 3, 640 + g * 64) for g in range(6)]
    yield from attn_core(K, QT, t_QT, KT, t_KT, Vd, t_Vd, dq, heads, mixT, t_mix, need_ctx, R)


def gen_mla_prep(K, A, l, pscr, t_p, DS):
    qkT = [K.sb([96, 2, 4, 128], BF16) for _ in range(2)]; t_qkT = [Trk(), Trk()]
    Vt = [K.sb([128, 4, 128], BF16) for _ in range(2)]; t_Vt = [Trk(), Trk()]
    ident, t_id = make_ident(K, BF16)
    W = make_work(K, 384)
    gq = K.sb([128, 256]); t_gq = Trk(); bcast_load(K, gq[:], A["mla_q_norm_g"][l, :], [], t_gq)
    gkv = K.sb([128, 128]); t_gkv = Trk(); bcast_load(K, gkv[:], A["mla_kv_norm_g"][l, :], [], t_gkv)
    gqk = K.sb([128, 2, 96]); t_gqk = Trk()
    K.dma(gqk[:], A["mla_qk_g"][l].partition_broadcast(128), writes=[t_gqk])
    K.op("dve", lambda e: e.tensor_scalar(out=gqk[:, 0, :], in0=gqk[:, 0, :], scalar1=96.0 ** -0.5, scalar2=None, op0=ALU.mult),
         reads=[t_gqk], writes=[t_gqk])
    wq_f = K.sb([128, 2, 384]); t_wqf = Trk()
    K.dma(wq_f[:], A["mla_w_qb"][l].rearrange("(k p) n -> p k n", p=128), writes=[t_wqf])
    wq = K.sb([128, 2, 384], BF16); t_wq = Trk()
    K.op("dve", lambda e: e.tensor_copy(out=wq[:], in_=wq_f[:]), reads=[t_wqf], writes=[t_wq])
    wkv_f = K.sb([128, 512]); t_wkvf = Trk()
    K.dma(wkv_f[:], A["mla_w_kvb"][l], writes=[t_wkvf])
    wkv = K.sb([128, 512], BF16); t_wkv = Trk()
    K.op("dve", lambda e: e.tensor_copy(out=wkv[:], in_=wkv_f[:]), reads=[t_wkvf], writes=[t_wkv])
    pm = [K.sb([128, 416]) for _ in range(2)]; t_pm = [Trk(), Trk()]
    rp = [K.sb([128, 32]) for _ in range(2)]; t_rp = [Trk(), Trk()]
    junk = K.sb([128, 256]); t_junk = Trk()
    ss = K.sb([128, 2]); t_ss = Trk()
    cn = K.sb([128, 384], BF16); t_cn = Trk()
    cT = K.sb([128, 3, 128], BF16); t_cT = Trk()
    pst = K.ps([128, 3, 128], BF16); t_pst = Trk()
    pq = K.ps([128, 384]); t_pq = Trk()
    pkv = K.ps([128, 512]); t_pkv = Trk()
    qs = K.sb([128, 4, 96]); t_qs = Trk()
    ks = K.sb([128, 4, 96]); t_ks = Trk()
    qb = K.sb([128, 4, 96], BF16); t_qb = Trk()
    kb = K.sb([128, 4, 96], BF16); t_kb = Trk()
    ptqk = K.ps([96, 2, 4, 128], BF16); t_ptq = Trk(); t_ptk = t_ptq
    ptq = ptqk[:, 0]; ptk = ptqk[:, 1]
    def body(tt):
        p_ = pm[tt % 2]; tp = t_pm[tt % 2]
        K.dma(p_[:], pscr[tt * 128:(tt + 1) * 128, C_MLA:C_MLA + 416], reads=[t_p], writes=[tp])
        rope = None; trp = None
        if tt >= NCTX_T:
            rope = rp[tt % 2]; trp = t_rp[tt % 2]
            K.dma(rope[:], A["rope_m"][(tt - 2) * 128:(tt - 1) * 128, :], writes=[trp])
        for j, (c0, n, g_, tg) in enumerate([(0, 256, gq, t_gq), (256, 128, gkv, t_gkv)]):
            K.op("act", lambda e, c0=c0, n=n, j=j: e.activation(out=junk[:, 0:n], in_=p_[:, c0:c0 + n], func=AF.Square, accum_out=ss[:, j:j + 1]),
                 reads=[tp], writes=[t_junk, t_ss])
            K.op("act", lambda e, n=n, j=j: e.activation(out=ss[:, j:j + 1], in_=ss[:, j:j + 1], func=AF.Sqrt, scale=1.0 / n, bias=EPS),
                 reads=[t_ss], writes=[t_ss])
            K.op("dve", lambda e, j=j: e.reciprocal(out=ss[:, j:j + 1], in_=ss[:, j:j + 1]), reads=[t_ss], writes=[t_ss])
            K.op("dve", lambda e, c0=c0, n=n, j=j, g_=g_: e.scalar_tensor_tensor(out=cn[:, c0:c0 + n], in0=p_[:, c0:c0 + n], scalar=ss[:, j:j + 1],
                                                                                in1=g_[:], op0=ALU.mult, op1=ALU.mult),
                 reads=[tp, t_ss, tg], writes=[t_cn])
        transpose_to(K, cn, t_cn, 3, ident, t_id, pst, t_pst, cT[:], t_cT)
        yield
        for k in range(2):
            K.op("pe", lambda e, k=k: e.matmul(pq[:], lhsT=cT[:, k, :], rhs=wq[:, k, :], start=(k == 0), stop=(k == 1)),
                 reads=[t_cT, t_wq], writes=[t_pq])
        K.op("pe", lambda e: e.matmul(pkv[:], lhsT=cT[:, 2, :], rhs=wkv[:], start=True, stop=True), reads=[t_cT, t_wkv], writes=[t_pkv])
        yield
        K.op("act", lambda e: e.copy(out=qs[:], in_=pq[:].rearrange("p (h d) -> p h d", h=4)), reads=[t_pq], writes=[t_qs])
        kv3 = pkv[:].rearrange("p (h d) -> p h d", h=4)
        K.op("act", lambda e: e.copy(out=ks[:, :, 0:64], in_=kv3[:, :, 0:64]), reads=[t_pkv], writes=[t_ks])
        K.op("pool", lambda e: e.tensor_copy(out=ks[:, :, 64:96], in_=p_[:, 384:416].unsqueeze(1).broadcast_to([128, 4, 32])),
             reads=[tp], writes=[t_ks])
        Vt_ = Vt[tt % 2]; tVt = t_Vt[tt % 2]
        K.op("act", lambda e: e.copy(out=Vt_[:, :, 0:64], in_=kv3[:, :, 64:128]), reads=[t_pkv], writes=[tVt])
        K.op("dve", lambda e: e.tensor_copy(out=Vt_[:, :, 64:128], in_=kv3[:, :, 64:128]), reads=[t_pkv], writes=[tVt])
        K.dma(DS["vdm"][tt * 128:(tt + 1) * 128], Vt_[:], reads=[tVt], writes=[DS["t_vdm"]])
        yield
        norm_rope_heads(K, qs[:], t_qs, 4, 96, gqk[:, 0, :], t_gqk, rope, trp, 32, 64, qb[:], t_qb, W)
        yield
        norm_rope_heads(K, ks[:], t_ks, 4, 96, gqk[:, 1, :], t_gqk, rope, trp, 32, 64, kb[:], t_kb, W)
        yield
        qk_ = qkT[tt % 2]; tqk = t_qkT[tt % 2]
        for i_, (src, ts, pt_) in enumerate([(qb, t_qb, ptq), (kb, t_kb, ptk)]):
            for h in range(4):
                K.op("pe", lambda e, h=h, src=src, pt_=pt_: e.transpose(out=pt_[:, h, :], in_=src[:, h, :], identity=ident[:]),
                     reads=[ts, t_id], writes=[t_ptq])
        K.op("act", lambda e: e.copy(out=qk_[:], in_=ptqk[:]), reads=[t_ptq], writes=[tqk])
        K.dma(DS["qtm"][:, :, tt * 128:(tt + 1) * 128], qk_[:, 0], reads=[tqk], writes=[DS["t_qtm"]])
        K.dma(DS["ktm"][:, :, tt * 128:(tt + 1) * 128], qk_[:, 1], reads=[tqk], writes=[DS["t_ktm"]])
    yield "mla_prep_%d" % l
    for tt in range(NT):
        yield from body(tt)
        yield


def gen_gqa_prep(K, A, l, pscr, t_p, DS):
    qkT = [K.sb([64, 8, 128], BF16) for _ in range(2)]; t_qkT = [Trk(), Trk()]
    Vt = [K.sb([128, 2, 128], BF16) for _ in range(2)]; t_Vt = [Trk(), Trk()]
    ident, t_id = make_ident(K, BF16)
    W = make_work(K, 384)
    gqk = K.sb([128, 2, 64]); t_gqk = Trk()
    K.dma(gqk[:], A["gqa_qk_g"][l].partition_broadcast(128), writes=[t_gqk])
    K.op("dve", lambda e: e.tensor_scalar(out=gqk[:, 0, :], in0=gqk[:, 0, :], scalar1=64.0 ** -0.5, scalar2=None, op0=ALU.mult),
         reads=[t_gqk], writes=[t_gqk])
    pg = [K.sb([128, 640]) for _ in range(2)]; t_pg = [Trk(), Trk()]
    rp = [K.sb([128, 64]) for _ in range(2)]; t_rp = [Trk(), Trk()]
    qb = K.sb([128, 6, 64], BF16); t_qb = Trk()
    kb = K.sb([128, 2, 64], BF16); t_kb = Trk()
    ptqk = K.ps([64, 8, 128], BF16); t_ptq = Trk(); t_ptk = t_ptq
    ptq = ptqk[:, 0:6]; ptk = ptqk[:, 6:8]
    def body(tt):
        p_ = pg[tt % 2]; tp = t_pg[tt % 2]
        K.dma(p_[:], pscr[tt * 128:(tt + 1) * 128, C_GQA:C_GQA + 640], reads=[t_p], writes=[tp])
        rope = None; trp = None
        if tt >= NCTX_T:
            rope = rp[tt % 2]; trp = t_rp[tt % 2]
            K.dma(rope[:], A["rope_g"][(tt - 2) * 128:(tt - 1) * 128, :], writes=[trp])
        q3 = p_[:, 0:384].rearrange("p (h d) -> p h d", h=6)
        k3 = p_[:, 384:512].rearrange("p (h d) -> p h d", h=2)
        v3 = p_[:, 512:640].rearrange("p (h d) -> p h d", h=2)
        Vt_ = Vt[tt % 2]; tVt = t_Vt[tt % 2]
        K.op("act", lambda e: e.copy(out=Vt_[:, :, 0:64], in_=v3), reads=[tp], writes=[tVt])
        K.op("pool", lambda e: e.tensor_copy(out=Vt_[:, :, 64:128], in_=v3), reads=[tp], writes=[tVt])
        K.dma(DS["vdg"][tt * 128:(tt + 1) * 128], Vt_[:], reads=[tVt], writes=[DS["t_vdg"]])
        yield
        norm_rope_heads(K, q3, tp, 6, 64, gqk[:, 0, :], t_gqk, rope, trp, 64, 0, qb[:], t_qb, W)
        yield
        norm_rope_heads(K, k3, tp, 2, 64, gqk[:, 1, :], t_gqk, rope, trp, 64, 0, kb[:], t_kb, W)
        yield
        qk_ = qkT[tt % 2]; tqk = t_qkT[tt % 2]
        for (src, ts, pt_, H) in [(qb, t_qb, ptq, 6), (kb, t_kb, ptk, 2)]:
            for h in range(H):
                K.op("pe", lambda e, h=h, src=src, pt_=pt_: e.transpose(out=pt_[:, h, :], in_=src[:, h, :], identity=ident[:]),
                     reads=[ts, t_id], writes=[t_ptq])
        K.op("act", lambda e: e.copy(out=qk_[:], in_=ptqk[:]), reads=[t_ptq], writes=[tqk])
        K.dma(DS["qtg"][:, :, tt * 128:(tt + 1) * 128], qk_[:, 0:6], reads=[tqk], writes=[DS["t_qtg"]])
        K.dma(DS["ktg"][:, :, tt * 128:(tt + 1) * 128], qk_[:, 6:8], reads=[tqk], writes=[DS["t_ktg"]])
    yield "gqa_prep_%d" % l
    for tt in range(NT):
        yield from body(tt)
        yield


def gen_rw_prep(K, A, l, pscr, t_p, rwA, t_rwA):
    identf, t_idf = make_ident(K, F32)
    RW = 1568
    mu0 = K.sb([128, RW]); mu1 = K.sb([128, RW]); cm = K.sb([128, RW]); t_mu = Trk()
    K.dma(mu0[:], A["rw_mu"][l, 0, :].partition_broadcast(128), writes=[t_mu])
    K.dma(mu1[:], A["rw_mu"][l, 1, :].partition_broadcast(128), writes=[t_mu])
    K.op("dve", lambda e: e.tensor_tensor(out=cm[:], in0=mu0[:], in1=mu1[:], op=ALU.add), reads=[t_mu], writes=[t_mu])
    K.op("dve", lambda e: e.tensor_scalar(out=cm[:], in0=cm[:], scalar1=-1.0, scalar2=1.0, op0=ALU.mult, op1=ALU.add), reads=[t_mu], writes=[t_mu])
    t_c = Trk()
    w0 = K.sb([128, 2, 384]); K.dma(w0[:], A["rw_w0"][l].partition_broadcast(128), writes=[t_c])
    a0 = K.sb([128, 2, 384]); K.dma(a0[:], A["rw_a0"][l].partition_broadcast(128), writes=[t_c])
    kk_ = K.sb([128, 384]); K.dma(kk_[:], A["rw_k_k"][l, :].partition_broadcast(128), writes=[t_c])
    ka_ = K.sb([128, 384]); K.dma(ka_[:], A["rw_k_a"][l, :].partition_broadcast(128), writes=[t_c])
    rk_ = K.sb([128, 384]); K.dma(rk_[:], A["rw_r_k"][l, :].partition_broadcast(128), writes=[t_c])
    W2 = K.sb([128, 384]); K.dma(W2[:], A["rw_w2"][l].rearrange("d r c -> (d r) c"), writes=[t_c])
    A2 = K.sb([128, 384]); K.dma(A2[:], A["rw_a2"][l].rearrange("d r c -> (d r) c"), writes=[t_c])
    G2 = K.sb([128, 384]); K.dma(G2[:], A["rw_g2"][l, 0:128, :], writes=[t_c])
    G2b = K.sb([32, 384]); K.dma(G2b[:], A["rw_g2"][l, 128:160, :], writes=[t_c])
    pc = [K.sb([128, RW]) for _ in range(2)]; t_pc = [Trk(), Trk()]
    pp = [K.sb([128, RW]) for _ in range(2)]; t_pp = [Trk(), Trk()]
    pn = [K.sb([128, RW]) for _ in range(2)]; t_pn = [Trk(), Trk()]
    xs = K.sb([128, RW]); t_xs = Trk()
    u1 = K.sb([128, RW]); t_u1 = Trk()
    u2 = K.sb([128, RW]); t_u2 = Trk()
    tw = K.sb([128, 416]); t_tw = Trk()
    lT = K.sb([128, 4, 128]); t_lT = Trk()
    pst = K.ps([128, 4, 128]); t_pst = Trk()
    plb = [K.ps([128, 512]) for _ in range(2)]; t_plb = [Trk(), Trk()]
    lwt = [K.sb([128, 384]) for _ in range(2)]; t_lwt = [Trk(), Trk()]
    npl = [0]
    stg = [K.sb([128, 11, 384]) for _ in range(2)]; t_stg = [Trk(), Trk()]
    ad = K.sb([128, 2, 384]); t_ad = Trk()
    tmp = K.sb([128, 384]); t_tmp = Trk()
    tmp2 = K.sb([128, 384]); t_tmp2 = Trk()
    ssq = K.sb([128, 6]); t_ssq = Trk()
    NEG = -float(np.exp(-0.5))

    def body(tt):
        c_ = pc[tt % 2]; tc = t_pc[tt % 2]; p_ = pp[tt % 2]; tp = t_pp[tt % 2]; n_ = pn[tt % 2]; tn = t_pn[tt % 2]
        r0 = tt * 128
        K.dma(c_[:], pscr[r0:r0 + 128, C_RW:C_RW + RW], reads=[t_p], writes=[tc])
        if tt in (0, NCTX_T):
            K.op("pool", lambda e: e.memset(p_[:], 0.0), writes=[tp])
            K.dma(p_[1:128, :], pscr[r0:r0 + 127, C_RW:C_RW + RW], reads=[t_p], writes=[tp])
        else:
            K.dma(p_[:], pscr[r0 - 1:r0 + 127, C_RW:C_RW + RW], reads=[t_p], writes=[tp])
        if tt in (NCTX_T - 1, NT - 1):
            K.op("pool", lambda e: e.memset(n_[:], 0.0), writes=[tn])
            K.dma(n_[0:127, :], pscr[r0 + 1:r0 + 128, C_RW:C_RW + RW], reads=[t_p], writes=[tn])
        else:
            K.dma(n_[:], pscr[r0 + 1:r0 + 129, C_RW:C_RW + RW], reads=[t_p], writes=[tn])
        K.op("dve", lambda e: e.tensor_tensor(out=xs[:], in0=c_[:], in1=cm[:], op=ALU.mult), reads=[tc, t_mu], writes=[t_xs])
        K.op("pool", lambda e: e.tensor_tensor(out=u1[:], in0=p_[:], in1=mu0[:], op=ALU.mult), reads=[tp, t_mu], writes=[t_u1])
        K.op("pool", lambda e: e.tensor_tensor(out=u2[:], in0=n_[:], in1=mu1[:], op=ALU.mult), reads=[tn, t_mu], writes=[t_u2])
        K.op("dve", lambda e: e.tensor_tensor(out=xs[:], in0=xs[:], in1=u1[:], op=ALU.add), reads=[t_xs, t_u1], writes=[t_xs])
        K.op("dve", lambda e: e.tensor_tensor(out=xs[:], in0=xs[:], in1=u2[:], op=ALU.add), reads=[t_xs, t_u2], writes=[t_xs])
        yield
        r = xs[:, 0:384]; k = xs[:, 384:768]; v = xs[:, 768:1152]
        K.op("act", lambda e: e.activation(out=tw[:, 0:128], in_=xs[:, 1152:1280], func=AF.Tanh), reads=[t_xs], writes=[t_tw])
        K.op("act", lambda e: e.copy(out=tw[:, 128:256], in_=xs[:, 1280:1408]), reads=[t_xs], writes=[t_tw])
        K.op("act", lambda e: e.activation(out=tw[:, 256:416], in_=xs[:, 1408:1568], func=AF.Sigmoid), reads=[t_xs], writes=[t_tw])
        for c in range(3):
            K.op("pe", lambda e, c=c: e.transpose(out=pst[:, c, :], in_=tw[:, c * 128:(c + 1) * 128], identity=identf[:]),
                 reads=[t_tw, t_idf], writes=[t_pst])
        K.op("pe", lambda e: e.transpose(out=pst[0:32, 3, :], in_=tw[:, 384:416], identity=identf[:]), reads=[t_tw, t_idf], writes=[t_pst])
        K.op("act", lambda e: e.copy(out=lT[:, 0:3, :], in_=pst[:, 0:3, :]), reads=[t_pst], writes=[t_lT])
        K.op("act", lambda e: e.copy(out=lT[0:32, 3, :], in_=pst[0:32, 3, :]), reads=[t_pst], writes=[t_lT])
        yield
        s_ = stg[tt % 2]; ts = t_stg[tt % 2]

        def nextpl():
            i_ = npl[0] % 2
            npl[0] += 1
            return plb[i_][:, 0:384], t_plb[i_]

        for d in range(2):
            p_w, t_w = nextpl()
            K.op("pe", lambda e, d=d, p_w=p_w: e.matmul(p_w, lhsT=lT[d * 64:(d + 1) * 64, 0, :], rhs=W2[d * 64:(d + 1) * 64, :], start=True, stop=True),
                 reads=[t_lT, t_c], writes=[t_w])
            K.op("dve", lambda e, d=d, p_w=p_w: e.tensor_tensor(out=lwt[d][:], in0=p_w, in1=w0[:, d, :], op=ALU.add), reads=[t_w, t_c], writes=[t_lwt[d]])
            K.op("act", lambda e, d=d: e.activation(out=lwt[d][:], in_=lwt[d][:], func=AF.Sigmoid), reads=[t_lwt[d]], writes=[t_lwt[d]])
            K.op("act", lambda e, d=d: e.mul(out=s_[:, 5 + 3 * d, :], in_=lwt[d][:], mul=NEG), reads=[t_lwt[d]], writes=[ts])
            p_a, t_a_ = nextpl()
            K.op("pe", lambda e, d=d, p_a=p_a: e.matmul(p_a, lhsT=lT[d * 64:(d + 1) * 64, 1, :], rhs=A2[d * 64:(d + 1) * 64, :], start=True, stop=True),
                 reads=[t_lT, t_c], writes=[t_a_])
            K.op("dve", lambda e, d=d, p_a=p_a: e.tensor_tensor(out=ad[:, d, :], in0=p_a, in1=a0[:, d, :], op=ALU.add), reads=[t_a_, t_c], writes=[t_ad])
            K.op("act", lambda e, d=d: e.activation(out=ad[:, d, :], in_=ad[:, d, :], func=AF.Sigmoid), reads=[t_ad], writes=[t_ad])
        p_g, t_g_ = nextpl()
        K.op("pe", lambda e: e.matmul(p_g, lhsT=lT[:, 2, :], rhs=G2[:], start=True, stop=False), reads=[t_lT, t_c], writes=[t_g_])
        K.op("pe", lambda e: e.matmul(p_g, lhsT=lT[0:32, 3, :], rhs=G2b[:], start=False, stop=True), reads=[t_lT, t_c], writes=[t_g_])
        K.op("act", lambda e: e.copy(out=s_[:, 3, :], in_=p_g), reads=[t_g_], writes=[ts])
        K.op("act", lambda e: e.copy(out=s_[:, 0, :], in_=r), reads=[t_xs], writes=[ts])
        K.op("act", lambda e: e.copy(out=s_[:, 1, :], in_=v), reads=[t_xs], writes=[ts])
        yield
        K.op("dve", lambda e: e.tensor_tensor(out=tmp[:], in0=k, in1=kk_[:], op=ALU.mult), reads=[t_xs, t_c], writes=[t_tmp])
        K.op("dve", lambda e: e.tensor_tensor(out=tmp2[:], in0=tmp[:], in1=tmp[:], op=ALU.mult), reads=[t_tmp], writes=[t_tmp2])
        K.op("dve", lambda e: e.tensor_reduce(out=ssq[:], in_=tmp2[:].rearrange("p (h n) -> p h n", h=6), axis=AX.X, op=ALU.add),
             reads=[t_tmp2], writes=[t_ssq])
        K.op("act", lambda e: e.activation(out=ssq[:], in_=ssq[:], func=AF.Sqrt, scale=1.0, bias=1e-12), reads=[t_ssq], writes=[t_ssq])
        K.op("dve", lambda e: e.reciprocal(out=ssq[:], in_=ssq[:]), reads=[t_ssq], writes=[t_ssq])
        K.op("dve", lambda e: e.tensor_tensor(out=s_[:, 2, :].rearrange("p (h n) -> p h n", h=6), in0=tmp[:].rearrange("p (h n) -> p h n", h=6),
                                              in1=ssq[:].unsqueeze(2).broadcast_to([128, 6, 64]), op=ALU.mult),
             reads=[t_tmp, t_ssq], writes=[ts])
        yield
        for d in range(2):
            K.op("dve", lambda e, d=d: e.scalar_tensor_tensor(out=tmp2[:], in0=ad[:, d, :], scalar=1.0, in1=ka_[:], op0=ALU.subtract, op1=ALU.mult),
                 reads=[t_ad, t_c], writes=[t_tmp2])
            K.op("dve", lambda e, d=d: e.scalar_tensor_tensor(out=s_[:, 6 + 3 * d, :], in0=tmp2[:], scalar=1.0, in1=k, op0=ALU.add, op1=ALU.mult),
                 reads=[t_tmp2, t_xs], writes=[ts])
            K.op("dve", lambda e, d=d: e.tensor_tensor(out=s_[:, 7 + 3 * d, :], in0=s_[:, 2, :], in1=ad[:, d, :], op=ALU.mult), reads=[ts, t_ad], writes=[ts])
            yield
        K.op("dve", lambda e: e.tensor_tensor(out=tmp[:], in0=s_[:, 6, :], in1=s_[:, 9, :], op=ALU.add), reads=[ts], writes=[t_tmp])
        K.op("dve", lambda e: e.tensor_tensor(out=tmp[:], in0=tmp[:], in1=rk_[:], op=ALU.mult), reads=[t_tmp, t_c], writes=[t_tmp])
        K.op("dve", lambda e: e.tensor_tensor(out=tmp[:], in0=tmp[:], in1=r, op=ALU.mult), reads=[t_tmp, t_xs], writes=[t_tmp])
        K.op("dve", lambda e: e.tensor_reduce(out=ssq[:], in_=tmp[:].rearrange("p (h n) -> p h n", h=6), axis=AX.X, op=ALU.add),
             reads=[t_tmp], writes=[t_ssq])
        K.op("dve", lambda e: e.tensor_tensor(out=s_[:, 4, :].rearrange("p (h n) -> p h n", h=6), in0=v.rearrange("p (h n) -> p h n", h=6),
                                              in1=ssq[:].unsqueeze(2).broadcast_to([128, 6, 64]), op=ALU.mult),
             reads=[t_xs, t_ssq], writes=[ts])
        K.dma(rwA[r0:r0 + 128, :, :], s_[:], reads=[ts], writes=[t_rwA])

    yield "rw_prep_%d" % l
    for tt in range(NT):
        yield from body(tt)
        yield


RW_FAST = [True]


class Banks:
    def __init__(self, K, n_rot=7):
        nb = (n_rot + 2) // 2
        self.t = [K.ps([128, 1024]) for _ in range(nb)]
        self.trk = [Trk() for _ in range(2 * nb)]
        self.i1 = 0
        self.i2 = 0
        self.n_rot = n_rot

    def bank(self, b):
        return self.t[b // 2][:, (b % 2) * 512:(b % 2) * 512 + 512], self.trk[b]

    def single(self):
        b = self.i1 % self.n_rot
        self.i1 += 1
        return self.bank(b)

    def double(self):
        p = self.i2 % 3
        self.i2 += 1
        return self.t[p], [self.trk[2 * p], self.trk[2 * p + 1]]


F32R_ = mybir.dt.float32r


def rw_consts(K, A, l):
    identf, t_idf = make_ident(K, F32)
    t_c = Trk()
    ones3 = K.sb([128, 3, 128])
    K.op("pool", lambda e: e.memset(ones3[:], 1.0), writes=[t_c])
    m_inc3 = K.sb([128, 3, 128]); m_str3 = K.sb([128, 3, 128]); id3 = K.sb([128, 3, 128]); Jm = K.sb([128, 128])
    blk = K.sb([128, 128]); cind = K.sb([128, 2])
    pat3 = [[0, 3], [1, 128]]
    K.op("pool", lambda e: e.affine_select(out=m_inc3[:], in_=ones3[:], pattern=pat3, compare_op=ALU.is_ge, fill=0.0, base=0, channel_multiplier=-1), reads=[t_c], writes=[t_c])
    K.op("pool", lambda e: e.affine_select(out=m_str3[:], in_=ones3[:], pattern=pat3, compare_op=ALU.is_ge, fill=0.0, base=-1, channel_multiplier=-1), reads=[t_c], writes=[t_c])
    K.op("pool", lambda e: e.affine_select(out=id3[:], in_=ones3[:], pattern=pat3, compare_op=ALU.is_equal, fill=0.0, base=0, channel_multiplier=-1), reads=[t_c], writes=[t_c])
    K.op("pool", lambda e: e.affine_select(out=Jm[:], in_=ones3[:, 0, :], pattern=[[1, 128]], compare_op=ALU.is_equal, fill=0.0, base=-127, channel_multiplier=1), reads=[t_c], writes=[t_c])
    K.op("pool", lambda e: e.memset(m_inc3[0:64, :, 64:128], 0.0), reads=[t_c], writes=[t_c])
    K.op("pool", lambda e: e.memset(m_str3[0:64, :, 64:128], 0.0), reads=[t_c], writes=[t_c])
    K.op("pool", lambda e: e.memset(blk[:], 0.0), writes=[t_c])
    K.op("pool", lambda e: e.memset(blk[0:64, 0:64], 1.0), reads=[t_c], writes=[t_c])
    K.op("pool", lambda e: e.memset(blk[64:128, 64:128], 1.0), reads=[t_c], writes=[t_c])
    K.op("pool", lambda e: e.memset(cind[:], 0.0), writes=[t_c])
    K.op("pool", lambda e: e.memset(cind[0:64, 0:1], 1.0), reads=[t_c], writes=[t_c])
    K.op("pool", lambda e: e.memset(cind[64:128, 1:2], 1.0), reads=[t_c], writes=[t_c])
    tri = m_inc3[:, 0, :]

    return dict(identf=identf, t_idf=t_idf, t_c=t_c, m_inc3=m_inc3, m_str3=m_str3, id3=id3, Jm=Jm, blk=blk, cind=cind, tri=tri)


def gen_rw_scan(K, A, l, C, d, rwA, t_rwA, ydst, t_yd, n_rot=3):
    identf = C["identf"]; t_idf = C["t_idf"]; t_c = C["t_c"]; m_inc3 = C["m_inc3"]; m_str3 = C["m_str3"]; id3 = C["id3"]
    Jm = C["Jm"]; blk = C["blk"]; cind = C["cind"]; tri = C["tri"]
    B = Banks(K, n_rot=n_rot)
    psY1, t_psY1 = B.bank(n_rot)
    a_in = [K.sb([128, 6, 384])]; t_a = [Trk()]
    if d == 1:
        af = K.sb([128, 6, 384]); t_af = Trk()
    names = ["cum", "d0", "d1", "e0", "em", "ep", "eh", "At", "Bt", "Kt", "Rt", "Bh", "Kh", "X1", "Z", "U", "Y1", "Y", "Vr"]
    S_ = {n: K.sb([128, 384]) for n in names}; T_ = {n: Trk() for n in names}
    fT = {n: K.sb([64, 6, 128]) for n in ["AtT", "BtT", "KtT", "RtT", "WmT"]}; t_fT = {n: Trk() for n in fT}
    gr = {n: K.sb([128, 6, 128]) for n in ["N", "LakT", "RbT", "RkT"]}; t_gr = {n: Trk() for n in gr}
    Nk = [K.sb([128, 6, 128]) for _ in range(2)]; t_Nk = [Trk(), Trk()]
    NkT = [K.sb([128, 6, 128]) for _ in range(2)]; t_NkT = [Trk(), Trk()]
    TT = K.sb([128, 6, 128]); t_TT = Trk()
    gamT = K.sb([64, 6, 2]); t_gam = Trk()
    M = K.sb([64, 6, 64]); t_M = Trk()
    Mt = K.sb([64, 6, 64]); t_Mt = Trk()
    cnt = [0]

    def RD(ap, r):
        return ap.bitcast(F32R_) if (r and RW_FAST[0]) else ap

    def ev(out, in_, reads, writes, r=False):
        out = RD(out, r)
        cnt[0] += 1
        if cnt[0] % 2:
            K.op("act", lambda e: e.copy(out=out, in_=in_), reads=reads, writes=writes)
        else:
            K.op("dve", lambda e: e.tensor_copy(out=out, in_=in_), reads=reads, writes=writes)

    def mm(out, lhsT, rhs, reads, writes, start=True, stop=True, fast=None):
        if fast is None:
            fast = RW_FAST[0]
        if fast and out.start_partition() == 0 and lhsT.start_partition() == 0:
            lhsT = lhsT.bitcast(mybir.dt.float32r); rhs = rhs.bitcast(mybir.dt.float32r)
        K.op("pe", lambda e: e.matmul(out, lhsT=lhsT, rhs=rhs, start=start, stop=stop), reads=reads, writes=writes)

    def tt_op(eng, out, in0, in1, op, reads, writes, r=False):
        out = RD(out, r)
        K.op(eng, lambda e: e.tensor_tensor(out=out, in0=in0, in1=in1, op=op), reads=reads, writes=writes)

    def h3(ap):
        return ap.rearrange("p (h n) -> p h n", n=128)

    def tile(tt, d, idx):
        r0 = tt * 128
        a_ = a_in[0]; ta = t_a[0]
        K.dma(a_[:, 0:3, :], rwA[r0:r0 + 128, 0:3, :], reads=[t_rwA], writes=[ta])
        K.dma(a_[:, 3:6, :], rwA[r0:r0 + 128, 5 + 3 * d:8 + 3 * d, :], reads=[t_rwA], writes=[ta])
        if d == 1:
            for i in range(6):
                pb, tb = B.single()
                mm(pb[:, 0:384], Jm[:], a_[:, i, :], [t_c, ta], [tb], fast=False)
                ev(af[:, i, :], pb[:, 0:384], [tb], [t_af])
            X = af; tX = t_af
        else:
            X = a_; tX = ta
        R_, V_, KK_, LW_, KD_, BD_ = [X[:, i, :] for i in range(6)]
        K.op("act", lambda e: e.copy(out=RD(S_["Vr"][:], True), in_=V_), reads=[tX], writes=[T_["Vr"]])
        Vh = lambda h: S_["Vr"][:, h * 64:(h + 1) * 64]
        pcum, tcum = B.single(); ptot, ttot = B.single(); pgT, tgT = B.single()
        mm(pcum[:, 0:384], tri, LW_, [t_c, tX], [tcum], fast=False)
        mm(ptot[:, 0:384], blk[:], LW_, [t_c, tX], [ttot], fast=False)
        gview = pgT[0:64, 0:12].rearrange("p (h c) -> p h c", c=2)
        for h in range(6):
            mm(gview[:, h, :], X[:, 3, h * 64:(h + 1) * 64], cind[:], [tX, t_c], [tgT], fast=False)
        K.op("act", lambda e: e.activation(out=gamT[:], in_=gview, func=AF.Exp), reads=[tgT], writes=[t_gam])
        K.op("act", lambda e: e.copy(out=S_["cum"][:], in_=pcum[:, 0:384]), reads=[tcum], writes=[T_["cum"]])
        tt_op("dve", S_["d0"][:], S_["cum"][:], LW_, ALU.subtract, [T_["cum"], tX], [T_["d0"]])
        tt_op("dve", S_["d1"][:], ptot[:, 0:384], S_["cum"][:], ALU.subtract, [ttot, T_["cum"]], [T_["d1"]])
        K.op("act", lambda e: e.activation(out=S_["e0"][:], in_=S_["d0"][:], func=AF.Exp), reads=[T_["d0"]], writes=[T_["e0"]])
        K.op("act", lambda e: e.activation(out=S_["em"][:], in_=S_["cum"][:], func=AF.Exp, scale=-1.0), reads=[T_["cum"]], writes=[T_["em"]])
        K.op("act", lambda e: e.activation(out=S_["ep"][:], in_=S_["cum"][:], func=AF.Exp), reads=[T_["cum"]], writes=[T_["ep"]])
        K.op("act", lambda e: e.activation(out=S_["eh"][:], in_=S_["d1"][:], func=AF.Exp), reads=[T_["d1"]], writes=[T_["eh"]])
        yield
        K.op("dve", lambda e: e.scalar_tensor_tensor(out=RD(S_["At"][:], True), in0=KK_, scalar=-1.0, in1=S_["e0"][:], op0=ALU.mult, op1=ALU.mult),
             reads=[tX, T_["e0"]], writes=[T_["At"]])
        tt_op("pool", S_["Bt"][:], BD_, S_["em"][:], ALU.mult, [tX, T_["em"]], [T_["Bt"]], r=True)
        tt_op("dve", S_["Kt"][:], KD_, S_["em"][:], ALU.mult, [tX, T_["em"]], [T_["Kt"]], r=True)
        tt_op("pool", S_["Rt"][:], R_, S_["ep"][:], ALU.mult, [tX, T_["ep"]], [T_["Rt"]], r=True)
        tt_op("dve", S_["Bh"][:], BD_, S_["eh"][:], ALU.mult, [tX, T_["eh"]], [T_["Bh"]], r=True)
        tt_op("pool", S_["Kh"][:], KD_, S_["eh"][:], ALU.mult, [tX, T_["eh"]], [T_["Kh"]], r=True)
        yield
        for src, dst in [("At", "AtT"), ("Bt", "BtT"), ("Kt", "KtT"), ("Rt", "RtT")]:
            for g in range(2):
                pb, tb = B.single()
                v = pb[0:64, 0:384].rearrange("p (h n) -> p h n", n=128)
                for j in range(3):
                    h = g * 3 + j
                    K.op("pe", lambda e, h=h, j=j, v=v, src=src: e.transpose(out=v[:, j, :], in_=S_[src][:, h * 64:(h + 1) * 64], identity=identf[:]),
                         reads=[T_[src], t_idf], writes=[tb])
                ev(fT[dst][:, g * 3:g * 3 + 3, :], v, [tb], [t_fT[dst]], r=True)
                yield
        for nm, a, b, msk in [("N", "BtT", "AtT", m_str3), ("LakT", "KtT", "AtT", m_str3), ("RbT", "BtT", "RtT", m_inc3), ("RkT", "KtT", "RtT", m_inc3)]:
            for g in range(2):
                pb, tb = B.single()
                v = h3(pb[:, 0:384])
                for j in range(3):
                    h = g * 3 + j
                    mm(v[:, j, :], fT[a][:, h, :], fT[b][:, h, :], [t_fT[a], t_fT[b]], [tb])
                tt_op("dve", gr[nm][:, g * 3:g * 3 + 3, :], v, msk[:], ALU.mult, [tb, t_c], [t_gr[nm]], r=True)
                yield
        cur = 0
        K.op("pool", lambda e: e.tensor_copy(out=RD(Nk[0][:], True), in_=gr["N"][:]), reads=[t_gr["N"]], writes=[t_Nk[0]])
        for g in range(2):
            pb, tb = B.single()
            v = h3(pb[:, 0:384])
            for j in range(3):
                h = g * 3 + j
                K.op("pe", lambda e, h=h, j=j, v=v: e.transpose(out=v[:, j, :], in_=gr["N"][:, h, :], identity=identf[:]),
                     reads=[t_gr["N"], t_idf], writes=[tb])
            ev(NkT[0][:, g * 3:g * 3 + 3, :], v, [tb], [t_NkT[0]], r=True)
            tt_op("dve", TT[:, g * 3:g * 3 + 3, :], gr["N"][:, g * 3:g * 3 + 3, :], id3[:], ALU.add, [t_gr["N"], t_c], [t_TT], r=True)
            yield
        for lev in range(5):
            nx = 1 - cur
            last = lev == 4
            for g in range(2):
                pb, tb = B.single()
                vb = h3(pb[:, 0:384])
                for j in range(3):
                    h = g * 3 + j
                    mm(vb[:, j, :], Nk[cur][:, h, :], NkT[cur][:, h, :], [t_NkT[cur], t_Nk[cur]], [tb])
                ev(NkT[nx][:, g * 3:g * 3 + 3, :], vb, [tb], [t_NkT[nx]], r=True)
                yield
                if not last:
                    pa, ta_ = B.single()
                    va = h3(pa[:, 0:384])
                    for j in range(3):
                        h = g * 3 + j
                        mm(va[:, j, :], NkT[cur][:, h, :], Nk[cur][:, h, :], [t_NkT[cur], t_Nk[cur]], [ta_])
                    ev(Nk[nx][:, g * 3:g * 3 + 3, :], va, [ta_], [t_Nk[nx]], r=True)
                    yield
            for g in range(2):
                pc_, tc_ = B.single()
                vc = h3(pc_[:, 0:384])
                for j in range(3):
                    h = g * 3 + j
                    mm(vc[:, j, :], NkT[nx][:, h, :], TT[:, h, :], [t_NkT[nx], t_TT], [tc_])
                tt_op("dve", TT[:, g * 3:g * 3 + 3, :], vc, TT[:, g * 3:g * 3 + 3, :], ALU.add, [tc_, t_TT], [t_TT], r=True)
                yield
            cur = nx
        pb, tb = B.single()
        for h in range(6):
            mm(pb[:, h * 64:(h + 1) * 64], gr["LakT"][:, h, :], Vh(h), [t_gr["LakT"], T_["Vr"]], [tb])
        ev(S_["X1"][:], pb[:, 0:384], [tb], [T_["X1"]], r=True)
        yield
        pb, tb = B.single()
        for h in range(6):
            mm(pb[:, h * 64:(h + 1) * 64], TT[:, h, :], S_["X1"][:, h * 64:(h + 1) * 64], [t_TT, T_["X1"]], [tb])
        ev(S_["Z"][:], pb[:, 0:384], [tb], [T_["Z"]])
        yield
        for g in range(2):
            pb, tb = B.single()
            v = pb[0:64, 0:384].rearrange("p (h n) -> p h n", n=128)
            for j in range(3):
                h = g * 3 + j
                mm(v[:, j, :], S_["At"][:, h * 64:(h + 1) * 64], TT[:, h, :], [T_["At"], t_TT], [tb])
            ev(fT["WmT"][:, g * 3:g * 3 + 3, :], v, [tb], [t_fT["WmT"]], r=True)
            yield
        for c in range(2):
            cs = slice(c * 64, (c + 1) * 64)
            pu, tu = B.single()
            for h in range(6):
                mm(pu[cs, h * 64:(h + 1) * 64], fT["WmT"][:, h, cs], M[:, h, :], [t_fT["WmT"], t_M], [tu])
                mm(psY1[cs, h * 64:(h + 1) * 64], fT["RtT"][:, h, cs], M[:, h, :], [t_fT["RtT"], t_M], [t_psY1])
            tt_op("dve", S_["U"][cs, :], pu[cs, 0:384], S_["Z"][cs, :], ALU.add, [tu, T_["Z"]], [T_["U"]], r=True)
            yield
            pm_, tm_ = B.single()
            mv = pm_[0:64, 0:384].rearrange("p (h n) -> p h n", n=64)
            for h in range(6):
                mm(mv[:, h, :], S_["Bh"][cs, h * 64:(h + 1) * 64], S_["U"][cs, h * 64:(h + 1) * 64], [T_["Bh"], T_["U"]], [tm_], start=True, stop=False)
                mm(mv[:, h, :], S_["Kh"][cs, h * 64:(h + 1) * 64], S_["Vr"][cs, h * 64:(h + 1) * 64], [T_["Kh"], T_["Vr"]], [tm_], start=False, stop=True)
            tt_op("dve", Mt[:], M[:], gamT[:, :, c:c + 1].broadcast_to([64, 6, 64]), ALU.mult, [t_M, t_gam], [t_Mt])
            tt_op("dve", M[:], mv, Mt[:], ALU.add, [tm_, t_Mt], [t_M], r=True)
            yield
        py, ty = B.single()
        for h in range(6):
            mm(py[:, h * 64:(h + 1) * 64], gr["RbT"][:, h, :], S_["U"][:, h * 64:(h + 1) * 64], [t_gr["RbT"], T_["U"]], [ty], start=True, stop=False)
            mm(py[:, h * 64:(h + 1) * 64], gr["RkT"][:, h, :], Vh(h), [t_gr["RkT"], T_["Vr"]], [ty], start=False, stop=True)
        K.op("act", lambda e: e.copy(out=S_["Y1"][:], in_=psY1[:, 0:384]), reads=[t_psY1], writes=[T_["Y1"]])
        tt_op("dve", S_["Y"][:], py[:, 0:384], S_["Y1"][:], ALU.add, [ty, T_["Y1"]], [T_["Y"]])
        yield
        if d == 0:
            K.dma(ydst[r0:r0 + 128, :], S_["Y"][:], reads=[T_["Y"]], writes=[t_yd])
        else:
            pb, tb = B.single()
            mm(pb[:, 0:384], Jm[:], S_["Y"][:], [t_c, T_["Y"]], [tb], fast=False)
            ev(S_["Y1"][:], pb[:, 0:384], [tb], [T_["Y1"]])
            K.dma(ydst[r0:r0 + 128, :], S_["Y1"][:], reads=[T_["Y1"]], writes=[t_yd])

    yield "rw_scan%d_%d" % (d, l)
    K.op("pool", lambda e: e.memset(M[:], 0.0), reads=[t_M], writes=[t_M])
    order = list(range(NT)) if d == 0 else [1, 0] + list(range(NT - 1, NCTX_T - 1, -1))
    for idx, tt in enumerate(order):
        yield from tile(tt, d, idx)
        yield


def gen_rw_readout(K, A, l, rwA, t_rwA, y0, t_y0, y1, t_y1, mixT, t_mix):
    identb, t_idb = make_ident(K, BF16)
    t_c = Trk()
    lng = K.sb([128, 384]); lnb = K.sb([128, 384])
    K.dma(lng[:], A["rw_ln_g"][l, :].partition_broadcast(128), writes=[t_c])
    K.dma(lnb[:], A["rw_ln_b"][l, :].partition_broadcast(128), writes=[t_c])
    NBUF = 2
    Y0 = [K.sb([128, 384]) for _ in range(NBUF)]; tY0 = [Trk() for _ in range(NBUF)]
    Y1_ = [K.sb([128, 384]) for _ in range(NBUF)]; tY1 = [Trk() for _ in range(NBUF)]
    GB = [K.sb([128, 2, 384]) for _ in range(NBUF)]; tGB = [Trk() for _ in range(NBUF)]
    W1 = [K.sb([128, 384]) for _ in range(NBUF)]; tW1 = [Trk() for _ in range(NBUF)]
    W2 = [K.sb([128, 384]) for _ in range(NBUF)]; tW2 = [Trk() for _ in range(NBUF)]
    ST = [K.sb([128, 6]) for _ in range(NBUF)]; tST = [Trk() for _ in range(NBUF)]
    OB = [K.sb([128, 384], BF16) for _ in range(NBUF)]; tOB = [Trk() for _ in range(NBUF)]
    OBT = [K.sb([128, 3, 128], BF16) for _ in range(NBUF)]; tOBT = [Trk() for _ in range(NBUF)]
    PS = [K.ps([128, 3, 128], BF16) for _ in range(2)]; tPS = [Trk(), Trk()]

    def tt_op(eng, out, in0, in1, op, reads, writes):
        K.op(eng, lambda e: e.tensor_tensor(out=out, in0=in0, in1=in1, op=op), reads=reads, writes=writes)

    def body(tt):
        n = tt % NBUF
        r0 = tt * 128
        y = Y0[n]; ty_ = tY0[n]; gb = GB[n]; t_gb = tGB[n]; w1 = W1[n]; w2 = W2[n]; st6 = ST[n]; t_st6 = tST[n]
        ob = OB[n]; t_ob = tOB[n]; obT = OBT[n]; t_obT = tOBT[n]
        T_ = {"w1": tW1[n], "w2": tW2[n]}
        K.dma(y[:], y0[r0:r0 + 128, :], reads=[t_y0], writes=[ty_])
        K.dma(Y1_[n][:], y1[r0:r0 + 128, :], reads=[t_y1], writes=[tY1[n]])
        K.dma(gb[:], rwA[r0:r0 + 128, 3:5, :], reads=[t_rwA], writes=[t_gb])
        tt_op("pool", y[:], y[:], Y1_[n][:], ALU.add, [ty_, tY1[n]], [ty_])
        y3 = y[:].rearrange("p (h n) -> p h n", h=6)
        w13 = w1[:].rearrange("p (h n) -> p h n", h=6)
        w23 = w2[:].rearrange("p (h n) -> p h n", h=6)
        K.op("dve", lambda e: e.tensor_reduce(out=st6[:], in_=y3, axis=AX.X, op=ALU.add), reads=[ty_], writes=[t_st6])
        K.op("dve", lambda e: e.tensor_scalar(out=st6[:], in0=st6[:], scalar1=-1.0 / 64, scalar2=None, op0=ALU.mult), reads=[t_st6], writes=[t_st6])
        tt_op("dve", w13, y3, st6[:].unsqueeze(2).broadcast_to([128, 6, 64]), ALU.add, [ty_, t_st6], [T_["w1"]])
        tt_op("pool", w2[:], w1[:], w1[:], ALU.mult, [T_["w1"]], [T_["w2"]])
        K.op("dve", lambda e: e.tensor_reduce(out=st6[:], in_=w23, axis=AX.X, op=ALU.add), reads=[T_["w2"]], writes=[t_st6])
        K.op("act", lambda e: e.activation(out=st6[:], in_=st6[:], func=AF.Sqrt, scale=1.0 / 64, bias=64e-5), reads=[t_st6], writes=[t_st6])
        K.op("dve", lambda e: e.reciprocal(out=st6[:], in_=st6[:]), reads=[t_st6], writes=[t_st6])
        tt_op("dve", w13, w13, st6[:].unsqueeze(2).broadcast_to([128, 6, 64]), ALU.mult, [T_["w1"], t_st6], [T_["w1"]])
        tt_op("pool", w1[:], w1[:], lng[:], ALU.mult, [T_["w1"], t_c], [T_["w1"]])
        tt_op("pool", w1[:], w1[:], lnb[:], ALU.add, [T_["w1"], t_c], [T_["w1"]])
        tt_op("dve", w1[:], w1[:], gb[:, 1, :], ALU.add, [T_["w1"], t_gb], [T_["w1"]])
        tt_op("dve", ob[:], w1[:], gb[:, 0, :], ALU.mult, [T_["w1"], t_gb], [t_ob])
        vb = PS[tt % 2]; tb = tPS[tt % 2]
        for c in range(3):
            K.op("pe", lambda e, c=c: e.transpose(out=vb[:, c, :], in_=ob[:, c * 128:(c + 1) * 128], identity=identb[:]), reads=[t_ob, t_idb], writes=[tb])
        K.op("act", lambda e: e.copy(out=obT[:], in_=vb[:]), reads=[tb], writes=[t_obT])
        K.dma(mixT[256:640, r0:r0 + 128].rearrange("(c p) n -> p c n", p=128), obT[:], reads=[t_obT], writes=[t_mix])

    yield "rw_readout_%d" % l
    for tt in range(NT):
        body(tt)
        yield


def phase_wout(K, A, l, xs, t_xs, modscr, t_mod, mixT, t_mix, need_ctx):
    K.push()
    wb = K.sb([128, 8, D], BF16); t_wb = Trk()
    stg = [K.sb([128, 8, 512]) for _ in range(2)]; t_stg = [Trk(), Trk()]
    for i in range(2):
        K.dma(stg[i][:], A["w_out"][l, :, i * 512:(i + 1) * 512].rearrange("(k p) n -> p k n", p=128), writes=[t_stg[i]])
        K.op("pool", lambda e, i=i: e.tensor_copy(out=wb[:, :, i * 512:(i + 1) * 512], in_=stg[i][:]), reads=[t_stg[i]], writes=[t_wb])
    m2 = [K.sb([128, D]) for _ in range(2)]; t_m2 = Trk()
    for which in range(2):
        K.dma(m2[which][:], modscr[which, 2 * D:3 * D].partition_broadcast(128), reads=[t_mod], writes=[t_m2])
    mt = [K.sb([128, 8, 128], BF16) for _ in range(2)]; t_mt = [Trk(), Trk()]
    xt = [K.sb([128, D]) for _ in range(2)]; t_xt = [Trk(), Trk()]
    tmp = K.sb([128, D]); t_tmp = Trk()
    ps = [K.ps([128, 512]) for _ in range(4)]; t_ps = [Trk() for _ in range(4)]

    def body(tt, n):
        m_ = mt[n % 2]; tm = t_mt[n % 2]; x_ = xt[n % 2]; tx = t_xt[n % 2]
        K.dma(m_[:], mixT[:, tt * 128:(tt + 1) * 128].rearrange("(c p) n -> p c n", p=128), reads=[t_mix], writes=[tm])
        K.dma(x_[:], xs[tt * 128:(tt + 1) * 128, :], reads=[t_xs], writes=[tx])
        mm_ = m2[1 if tt < NCTX_T else 0]
        for hf in range(2):
            p_ = ps[(2 * n + hf) % 4]; tp = t_ps[(2 * n + hf) % 4]
            for k in range(8):
                K.op("pe", lambda e, k=k, p_=p_, hf=hf: e.matmul(p_[:], lhsT=m_[:, k, :], rhs=wb[:, k, hf * 512:(hf + 1) * 512],
                                                                 start=(k == 0), stop=(k == 7)), reads=[tm, t_wb], writes=[tp])
            K.op("dve", lambda e, p_=p_, hf=hf: e.tensor_tensor(out=tmp[:, hf * 512:(hf + 1) * 512], in0=p_[:], in1=mm_[:, hf * 512:(hf + 1) * 512], op=ALU.mult),
                 reads=[tp, t_m2], writes=[t_tmp])
        K.op("pool", lambda e: e.tensor_tensor(out=x_[:], in0=x_[:], in1=tmp[:], op=ALU.add), reads=[tx, t_tmp], writes=[tx])
        K.dma(xs[tt * 128:(tt + 1) * 128, :], x_[:], reads=[tx], writes=[t_xs])

    for n, tt in enumerate(range(0 if need_ctx else NCTX_T, NT)):
        body(tt, n)
    K.pop()


def gen_conv(K, A, l, ub, vb, t_ub, t_vb, part=0, nparts=1, NC_=6):
    cf = [K.sb([128, D]) for _ in range(NC_)]; t_cf = [Trk() for _ in range(NC_)]
    cb = [K.sb([128, D], BF16) for _ in range(NC_)]; t_cb = [Trk() for _ in range(NC_)]
    engs = ["act", "dve", "pool"]
    jobs = []
    for i in range(128):
        jobs.append((A["pe_ut"][l, i].rearrange("p c e -> p (c e)"), ub[i].rearrange("p c e -> p (c e)"), t_ub))
        jobs.append((A["pe_v"][l, i * 128:(i + 1) * 128, :], vb[i], t_vb))
    per = len(jobs) // nparts
    jobs = jobs[part * per:(part + 1) * per]
    AH = min(4, NC_ - 1)

    def cv_load(n):
        K.dma(cf[n % NC_][:], jobs[n][0], writes=[t_cf[n % NC_]])

    def job(n):
        b_ = n % NC_
        eng = engs[n % 3]
        if eng == "act":
            K.op("act", lambda e: e.copy(out=cb[b_][:], in_=cf[b_][:]), reads=[t_cf[b_]], writes=[t_cb[b_]])
        else:
            K.op(eng, lambda e: e.tensor_copy(out=cb[b_][:], in_=cf[b_][:]), reads=[t_cf[b_]], writes=[t_cb[b_]])
        if n + AH < len(jobs):
            cv_load(n + AH)
        K.dma(jobs[n][1], cb[b_][:], reads=[t_cb[b_]], writes=[jobs[n][2]])

    yield "conv%d_%d" % (part, l)
    for n in range(AH):
        cv_load(n)
    for n in range(len(jobs)):
        job(n)
        if n % 2 == 1:
            yield


def phase_peer(K, A, l, xs, t_xs, modscr, t_mod, need_ctx, ubs, vbs, tconv):
    ub = ubs[l]; vb = vbs[l]; t_ub, t_vb = tconv
    K.push()
    identb, t_idb = make_ident(K, BF16)
    mods = load_mod(K, modscr, t_mod, A["norm_ffn_g"][l, :], 3, 4)
    m5 = [K.sb([128, D]) for _ in range(2)]; t_m5 = Trk()
    for which in range(2):
        K.dma(m5[which][:], modscr[which, 5 * D:6 * D].partition_broadcast(128), reads=[t_mod], writes=[t_m5])
    wq = K.sb([128, 8, 2048], BF16); t_wq = Trk()
    K.push()
    stg = [K.sb([128, 8, 512]) for _ in range(2)]; t_stg = [Trk(), Trk()]
    for i in range(4):
        K.dma(stg[i % 2][:], A["pe_wq"][l, :, i * 512:(i + 1) * 512].rearrange("(k p) n -> p k n", p=128), writes=[t_stg[i % 2]])
        K.op("pool", lambda e, i=i: e.tensor_copy(out=wq[:, :, i * 512:(i + 1) * 512], in_=stg[i % 2][:]), reads=[t_stg[i % 2]], writes=[t_wq])
    K.pop()
    keysT = K.sb([128, 16, 128]); t_keys = Trk()
    K.dma(keysT[:], A["keysT"][l].rearrange("c d k -> d c k"), writes=[t_keys])
    bk = [K.ps([128, 512]) for _ in range(8)]; t_bk = [Trk() for _ in range(8)]
    rot = [0]

    def rb():
        b = rot[0] % 4
        rot[0] += 1
        return bk[b], t_bk[b]

    xk = [K.sb([128, D]) for _ in range(2)]; t_xk = [Trk(), Trk()]
    scr = K.sb([128, D]); t_scr = Trk()
    ss = K.sb([128, 1]); t_ss = Trk()
    h = K.sb([128, D], BF16); t_h = Trk()
    hT = K.sb([128, 8, 256], BF16); t_hT = Trk()
    qT = K.sb([128, 16, 256]); t_qT = Trk()
    Ssb = [K.sb([128, 16, 128]) for _ in range(2)]; t_S = [Trk(), Trk()]
    s2pp = [K.sb([128, 8, 128]) for _ in range(2)]; t_s2 = [Trk(), Trk()]
    Dp = [K.sb([128, 8, 128], BF16) for _ in range(2)]; t_Dp = [Trk(), Trk()]
    top = K.sb([128, 8, 2, 16]); t_top = Trk()
    wk = K.sb([128, 256]); t_wk = Trk()
    cand = K.sb([128, 256]); t_cand = Trk()
    ctop = K.sb([128, 8, 16]); t_ctop = Trk()
    zs = K.sb([128, 8, 16]); t_zs = Trk()
    st8 = K.sb([128, 8]); t_st8 = Trk()
    NB = 4
    PSPL = 2
    ut = [K.sb([128, 8, 128], BF16) for _ in range(NB)]; t_ut = [Trk() for _ in range(NB)]
    vt = [K.sb([128, D], BF16) for _ in range(NB)]; t_vt = [Trk() for _ in range(NB)]
    ga = [K.sb([128, 256]) for _ in range(3)]; t_ga = [Trk() for _ in range(3)]
    EE = [[K.sb([128, 8, 128]) for _ in range(2)] for _ in range(2)]; t_EE = [[Trk(), Trk()], [Trk(), Trk()]]
    E2 = [K.sb([128, 8, 128]) for _ in range(2)]; t_E2 = [Trk(), Trk()]
    e1 = [K.sb([128, 8, 128]) for _ in range(2)]; t_e1 = [Trk(), Trk()]
    GG = [[K.sb([128, 8, 128], BF16) for _ in range(2)] for _ in range(2)]; t_GG = [[Trk(), Trk()], [Trk(), Trk()]]
    AW = [K.sb([128, 256], BF16) for _ in range(2)]; t_AW = [Trk(), Trk()]
    WS = [K.sb([128, 256]) for _ in range(2)]; t_WS = [Trk(), Trk()]
    tmpo = K.sb([128, D]); t_tmpo = Trk()
    NEGBIG = -1.0e30

    def block(t0, which):
        for j in range(2):
            tt = t0 + j
            K.dma(xk[j][:], xs[tt * 128:(tt + 1) * 128, :], reads=[t_xs], writes=[t_xk[j]])
            norm_mod_tile(K, xk[j][:], t_xk[j], mods[which], h, t_h, scr, t_scr, ss, t_ss)
            pb, tb = rb()
            pst = pb.bitcast(BF16).rearrange("p (c n) -> p c n", n=128)
            transpose_to(K, h, t_h, 8, identb, t_idb, pst, tb, hT[:, :, j * 128:(j + 1) * 128], t_hT)
        for cs in range(16):
            pb, tb = rb()
            for k in range(8):
                K.op("pe", lambda e, k=k, cs=cs, pb=pb: e.matmul(pb[:, 0:256], lhsT=wq[:, k, cs * 128:(cs + 1) * 128], rhs=hT[:, k, :],
                                                                 start=(k == 0), stop=(k == 7)), reads=[t_wq, t_hT], writes=[tb])
            if cs % 2:
                K.op("act", lambda e, cs=cs, pb=pb: e.copy(out=qT[:, cs, :], in_=pb[:, 0:256]), reads=[tb], writes=[t_qT])
            else:
                K.op("dve", lambda e, cs=cs, pb=pb: e.tensor_copy(out=qT[:, cs, :], in_=pb[:, 0:256]), reads=[tb], writes=[t_qT])
        for j in range(2):
            for q4 in range(4):
                pb, tb = rb()
                for u in range(4):
                    cs = q4 * 4 + u
                    K.op("pe", lambda e, cs=cs, u=u, pb=pb, j=j: e.matmul(pb[:, u * 128:(u + 1) * 128], lhsT=qT[:, cs, j * 128:(j + 1) * 128],
                                                                         rhs=keysT[:, cs, :], start=True, stop=True),
                         reads=[t_qT, t_keys], writes=[tb])
                K.op("act", lambda e, q4=q4, pb=pb, j=j: e.copy(out=Ssb[j][:, q4 * 4:(q4 + 1) * 4, :], in_=pb[:].rearrange("p (c n) -> p c n", n=128)),
                     reads=[tb], writes=[t_S[j]])
            for p in range(8):
                for side in range(2):
                    src = Ssb[j][:, 2 * p + side, :]
                    K.op("dve", lambda e, p=p, side=side, src=src: e.max(out=top[:, p, side, 0:8], in_=src), reads=[t_S[j]], writes=[t_top])
                    K.op("dve", lambda e, p=p, side=side, src=src: e.match_replace(out=wk[:, 0:128], in_to_replace=top[:, p, side, 0:8], in_values=src,
                                                                                  imm_value=NEGBIG), reads=[t_S[j], t_top], writes=[t_wk])
                    K.op("dve", lambda e, p=p, side=side: e.max(out=top[:, p, side, 8:16], in_=wk[:, 0:128]), reads=[t_wk], writes=[t_top])
                c3 = cand[:].rearrange("p (a b) -> p a b", a=16)
                K.op("pool", lambda e, p=p, c3=c3: e.tensor_tensor(out=c3, in0=top[:, p, 0, :].unsqueeze(2).broadcast_to([128, 16, 16]),
                                                                   in1=top[:, p, 1, :].unsqueeze(1).broadcast_to([128, 16, 16]), op=ALU.add),
                     reads=[t_top], writes=[t_cand])
                K.op("dve", lambda e, p=p: e.max(out=ctop[:, p, 0:8], in_=cand[:]), reads=[t_cand], writes=[t_ctop])
                K.op("dve", lambda e, p=p: e.match_replace(out=wk[:], in_to_replace=ctop[:, p, 0:8], in_values=cand[:], imm_value=NEGBIG),
                     reads=[t_cand, t_ctop], writes=[t_wk])
                K.op("dve", lambda e, p=p: e.max(out=ctop[:, p, 8:16], in_=wk[:]), reads=[t_wk], writes=[t_ctop])
            tau = ctop[:, :, 15:16]
            K.op("dve", lambda e: e.tensor_tensor(out=zs[:], in0=ctop[:], in1=tau.broadcast_to([128, 8, 16]), op=ALU.subtract),
                 reads=[t_ctop], writes=[t_zs])
            K.op("act", lambda e: e.activation(out=zs[:], in_=zs[:], func=AF.Exp), reads=[t_zs], writes=[t_zs])
            K.op("dve", lambda e: e.tensor_reduce(out=st8[:], in_=zs[:], axis=AX.X, op=ALU.add), reads=[t_zs], writes=[t_st8])
            K.op("dve", lambda e: e.reciprocal(out=st8[:], in_=st8[:]), reads=[t_st8], writes=[t_st8])
            S4 = Ssb[j][:].rearrange("p (h s) n -> p h s n", s=2)
            K.op("dve", lambda e, S4=S4, j=j: e.tensor_tensor(out=s2pp[j][:], in0=S4[:, :, 1, :], in1=tau.broadcast_to([128, 8, 128]), op=ALU.subtract),
                 reads=[t_S[j], t_ctop], writes=[t_s2[j]])
            m1 = top[:, :, 0, 0:1]
            K.op("dve", lambda e, j=j: e.tensor_tensor(out=s2pp[j][:], in0=s2pp[j][:], in1=m1.broadcast_to([128, 8, 128]), op=ALU.add),
                 reads=[t_s2[j], t_top], writes=[t_s2[j]])
            K.op("act", lambda e, j=j: e.activation(out=E2[j][:], in_=s2pp[j][:], func=AF.Exp, bias=1.0e-3), reads=[t_s2[j]], writes=[t_E2[j]])
            K.op("dve", lambda e, S4=S4, j=j: e.tensor_tensor(out=e1[j][:], in0=S4[:, :, 0, :], in1=m1.broadcast_to([128, 8, 128]), op=ALU.subtract),
                 reads=[t_S[j], t_top], writes=[t_e1[j]])
            K.op("act", lambda e, j=j: e.activation(out=e1[j][:], in_=e1[j][:], func=AF.Exp), reads=[t_e1[j]], writes=[t_e1[j]])
            K.op("dve", lambda e, j=j: e.tensor_tensor(out=Dp[j][:], in0=identb[:].unsqueeze(1).broadcast_to([128, 8, 128]),
                                                      in1=st8[:].unsqueeze(2).broadcast_to([128, 8, 128]), op=ALU.mult),
                 reads=[t_idb, t_st8], writes=[t_Dp[j]])
        S4 = [Ssb[j][:].rearrange("p (h s) n -> p h s n", s=2) for j in range(2)]

        def st_load(c):
            K.dma(ut[c % NB][:], ub[c], reads=[t_ub], writes=[t_ut[c % NB]])
            K.dma(vt[c % NB][:], vb[c], reads=[t_vb], writes=[t_vt[c % NB]])

        def st_gate(c):
            for j in range(2):
                E_ = EE[j][c % 2]; tE = t_EE[j][c % 2]; G_ = GG[j][c % 2]; tG = t_GG[j][c % 2]
                if j == 0:
                    K.op("pool", lambda e, E_=E_, j=j: e.tensor_tensor(out=E_[:], in0=E2[j][:], in1=e1[j][:, :, c:c + 1].broadcast_to([128, 8, 128]), op=ALU.mult),
                         reads=[t_E2[j], t_e1[j]], writes=[tE])
                else:
                    if PSPL > 0:
                        K.op("pool", lambda e, E_=E_, j=j: e.tensor_tensor(out=E_[:, 0:PSPL, :], in0=E2[j][:, 0:PSPL, :],
                                                                           in1=e1[j][:, 0:PSPL, c:c + 1].broadcast_to([128, PSPL, 128]), op=ALU.mult),
                             reads=[t_E2[j], t_e1[j]], writes=[tE])
                    for p in range(PSPL, 8):
                        K.op("act", lambda e, E_=E_, j=j, p=p: e.activation(out=E_[:, p, :], in_=E2[j][:, p, :], func=AF.Identity, scale=e1[j][:, p, c:c + 1]),
                             reads=[t_E2[j], t_e1[j]], writes=[tE])
                K.op("dve", lambda e, E_=E_, G_=G_: e.scalar_tensor_tensor(out=G_[:], in0=E_[:], scalar=1.0, in1=E_[:], op0=ALU.is_ge, op1=ALU.mult),
                     reads=[tE], writes=[tG])

        def st_A(c):
            pa, ta = bk[c % 2], t_bk[c % 2]
            u_ = ut[c % NB]
            for k in range(8):
                K.op("pe", lambda e, k=k: e.matmul(pa[:, 0:256], lhsT=u_[:, k, :], rhs=hT[:, k, :], start=(k == 0), stop=(k == 7)),
                     reads=[t_ut[c % NB], t_hT], writes=[ta])

        def st_gelu(c):
            pa, ta = bk[c % 2], t_bk[c % 2]
            g_ = ga[c % 3]
            K.op("act", lambda e: e.activation(out=g_[:], in_=pa[:, 0:256], func=AF.Gelu_apprx_tanh), reads=[ta], writes=[t_ga[c % 3]])

        def st_W(c):
            pw, tw = bk[2 + c % 2], t_bk[2 + c % 2]
            for j in range(2):
                G_ = GG[j][c % 2]; tG = t_GG[j][c % 2]
                for p in range(8):
                    K.op("pe", lambda e, p=p, j=j, G_=G_: e.matmul(pw[:, j * 128:(j + 1) * 128], lhsT=G_[:, p, :], rhs=Dp[j][:, p, :],
                                                                  start=(p == 0), stop=(p == 7)), reads=[tG, t_Dp[j]], writes=[tw])

        def st_AW(c):
            pw, tw = bk[2 + c % 2], t_bk[2 + c % 2]
            aw = AW[c % 2]; taw = t_AW[c % 2]; g_ = ga[c % 3]; ws = WS[c % 2]; tws = t_WS[c % 2]
            K.op("act", lambda e: e.copy(out=ws[:], in_=pw[:, 0:256]), reads=[tw], writes=[tws])
            K.op("pool", lambda e: e.tensor_tensor(out=aw[:], in0=g_[:], in1=ws[:], op=ALU.mult), reads=[t_ga[c % 3], tws], writes=[taw])

        def st_out(c):
            aw = AW[c % 2]; taw = t_AW[c % 2]; v_ = vt[c % NB]
            for j in range(2):
                for hf in range(2):
                    b = 4 + 2 * j + hf
                    K.op("pe", lambda e, j=j, hf=hf, b=b: e.matmul(bk[b][:], lhsT=aw[:, j * 128:(j + 1) * 128], rhs=v_[:, hf * 512:(hf + 1) * 512],
                                                                  start=(c == 0), stop=(c == 127)), reads=[taw, t_vt[c % NB]], writes=[t_bk[b]])

        for c in range(NB):
            st_load(c)
        for s_ in range(-2, 128):
            if s_ >= 0:
                st_AW(s_)
            if s_ + 2 < 128:
                st_A(s_ + 2)
            if s_ >= 0:
                st_out(s_)
            if 0 <= s_ + 1 < 128:
                st_W(s_ + 1)
            if s_ + 2 < 128:
                st_gate(s_ + 2)
                st_gelu(s_ + 2)
            if s_ >= 0 and s_ + NB < 128:
                st_load(s_ + NB)
        for j in range(2):
            tt = t0 + j
            for hf in range(2):
                b = 4 + 2 * j + hf
                K.op("dve", lambda e, b=b, hf=hf: e.tensor_tensor(out=tmpo[:, hf * 512:(hf + 1) * 512], in0=bk[b][:], in1=m5[which][:, hf * 512:(hf + 1) * 512], op=ALU.mult),
                     reads=[t_bk[b], t_m5], writes=[t_tmpo])
            K.op("dve", lambda e, j=j: e.tensor_tensor(out=xk[j][:], in0=xk[j][:], in1=tmpo[:], op=ALU.add), reads=[t_xk[j], t_tmpo], writes=[t_xk[j]])
            K.dma(xs[tt * 128:(tt + 1) * 128, :], xk[j][:], reads=[t_xk[j]], writes=[t_xs])

    for t0 in range(0 if need_ctx else NCTX_T, NT, 2):
        block(t0, 1 if t0 < NCTX_T else 0)
    K.pop()


IN_SPECS = {
    "xall": ([NTOK, D], F32), "cvec": ([128, 16], F32),
    "ada_w": ([2, D, 6144], F32), "ada_b": ([2, 6144], F32),
    "norm_mix_g": ([2, D], F32), "norm_ffn_g": ([2, D], F32),
    "w_in": ([2, D, IN_COLS], F32), "w_out": ([2, D, D], F32),
    "mla_q_norm_g": ([2, 256], F32), "mla_w_qb": ([2, 256, 384], F32), "mla_kv_norm_g": ([2, 128], F32),
    "mla_w_kvb": ([2, 128, 512], F32), "mla_qk_g": ([2, 2, 96], F32),
    "rw_mu": ([2, 2, 1568], F32), "rw_w0": ([2, 2, 384], F32), "rw_w2": ([2, 2, 64, 384], F32),
    "rw_a0": ([2, 2, 384], F32), "rw_a2": ([2, 2, 64, 384], F32), "rw_g2": ([2, 160, 384], F32),
    "rw_k_k": ([2, 384], F32), "rw_k_a": ([2, 384], F32), "rw_r_k": ([2, 384], F32),
    "rw_ln_g": ([2, 384], F32), "rw_ln_b": ([2, 384], F32), "gqa_qk_g": ([2, 2, 64], F32),
    "pe_wq": ([2, D, 2048], F32), "keysT": ([2, 16, 128, 128], F32),
    "pe_ut": ([2, 128, 128, 8, 128], F32), "pe_v": ([2, 16384, D], F32),
    "rope_m": ([2048, 32], F32), "rope_g": ([2048, 64], F32),
}


def build(layers=(0, 1), upto=None, dbg=False, scopes=False, same=True):
    nc = bass.Bass("TRN2", target_bir_lowering=False)
    A = {k: nc.dram_tensor(k, sh, dt, kind="ExternalInput").ap() for k, (sh, dt) in IN_SPECS.items()}
    skind = "ExternalOutput" if dbg else "Internal"
    out = nc.dram_tensor("out", [2048, D], F32, kind="ExternalOutput").ap()
    S = {}
    S["xs"] = nc.dram_tensor("xs", [NTOK, D], F32, kind=skind).ap()
    S["mod"] = nc.dram_tensor("modscr", [2, 6144], F32, kind=skind).ap()
    S["p"] = nc.dram_tensor("pscr", [NTOK, IN_COLS], F32, kind=skind).ap()
    S["rwA"] = nc.dram_tensor("rwA", [NTOK, 11, 384], F32, kind=skind).ap()
    S["yscr"] = nc.dram_tensor("yscr", [NTOK, 384], F32, kind=skind).ap()
    S["yscr1"] = nc.dram_tensor("yscr1", [NTOK, 384], F32, kind=skind).ap()
    S["mixT"] = nc.dram_tensor("mixT", [D, NTOK], BF16, kind=skind).ap()
    DS = {}
    for nm, sh in [("qtm", [96, 4, NTOK]), ("ktm", [96, 4, NTOK]), ("vdm", [NTOK, 4, 128]),
                   ("qtg", [64, 6, NTOK]), ("ktg", [64, 2, NTOK]), ("vdg", [NTOK, 2, 128])]:
        DS[nm] = nc.dram_tensor(nm, sh, BF16, kind="Internal").ap()
        DS["t_" + nm] = Trk()
    UB = [nc.dram_tensor("ub%d" % l, [128, 128, 8, 128], BF16, kind="Internal").ap() for l in range(2)]
    VB = [nc.dram_tensor("vb%d" % l, [128, 128, D], BF16, kind="Internal").ap() for l in range(2)]
    with ExitStack() as es:
        K = Kb(nc, es, same_eng_sync=same, scopes=scopes)
        t_out = Trk()
        T = {k: Trk() for k in S}
        K.push()
        stg = [K.sb([128, D]) for _ in range(2)]; t_stg = [Trk(), Trk()]
        for tt in range(NT):
            K.dma(stg[tt % 2][:], A["xall"][tt * 128:(tt + 1) * 128, :], writes=[t_stg[tt % 2]])
            K.dma(S["xs"][tt * 128:(tt + 1) * 128, :], stg[tt % 2][:], reads=[t_stg[tt % 2]], writes=[T["xs"]])
        K.pop()
        for l in layers:
            need_ctx = l < 1
            K.cur = "ada_%d" % l
            phase_ada(K, A, l, S["mod"], T["mod"])
            tconv = (Trk(), Trk())
            K.run_lanes([("proj_in", gen_proj_in(K, A, l, S["xs"], T["xs"], S["mod"], T["mod"], S["p"], T["p"]), 1.0),
                         ("conv", gen_conv(K, A, l, UB[l], VB[l], tconv[0], tconv[1], 0, 2), 3.6)])
            if upto == "projin":
                break

            def prep_lane():
                yield from gen_mla_prep(K, A, l, S["p"], T["p"], DS)
                yield from gen_gqa_prep(K, A, l, S["p"], T["p"], DS)

            K.run_lanes([("mla_prep", gen_mla_prep(K, A, l, S["p"], T["p"], DS), 1.0),
                         ("gqa_prep", gen_gqa_prep(K, A, l, S["p"], T["p"], DS), 0.7),
                         ("rw_prep", gen_rw_prep(K, A, l, S["p"], T["p"], S["rwA"], T["rwA"]), 1.0),
                         ("conv", gen_conv(K, A, l, UB[l], VB[l], tconv[0], tconv[1], 1, 2, NC_=2), 3.6)])
            if upto == "prep":
                break
            K.run_lanes([("attn_m", gen_attn(K, A, l, DS, S["mixT"], T["mixT"], need_ctx, "m"), 1.0),
                         ("attn_g", gen_attn(K, A, l, DS, S["mixT"], T["mixT"], need_ctx, "g"), 1.5)])
            K.push()
            RC = rw_consts(K, A, l)
            K.run_lanes([("rw_scan0", gen_rw_scan(K, A, l, RC, 0, S["rwA"], T["rwA"], S["yscr"], T["yscr"], n_rot=3), 1.0),
                         ("rw_scan1", gen_rw_scan(K, A, l, RC, 1, S["rwA"], T["rwA"], S["yscr1"], T["yscr1"], n_rot=3), 1.0)])
            K.pop()
            K.run_lanes([("rw_readout", gen_rw_readout(K, A, l, S["rwA"], T["rwA"], S["yscr"], T["yscr"], S["yscr1"], T["yscr1"], S["mixT"], T["mixT"]), 1.0)])
            if upto == "rwscan":
                break
            K.cur = "wout_%d" % l
            phase_wout(K, A, l, S["xs"], T["xs"], S["mod"], T["mod"], S["mixT"], T["mixT"], need_ctx)
            if upto == "wout":
                break
            K.cur = "peer_%d" % l
            phase_peer(K, A, l, S["xs"], T["xs"], S["mod"], T["mod"], need_ctx, UB, VB, tconv)
            if upto == "peer":
                break
        K.push()
        stg = [K.sb([128, D]) for _ in range(2)]; t_stg = [Trk(), Trk()]
        for tt in range(16):
            K.dma(stg[tt % 2][:], S["xs"][(tt + 2) * 128:(tt + 3) * 128, :], reads=[T["xs"]], writes=[t_stg[tt % 2]])
            K.dma(out[tt * 128:(tt + 1) * 128, :], stg[tt % 2][:], reads=[t_stg[tt % 2]], writes=[t_out])
        K.pop()
        K.emit([t_out] + list(T.values()))
    return nc


def rope_tables():
    n = np.arange(2048)
    row = (n // 64).astype(np.float32); col = (n % 64).astype(np.float32)
    tabs = []
    for rdim in (32, 64):
        q = rdim // 4
        inv = (10000.0 ** (-np.arange(q, dtype=np.float32) / q)).astype(np.float32)
        ang = np.concatenate([row[:, None] * inv, col[:, None] * inv], -1).astype(np.float32)
        tabs.append(np.concatenate([np.cos(ang), np.sin(ang)], -1).astype(np.float32))
    return tabs


def prep_inputs(inp, batches):
    f = lambda a: np.ascontiguousarray(np.asarray(a, dtype=np.float32))
    rm, rg = rope_tables()
    shared = {k: f(inp[k]) for k in ["ada_w", "ada_b", "norm_mix_g", "norm_ffn_g", "w_in", "w_out", "mla_q_norm_g", "mla_w_qb",
                                     "mla_kv_norm_g", "mla_w_kvb", "mla_qk_g", "rw_mu", "rw_w0", "rw_w2", "rw_a0", "rw_a2", "rw_g2",
                                     "rw_k_k", "rw_k_a", "rw_ln_g", "rw_ln_b", "gqa_qk_g", "pe_wq", "pe_v"]}
    shared["rw_r_k"] = f(inp["rw_r_k"]).reshape(2, 384)
    shared["keysT"] = f(np.transpose(f(inp["pe_keys"]).reshape(2, 16, 128, 128), (0, 1, 3, 2)))
    u = f(inp["pe_u"]).reshape(2, 128, 128, 8, 128)
    shared["pe_ut"] = f(np.transpose(u, (0, 1, 4, 3, 2)))
    shared["rope_m"] = rm; shared["rope_g"] = rg
    maps = []
    for b in batches:
        m = dict(shared)
        m["xall"] = f(np.concatenate([inp["ctx"][b], inp["x"][b]], 0))
        cv = np.stack([f(inp["c"][b]).reshape(8, 128), f(inp["c_ctx"]).reshape(8, 128)], -1)
        m["cvec"] = f(np.transpose(cv, (1, 0, 2)).reshape(128, 16))
        maps.append(m)
    return maps


def kernel(**inputs):
    from concourse.bass_utils import run_bass_kernel_spmd
    nc = build()
    maps = prep_inputs(inputs, list(range(8)))
    res = run_bass_kernel_spmd(nc, maps, core_ids=list(range(8)))
    return np.stack([np.asarray(r["out"], dtype=np.float32) for r in res.results], 0)
```

```python
from contextlib import ExitStack
import numpy as np
import concourse.bass as bass
import concourse.mybir as mybir

F32 = mybir.dt.float32
BF16 = mybir.dt.bfloat16
U32 = mybir.dt.uint32
AF = mybir.ActivationFunctionType
ALU = mybir.AluOpType
AX = mybir.AxisListType

COMPUTE = ("pe", "dve", "act", "pool")
NDS = 24


class Trk:
    __slots__ = ("w", "r")

    def __init__(self):
        self.w = None
        self.r = {}


class Kb:
    def __init__(self, nc, es, same_eng_sync=True, scopes=False):
        self.scopes = scopes
        self.nc = nc
        self.es = es
        self.same = ("dve", "act", "pool") if same_eng_sync is True else tuple(same_eng_sync or ())
        self.eng = {"pe": nc.tensor, "dve": nc.vector, "act": nc.scalar, "pool": nc.gpsimd, "sp": nc.sync}
        self.q = {e: [] for e in self.eng}
        self.cnt = {e: 0 for e in COMPUTE}
        self.sem = {e: es.enter_context(nc.semaphore("sem_" + e)) for e in COMPUTE}
        self.dsem = [es.enter_context(nc.semaphore("dsem%d" % i)) for i in range(NDS)]
        self.dcount = 0
        self.waited = {}
        self.ntens = 0
        self.pend = {e: [] for e in self.eng}
        self.stacks = []
        self.cur = "main"

    def push(self):
        st = ExitStack()
        st.__enter__()
        self.stacks.append(self.es)
        self.es = st

    def pop(self):
        self.barrier()
        self.es.__exit__(None, None, None)
        self.es = self.stacks.pop()

    def run_lanes(self, lanes):
        self.push()
        active = [[nm, g, w, 0.0] for nm, g, w in lanes]
        while active:
            active.sort(key=lambda a: a[3])
            a = active[0]
            self.cur = a[0]
            try:
                nm = next(a[1])
                if isinstance(nm, str):
                    a[0] = nm
                a[3] += 1.0 / a[2]
            except StopIteration:
                active.remove(a)
        self.pop()

    def barrier(self):
        for eng in self.eng:
            deps = [("e", E, self.cnt[E]) for E in COMPUTE if self.cnt[E] > 0 and E != eng]
            for k in range(NDS):
                if self.dcount > k:
                    deps.append(("d", k, 16 * ((self.dcount - 1 - k) // NDS + 1)))
            self.pend[eng].extend(self._filter(eng, deps))

    def sb(self, shape, dtype=F32, name=None):
        self.ntens += 1
        t = self.es.enter_context(self.nc.sbuf_tensor(name or ("sb%d" % self.ntens), list(shape), dtype))
        return t

    def ps(self, shape, dtype=F32, name=None):
        self.ntens += 1
        t = self.es.enter_context(self.nc.psum_tensor(name or ("ps%d" % self.ntens), list(shape), dtype))
        return t

    def _deps(self, reads, writes):
        deps = []
        for t in reads:
            if t.w is not None:
                deps.append(t.w)
        for t in writes:
            if t.w is not None:
                deps.append(t.w)
            deps.extend(t.r.values())
        return deps

    def _filter(self, eng, deps):
        waits = []
        for d in deps:
            if d[0] == "e":
                _, E, n = d
                if E == eng and E not in self.same:
                    continue
                key = (eng, E)
                if self.waited.get(key, 0) >= n:
                    continue
                self.waited[key] = n
                waits.append((self.sem[E], n))
            else:
                _, k, v = d
                key = (eng, "d%d" % k)
                if self.waited.get(key, 0) >= v:
                    continue
                self.waited[key] = v
                waits.append((self.dsem[k], v))
        return waits

    def _mark(self, tok, reads, writes):
        for t in reads:
            t.r[tok[1] if tok[0] == "e" else ("d%d" % tok[1])] = tok
        for t in writes:
            t.w = tok
            t.r = {}

    def op(self, eng, fn, reads=(), writes=()):
        deps = self._deps(reads, writes)
        waits = self.pend[eng] + self._filter(eng, deps)
        self.pend[eng] = []
        n = self.cnt[eng] + 1
        self.cnt[eng] = n
        self.q[eng].append((waits, fn, self.sem[eng], 1, self.cur))
        self._mark(("e", eng, n), reads, writes)

    def dma(self, out, in_, reads=(), writes=(), queue="sp", **kw):
        deps = self._deps(reads, writes)
        k = self.dcount % NDS
        v = 16 * (self.dcount // NDS + 1)
        self.dcount += 1
        if v > 16:
            deps.append(("d", k, v - 16))
        waits = self.pend[queue] + self._filter(queue, deps)
        self.pend[queue] = []
        self.q[queue].append((waits, lambda e: e.dma_start(out=out, in_=in_, **kw), self.dsem[k], 16, self.cur))
        self._mark(("d", k, v), reads, writes)

    def emit(self, final_tracks=()):
        deps = []
        for t in final_tracks:
            if t.w is not None:
                deps.append(t.w)
        fw = self._filter("sp", deps)
        nc = self.nc
        q = self.q
        with nc.Block() as block:
            def run(e, recs, extra=()):
                i = 0
                while i < len(recs):
                    sc = recs[i][4]
                    j = i
                    while j < len(recs) and recs[j][4] == sc:
                        j += 1
                    if self.scopes:
                        with nc.named_scope(sc):
                            for waits, fn, sem, inc, _ in recs[i:j]:
                                for s, v in waits:
                                    e.wait_ge(s, v)
                                fn(e).then_inc(sem, inc)
                    else:
                        for waits, fn, sem, inc, _ in recs[i:j]:
                            for s, v in waits:
                                e.wait_ge(s, v)
                            fn(e).then_inc(sem, inc)
                    i = j
                for s, v in extra:
                    e.wait_ge(s, v)

            pend = self.pend

            @block.sync
            def _(e):
                run(e, q["sp"], pend["sp"] + fw)

            @block.tensor
            def _(e):
                run(e, q["pe"], pend["pe"])

            @block.vector
            def _(e):
                run(e, q["dve"], pend["dve"])

            @block.scalar
            def _(e):
                run(e, q["act"], pend["act"])

            @block.gpsimd
            def _(e):
                run(e, q["pool"], pend["pool"])


D = 1024
NTOK = 2304
NT = 18
NCTX_T = 2
IN_COLS = 2624
C_MLA, C_RW, C_GQA = 0, 416, 1984
EPS = 1e-6


def make_ident(K, dtype):
    idf = K.sb([128, 128], F32)
    t = Trk()
    K.op("pool", lambda e: e.memset(idf[:], 0.0), writes=[t])
    K.op("pool", lambda e: e.affine_select(out=idf[:], in_=idf[:], pattern=[[-1, 128]], compare_op=ALU.not_equal,
                                           fill=1.0, base=0, channel_multiplier=1), reads=[t], writes=[t])
    if dtype == F32:
        return idf, t
    idb = K.sb([128, 128], dtype)
    t2 = Trk()
    K.op("dve", lambda e: e.tensor_copy(out=idb[:], in_=idf[:]), reads=[t], writes=[t2])
    return idb, t2


def bcast_load(K, dst, src_row, tr_src, tr_dst, parts=128):
    K.dma(dst, src_row.partition_broadcast(parts), reads=tr_src, writes=[tr_dst])


def phase_ada(K, A, l, modscr, t_mod):
    K.push()
    cv = K.sb([128, 16]); t_cv = Trk()
    K.dma(cv[:], A["cvec"], writes=[t_cv])
    sc = K.sb([128, 16]); t_sc = Trk()
    K.op("act", lambda e: e.activation(out=sc[:], in_=cv[:], func=AF.Silu), reads=[t_cv], writes=[t_sc])
    bias = K.sb([2, 6144]); t_b = Trk()
    K.dma(bias[:], A["ada_b"][l, :].partition_broadcast(2), writes=[t_b])
    res = K.sb([2, 6144]); t_res = Trk()
    wt = [K.sb([128, 8, 512]) for _ in range(2)]; t_w = [Trk(), Trk()]
    ps = [K.ps([2, 512]) for _ in range(2)]; t_ps = [Trk(), Trk()]
    for cb in range(12):
        w = wt[cb % 2]; tw = t_w[cb % 2]; p = ps[cb % 2]; tp = t_ps[cb % 2]
        K.dma(w[:], A["ada_w"][l, :, cb * 512:(cb + 1) * 512].rearrange("(k p) n -> p k n", p=128), writes=[tw])
        for k in range(8):
            K.op("pe", lambda e, k=k, w=w, p=p: e.matmul(p[:], lhsT=sc[:, 2 * k:2 * k + 2], rhs=w[:, k, :],
                                                       start=(k == 0), stop=(k == 7)), reads=[t_sc, tw], writes=[tp])
        K.op("dve", lambda e, p=p, cb=cb: e.tensor_tensor(out=res[:, cb * 512:(cb + 1) * 512], in0=p[:],
                                                          in1=bias[:, cb * 512:(cb + 1) * 512], op=ALU.add),
             reads=[tp, t_b], writes=[t_res])
    K.dma(modscr, res[:], reads=[t_res], writes=[t_mod])
    K.pop()


def load_mod(K, modscr, t_mod, g_row, i_shift, i_scale):
    out = []
    g = K.sb([128, D]); t_g = Trk()
    K.dma(g[:], g_row.partition_broadcast(128), writes=[t_g])
    for which in range(2):
        gm = K.sb([128, D]); sh = K.sb([128, D]); t1 = Trk(); t2 = Trk()
        K.dma(gm[:], modscr[which, i_scale * D:(i_scale + 1) * D].partition_broadcast(128), reads=[t_mod], writes=[t1])
        K.dma(sh[:], modscr[which, i_shift * D:(i_shift + 1) * D].partition_broadcast(128), reads=[t_mod], writes=[t2])
        K.op("pool", lambda e, gm=gm: e.scalar_tensor_tensor(out=gm[:], in0=gm[:], scalar=1.0, in1=g[:], op0=ALU.add, op1=ALU.mult),
             reads=[t1, t_g], writes=[t1]) if False else \
            K.op("dve", lambda e, gm=gm: e.scalar_tensor_tensor(out=gm[:], in0=gm[:], scalar=1.0, in1=g[:], op0=ALU.add, op1=ALU.mult),
                 reads=[t1, t_g], writes=[t1])
        out.append((gm, t1, sh, t2))
    return out


def norm_mod_tile(K, xt, t_x, mods, h, t_h, scr, t_scr, ss, t_ss):
    gm, t_gm, sh, t_sh = mods
    K.op("act", lambda e: e.activation(out=scr[:], in_=xt, func=AF.Square, accum_out=ss[:]), reads=[t_x], writes=[t_scr, t_ss])
    K.op("act", lambda e: e.activation(out=ss[:], in_=ss[:], func=AF.Sqrt, scale=1.0 / D, bias=EPS), reads=[t_ss], writes=[t_ss])
    K.op("dve", lambda e: e.reciprocal(out=ss[:], in_=ss[:]), reads=[t_ss], writes=[t_ss])
    K.op("dve", lambda e: e.scalar_tensor_tensor(out=scr[:], in0=xt, scalar=ss[:, 0:1], in1=gm[:], op0=ALU.mult, op1=ALU.mult),
         reads=[t_x, t_ss, t_gm], writes=[t_scr])
    K.op("dve", lambda e: e.tensor_tensor(out=h[:], in0=scr[:], in1=sh[:], op=ALU.add), reads=[t_scr, t_sh], writes=[t_h])


def transpose_to(K, src, t_src, nchunks, ident, t_id, pst, t_pst, dst, t_dst, eng="act"):
    for c in range(nchunks):
        K.op("pe", lambda e, c=c: e.transpose(out=pst[:, c, :], in_=src[:, c * 128:(c + 1) * 128], identity=ident[:]),
             reads=[t_src, t_id], writes=[t_pst])
    if eng == "act":
        K.op("act", lambda e: e.copy(out=dst, in_=pst[:, 0:nchunks, :]), reads=[t_pst], writes=[t_dst])
    else:
        K.op("dve", lambda e: e.tensor_copy(out=dst, in_=pst[:, 0:nchunks, :]), reads=[t_pst], writes=[t_dst])


def gen_proj_in(K, A, l, xs, t_xs, modscr, t_mod, pscr, t_p):
    ident, t_id = make_ident(K, BF16)
    mods = load_mod(K, modscr, t_mod, A["norm_mix_g"][l, :], 0, 1)
    wb = K.sb([128, 8, IN_COLS], BF16); t_wb = Trk()
    stg = [K.sb([128, 8, 512]) for _ in range(2)]; t_stg = [Trk(), Trk()]
    blocks = [(c0, min(512, IN_COLS - c0)) for c0 in range(0, IN_COLS, 512)]
    for i, (c0, n) in enumerate(blocks):
        s = stg[i % 2]; ts = t_stg[i % 2]
        K.dma(s[:, :, 0:n], A["w_in"][l, :, c0:c0 + n].rearrange("(k p) n -> p k n", p=128), writes=[ts])
        K.op("pool", lambda e, s=s, c0=c0, n=n: e.tensor_copy(out=wb[:, :, c0:c0 + n], in_=s[:, :, 0:n]), reads=[ts], writes=[t_wb])
    xt = [K.sb([128, D]) for _ in range(2)]; t_xt = [Trk(), Trk()]
    scr = K.sb([128, D]); t_scr = Trk()
    ss = K.sb([128, 1]); t_ss = Trk()
    h = K.sb([128, D], BF16); t_h = Trk()
    hT = [K.sb([128, 8, 128], BF16) for _ in range(2)]; t_hT = [Trk(), Trk()]
    pst = K.ps([128, 8, 128], BF16); t_pst = Trk()
    pp = [K.ps([128, 512]) for _ in range(3)]; t_pp = [Trk() for _ in range(3)]
    po = [K.sb([128, IN_COLS]) for _ in range(2)]; t_po = [Trk(), Trk()]
    nmm = [0]

    def body(tt):
        x_ = xt[tt % 2]; tx = t_xt[tt % 2]
        K.dma(x_[:], xs[tt * 128:(tt + 1) * 128, :], reads=[t_xs], writes=[tx])
        norm_mod_tile(K, x_[:], tx, mods[1 if tt < NCTX_T else 0], h, t_h, scr, t_scr, ss, t_ss)
        hT_ = hT[tt % 2]; thT = t_hT[tt % 2]
        transpose_to(K, h, t_h, 8, ident, t_id, pst, t_pst, hT_[:], thT)
        o = po[tt % 2]; to = t_po[tt % 2]
        for i, (c0, n) in enumerate(blocks):
            p_ = pp[nmm[0] % 3]; tp = t_pp[nmm[0] % 3]; nmm[0] += 1
            for k in range(8):
                K.op("pe", lambda e, k=k, p_=p_, c0=c0, n=n: e.matmul(p_[:, 0:n], lhsT=hT_[:, k, :], rhs=wb[:, k, c0:c0 + n], start=(k == 0), stop=(k == 7)),
                     reads=[thT, t_wb], writes=[tp])
            if i % 2 == 0:
                K.op("act", lambda e, p_=p_, c0=c0, n=n: e.copy(out=o[:, c0:c0 + n], in_=p_[:, 0:n]), reads=[tp], writes=[to])
            else:
                K.op("dve", lambda e, p_=p_, c0=c0, n=n: e.tensor_copy(out=o[:, c0:c0 + n], in_=p_[:, 0:n]), reads=[tp], writes=[to])
        K.dma(pscr[tt * 128:(tt + 1) * 128, :], o[:], reads=[to], writes=[t_p])

    yield "proj_in_%d" % l
    for tt in range(NT):
        body(tt)
        yield


def norm_rope_heads(K, src, t_src, H, dh, g, t_g, rope, t_rope, rdim, roff, out_bf, t_out, W):
    sq3 = W["sq"][:, 0:H * dh].rearrange("p (h d) -> p h d", h=H)
    qn = W["qn"][:, 0:H * dh].rearrange("p (h d) -> p h d", h=H)
    ssq = W["ssq"][:, 0:H]
    K.op("dve", lambda e: e.tensor_tensor(out=sq3, in0=src, in1=src, op=ALU.mult), reads=[t_src], writes=[W["t_sq"]])
    K.op("dve", lambda e: e.tensor_reduce(out=ssq, in_=sq3, axis=AX.X, op=ALU.add), reads=[W["t_sq"]], writes=[W["t_ssq"]])
    K.op("act", lambda e: e.activation(out=ssq, in_=ssq, func=AF.Sqrt, scale=1.0 / dh, bias=EPS), reads=[W["t_ssq"]], writes=[W["t_ssq"]])
    K.op("dve", lambda e: e.reciprocal(out=ssq, in_=ssq), reads=[W["t_ssq"]], writes=[W["t_ssq"]])
    K.op("dve", lambda e: e.tensor_tensor(out=qn, in0=src, in1=ssq.unsqueeze(2).broadcast_to([128, H, dh]), op=ALU.mult),
         reads=[t_src, W["t_ssq"]], writes=[W["t_qn"]])
    gb = g.unsqueeze(1).broadcast_to([128, H, dh])
    if rope is None:
        K.op("dve", lambda e: e.tensor_tensor(out=out_bf, in0=qn, in1=gb, op=ALU.mult), reads=[W["t_qn"], t_g], writes=[t_out])
        return
    K.op("dve", lambda e: e.tensor_tensor(out=qn, in0=qn, in1=gb, op=ALU.mult), reads=[W["t_qn"], t_g], writes=[W["t_qn"]])
    hf = rdim // 2
    xa = qn[:, :, roff:roff + hf]; xb = qn[:, :, roff + hf:roff + rdim]
    cos = rope[:, 0:hf].unsqueeze(1).broadcast_to([128, H, hf]); sin = rope[:, hf:rdim].unsqueeze(1).broadcast_to([128, H, hf])
    tm = [W["rt%d" % i][:, 0:H * hf].rearrange("p (h d) -> p h d", h=H) for i in range(4)]
    for i, (a_, b_) in enumerate([(xa, cos), (xb, sin), (xa, sin), (xb, cos)]):
        K.op("pool", lambda e, i=i, a_=a_, b_=b_: e.tensor_tensor(out=tm[i], in0=a_, in1=b_, op=ALU.mult),
             reads=[W["t_qn"], t_rope], writes=[W["t_rt%d" % i]])
    if roff > 0:
        K.op("act", lambda e: e.copy(out=out_bf[:, :, 0:roff], in_=qn[:, :, 0:roff]), reads=[W["t_qn"]], writes=[t_out])
    K.op("dve", lambda e: e.tensor_tensor(out=out_bf[:, :, roff:roff + hf], in0=tm[0], in1=tm[1], op=ALU.subtract),
         reads=[W["t_rt0"], W["t_rt1"]], writes=[t_out])
    K.op("dve", lambda e: e.tensor_tensor(out=out_bf[:, :, roff + hf:roff + rdim], in0=tm[2], in1=tm[3], op=ALU.add),
         reads=[W["t_rt2"], W["t_rt3"]], writes=[t_out])


def make_work(K, n):
    W = {}
    for nm, sh in [("sq", [128, n]), ("qn", [128, n]), ("ssq", [128, 8]), ("rt0", [128, n]), ("rt1", [128, n]), ("rt2", [128, n]), ("rt3", [128, n])]:
        W[nm] = K.sb(sh); W["t_" + nm] = Trk()
    return W


def attn_core(K, QT, t_QT, KT, t_KT, Vd, t_Vd, dq, heads, mixT, t_mix, need_ctx, R):
    ones, t_ones = R["ones"]
    ps_s, t_ps_s = R["ps_s"]
    PT, t_PT = R["PT"]; rden, t_rden = R["rden"]; osb, t_osb = R["osb"]
    blocks = [(256 + i * 512, 512, list(range(NT))) for i in range(4)]
    if need_ctx:
        blocks.append((0, 256, [0, 1]))
    st = R["cnt"]

    def unit(qh, kh, row0, q0, nq, kts):
        half = (row0 % 128) // 64
        po, tpo = R["ps_o"][st[1] % len(R["ps_o"])]; pd, tpd = R["ps_d"][st[1] % len(R["ps_d"])]
        for j, kt in enumerate(kts):
            ns = st[0]; st[0] += 1
            pS = ps_s[ns % 2]; tS = t_ps_s[ns % 2]; pt_ = PT[ns % 3]; tpt = t_PT[ns % 3]
            K.op("pe", lambda e, pS=pS, kt=kt: e.matmul(pS[:, 0:nq], lhsT=KT[0:dq, kh, kt * 128:(kt + 1) * 128], rhs=QT[0:dq, qh, q0:q0 + nq], start=True, stop=True),
                 reads=[t_KT, t_QT], writes=[tS])
            K.op("act", lambda e, pS=pS, pt_=pt_: e.activation(out=pt_[:, 0:nq], in_=pS[:, 0:nq], func=AF.Exp), reads=[tS], writes=[tpt])
            K.op("pe", lambda e, kt=kt, pt_=pt_, j=j: e.matmul(po[:, 0:nq], lhsT=Vd[:, kt, kh, :], rhs=pt_[:, 0:nq], start=(j == 0), stop=(j == len(kts) - 1)),
                 reads=[t_Vd, tpt], writes=[tpo])
            K.op("pe", lambda e, pt_=pt_, j=j: e.matmul(pd[:, 0:nq], lhsT=ones[:], rhs=pt_[:, 0:nq], start=(j == 0), stop=(j == len(kts) - 1)),
                 reads=[t_ones, tpt], writes=[tpd])
            if j % 2 == 1:
                yield
        nb = st[1]; st[1] += 1
        rd = rden[nb % 2]; trd = t_rden[nb % 2]; ob = osb[nb % 2]; tob = t_osb[nb % 2]
        K.op("dve", lambda e: e.reciprocal(out=rd[:, 0:nq], in_=pd[:, 0:nq]), reads=[tpd], writes=[trd])
        K.op("dve", lambda e: e.tensor_tensor(out=ob[:, 0:nq], in0=po[:, 0:nq], in1=rd[:, 0:nq], op=ALU.mult), reads=[tpo, trd], writes=[tob])
        K.dma(mixT[row0:row0 + 64, q0:q0 + nq], ob[half * 64:half * 64 + 64, 0:nq], reads=[tob], writes=[t_mix])

    for (qh, kh, row0) in heads:
        for (q0, nq, kts) in blocks:
            yield from unit(qh, kh, row0, q0, nq, kts)
            yield


def gen_attn(K, A, l, DS, mixT, t_mix, need_ctx, which):
    dq, nqh, nkh = (96, 4, 4) if which == "m" else (64, 6, 2)
    QT = K.sb([dq, nqh, NTOK], BF16); t_QT = Trk()
    KT = K.sb([dq, nkh, NTOK], BF16); t_KT = Trk()
    Vd = K.sb([128, NT, nkh, 128], BF16); t_Vd = Trk()
    ones = K.sb([128, 128], BF16); t_ones = Trk()
    K.op("pool", lambda e: e.memset(ones[:], 1.0), writes=[t_ones])
    R = {"ones": (ones, t_ones),
         "ps_s": ([K.ps([128, 512]) for _ in range(2)], [Trk(), Trk()]),
         "ps_o": [(K.ps([128, 512]), Trk())], "ps_d": [(K.ps([128, 512]), Trk())],
         "PT": ([K.sb([128, 512], BF16) for _ in range(3)], [Trk() for _ in range(3)]),
         "rden": ([K.sb([128, 512]) for _ in range(2)], [Trk(), Trk()]),
         "osb": ([K.sb([128, 512], BF16) for _ in range(2)], [Trk(), Trk()]), "cnt": [0, 0]}
    sfx = "m" if which == "m" else "g"
    yield ("attn_mla_%d" if which == "m" else "attn_gqa_%d") % l
    K.dma(QT[:], DS["qt" + sfx], reads=[DS["t_qt" + sfx]], writes=[t_QT])
    K.dma(KT[:], DS["kt" + sfx], reads=[DS["t_kt" + sfx]], writes=[t_KT])
    K.dma(Vd[:], DS["vd" + sfx].rearrange("(t p) h d -> p t h d", p=128), reads=[DS["t_vd" + sfx]], writes=[t_Vd])
    yield
    if which == "m":
        heads = [(h, h, h * 64) for h in range(4)]
    else:
        heads = [(g, g // 3, 640 + g * 64) for g in range(6)]
    yield from attn_core(K, QT, t_QT, KT, t_KT, Vd, t_Vd, dq, heads, mixT, t_mix, need_ctx, R)


def gen_mla_prep(K, A, l, pscr, t_p, DS):
    qkT = [K.sb([96, 2, 4, 128], BF16) for _ in range(2)]; t_qkT = [Trk(), Trk()]
    Vt = [K.sb([128, 4, 128], BF16) for _ in range(2)]; t_Vt = [Trk(), Trk()]
    ident, t_id = make_ident(K, BF16)
    W = make_work(K, 384)
    gq = K.sb([128, 256]); t_gq = Trk(); bcast_load(K, gq[:], A["mla_q_norm_g"][l, :], [], t_gq)
    gkv = K.sb([128, 128]); t_gkv = Trk(); bcast_load(K, gkv[:], A["mla_kv_norm_g"][l, :], [], t_gkv)
    gqk = K.sb([128, 2, 96]); t_gqk = Trk()
    K.dma(gqk[:], A["mla_qk_g"][l].partition_broadcast(128), writes=[t_gqk])
    K.op("dve", lambda e: e.tensor_scalar(out=gqk[:, 0, :], in0=gqk[:, 0, :], scalar1=96.0 ** -0.5, scalar2=None, op0=ALU.mult),
         reads=[t_gqk], writes=[t_gqk])
    wq_f = K.sb([128, 2, 384]); t_wqf = Trk()
    K.dma(wq_f[:], A["mla_w_qb"][l].rearrange("(k p) n -> p k n", p=128), writes=[t_wqf])
    wq = K.sb([128, 2, 384], BF16); t_wq = Trk()
    K.op("dve", lambda e: e.tensor_copy(out=wq[:], in_=wq_f[:]), reads=[t_wqf], writes=[t_wq])
    wkv_f = K.sb([128, 512]); t_wkvf = Trk()
    K.dma(wkv_f[:], A["mla_w_kvb"][l], writes=[t_wkvf])
    wkv = K.sb([128, 512], BF16); t_wkv = Trk()
    K.op("dve", lambda e: e.tensor_copy(out=wkv[:], in_=wkv_f[:]), reads=[t_wkvf], writes=[t_wkv])
    pm = [K.sb([128, 416]) for _ in range(2)]; t_pm = [Trk(), Trk()]
    rp = [K.sb([128, 32]) for _ in range(2)]; t_rp = [Trk(), Trk()]
    junk = K.sb([128, 256]); t_junk = Trk()
    ss = K.sb([128, 2]); t_ss = Trk()
    cn = K.sb([128, 384], BF16); t_cn = Trk()
    cT = K.sb([128, 3, 128], BF16); t_cT = Trk()
    pst = K.ps([128, 3, 128], BF16); t_pst = Trk()
    pq = K.ps([128, 384]); t_pq = Trk()
    pkv = K.ps([128, 512]); t_pkv = Trk()
    qs = K.sb([128, 4, 96]); t_qs = Trk()
    ks = K.sb([128, 4, 96]); t_ks = Trk()
    qb = K.sb([128, 4, 96], BF16); t_qb = Trk()
    kb = K.sb([128, 4, 96], BF16); t_kb = Trk()
    ptqk = K.ps([96, 2, 4, 128], BF16); t_ptq = Trk(); t_ptk = t_ptq
    ptq = ptqk[:, 0]; ptk = ptqk[:, 1]
    def body(tt):
        p_ = pm[tt % 2]; tp = t_pm[tt % 2]
        K.dma(p_[:], pscr[tt * 128:(tt + 1) * 128, C_MLA:C_MLA + 416], reads=[t_p], writes=[tp])
        rope = None; trp = None
        if tt >= NCTX_T:
            rope = rp[tt % 2]; trp = t_rp[tt % 2]
            K.dma(rope[:], A["rope_m"][(tt - 2) * 128:(tt - 1) * 128, :], writes=[trp])
        for j, (c0, n, g_, tg) in enumerate([(0, 256, gq, t_gq), (256, 128, gkv, t_gkv)]):
            K.op("act", lambda e, c0=c0, n=n, j=j: e.activation(out=junk[:, 0:n], in_=p_[:, c0:c0 + n], func=AF.Square, accum_out=ss[:, j:j + 1]),
                 reads=[tp], writes=[t_junk, t_ss])
            K.op("act", lambda e, n=n, j=j: e.activation(out=ss[:, j:j + 1], in_=ss[:, j:j + 1], func=AF.Sqrt, scale=1.0 / n, bias=EPS),
                 reads=[t_ss], writes=[t_ss])
            K.op("dve", lambda e, j=j: e.reciprocal(out=ss[:, j:j + 1], in_=ss[:, j:j + 1]), reads=[t_ss], writes=[t_ss])
            K.op("dve", lambda e, c0=c0, n=n, j=j, g_=g_: e.scalar_tensor_tensor(out=cn[:, c0:c0 + n], in0=p_[:, c0:c0 + n], scalar=ss[:, j:j + 1],
                                                                                in1=g_[:], op0=ALU.mult, op1=ALU.mult),
                 reads=[tp, t_ss, tg], writes=[t_cn])
        transpose_to(K, cn, t_cn, 3, ident, t_id, pst, t_pst, cT[:], t_cT)
        yield
        for k in range(2):
            K.op("pe", lambda e, k=k: e.matmul(pq[:], lhsT=cT[:, k, :], rhs=wq[:, k, :], start=(k == 0), stop=(k == 1)),
                 reads=[t_cT, t_wq], writes=[t_pq])
        K.op("pe", lambda e: e.matmul(pkv[:], lhsT=cT[:, 2, :], rhs=wkv[:], start=True, stop=True), reads=[t_cT, t_wkv], writes=[t_pkv])
        yield
        K.op("act", lambda e: e.copy(out=qs[:], in_=pq[:].rearrange("p (h d) -> p h d", h=4)), reads=[t_pq], writes=[t_qs])
        kv3 = pkv[:].rearrange("p (h d) -> p h d", h=4)
        K.op("act", lambda e: e.copy(out=ks[:, :, 0:64], in_=kv3[:, :, 0:64]), reads=[t_pkv], writes=[t_ks])
        K.op("pool", lambda e: e.tensor_copy(out=ks[:, :, 64:96], in_=p_[:, 384:416].unsqueeze(1).broadcast_to([128, 4, 32])),
             reads=[tp], writes=[t_ks])
        Vt_ = Vt[tt % 2]; tVt = t_Vt[tt % 2]
        K.op("act", lambda e: e.copy(out=Vt_[:, :, 0:64], in_=kv3[:, :, 64:128]), reads=[t_pkv], writes=[tVt])
        K.op("dve", lambda e: e.tensor_copy(out=Vt_[:, :, 64:128], in_=kv3[:, :, 64:128]), reads=[t_pkv], writes=[tVt])
        K.dma(DS["vdm"][tt * 128:(tt + 1) * 128], Vt_[:], reads=[tVt], writes=[DS["t_vdm"]])
        yield
        norm_rope_heads(K, qs[:], t_qs, 4, 96, gqk[:, 0, :], t_gqk, rope, trp, 32, 64, qb[:], t_qb, W)
        yield
        norm_rope_heads(K, ks[:], t_ks, 4, 96, gqk[:, 1, :], t_gqk, rope, trp, 32, 64, kb[:], t_kb, W)
        yield
        qk_ = qkT[tt % 2]; tqk = t_qkT[tt % 2]
        for i_, (src, ts, pt_) in enumerate([(qb, t_qb, ptq), (kb, t_kb, ptk)]):
            for h in range(4):
                K.op("pe", lambda e, h=h, src=src, pt_=pt_: e.transpose(out=pt_[:, h, :], in_=src[:, h, :], identity=ident[:]),
                     reads=[ts, t_id], writes=[t_ptq])
        K.op("act", lambda e: e.copy(out=qk_[:], in_=ptqk[:]), reads=[t_ptq], writes=[tqk])
        K.dma(DS["qtm"][:, :, tt * 128:(tt + 1) * 128], qk_[:, 0], reads=[tqk], writes=[DS["t_qtm"]])
        K.dma(DS["ktm"][:, :, tt * 128:(tt + 1) * 128], qk_[:, 1], reads=[tqk], writes=[DS["t_ktm"]])
    yield "mla_prep_%d" % l
    for tt in range(NT):
        yield from body(tt)
        yield


def gen_gqa_prep(K, A, l, pscr, t_p, DS):
    qkT = [K.sb([64, 8, 128], BF16) for _ in range(2)]; t_qkT = [Trk(), Trk()]
    Vt = [K.sb([128, 2, 128], BF16) for _ in range(2)]; t_Vt = [Trk(), Trk()]
    ident, t_id = make_ident(K, BF16)
    W = make_work(K, 384)
    gqk = K.sb([128, 2, 64]); t_gqk = Trk()
    K.dma(gqk[:], A["gqa_qk_g"][l].partition_broadcast(128), writes=[t_gqk])
    K.op("dve", lambda e: e.tensor_scalar(out=gqk[:, 0, :], in0=gqk[:, 0, :], scalar1=64.0 ** -0.5, scalar2=None, op0=ALU.mult),
         reads=[t_gqk], writes=[t_gqk])
    pg = [K.sb([128, 640]) for _ in range(2)]; t_pg = [Trk(), Trk()]
    rp = [K.sb([128, 64]) for _ in range(2)]; t_rp = [Trk(), Trk()]
    qb = K.sb([128, 6, 64], BF16); t_qb = Trk()
    kb = K.sb([128, 2, 64], BF16); t_kb = Trk()
    ptqk = K.ps([64, 8, 128], BF16); t_ptq = Trk(); t_ptk = t_ptq
    ptq = ptqk[:, 0:6]; ptk = ptqk[:, 6:8]
    def body(tt):
        p_ = pg[tt % 2]; tp = t_pg[tt % 2]
        K.dma(p_[:], pscr[tt * 128:(tt + 1) * 128, C_GQA:C_GQA + 640], reads=[t_p], writes=[tp])
        rope = None; trp = None
        if tt >= NCTX_T:
            rope = rp[tt % 2]; trp = t_rp[tt % 2]
            K.dma(rope[:], A["rope_g"][(tt - 2) * 128:(tt - 1) * 128, :], writes=[trp])
        q3 = p_[:, 0:384].rearrange("p (h d) -> p h d", h=6)
        k3 = p_[:, 384:512].rearrange("p (h d) -> p h d", h=2)
        v3 = p_[:, 512:640].rearrange("p (h d) -> p h d", h=2)
        Vt_ = Vt[tt % 2]; tVt = t_Vt[tt % 2]
        K.op("act", lambda e: e.copy(out=Vt_[:, :, 0:64], in_=v3), reads=[tp], writes=[tVt])
        K.op("pool", lambda e: e.tensor_copy(out=Vt_[:, :, 64:128], in_=v3), reads=[tp], writes=[tVt])
        K.dma(DS["vdg"][tt * 128:(tt + 1) * 128], Vt_[:], reads=[tVt], writes=[DS["t_vdg"]])
        yield
        norm_rope_heads(K, q3, tp, 6, 64, gqk[:, 0, :], t_gqk, rope, trp, 64, 0, qb[:], t_qb, W)
        yield
        norm_rope_heads(K, k3, tp, 2, 64, gqk[:, 1, :], t_gqk, rope, trp, 64, 0, kb[:], t_kb, W)
        yield
        qk_ = qkT[tt % 2]; tqk = t_qkT[tt % 2]
        for (src, ts, pt_, H) in [(qb, t_qb, ptq, 6), (kb, t_kb, ptk, 2)]:
            for h in range(H):
                K.op("pe", lambda e, h=h, src=src, pt_=pt_: e.transpose(out=pt_[:, h, :], in_=src[:, h, :], identity=ident[:]),
                     reads=[ts, t_id], writes=[t_ptq])
        K.op("act", lambda e: e.copy(out=qk_[:], in_=ptqk[:]), reads=[t_ptq], writes=[tqk])
        K.dma(DS["qtg"][:, :, tt * 128:(tt + 1) * 128], qk_[:, 0:6], reads=[tqk], writes=[DS["t_qtg"]])
        K.dma(DS["ktg"][:, :, tt * 128:(tt + 1) * 128], qk_[:, 6:8], reads=[tqk], writes=[DS["t_ktg"]])
    yield "gqa_prep_%d" % l
    for tt in range(NT):
        yield from body(tt)
        yield


def gen_rw_prep(K, A, l, pscr, t_p, rwA, t_rwA):
    identf, t_idf = make_ident(K, F32)
    RW = 1568
    mu0 = K.sb([128, RW]); mu1 = K.sb([128, RW]); cm = K.sb([128, RW]); t_mu = Trk()
    K.dma(mu0[:], A["rw_mu"][l, 0, :].partition_broadcast(128), writes=[t_mu])
    K.dma(mu1[:], A["rw_mu"][l, 1, :].partition_broadcast(128), writes=[t_mu])
    K.op("dve", lambda e: e.tensor_tensor(out=cm[:], in0=mu0[:], in1=mu1[:], op=ALU.add), reads=[t_mu], writes=[t_mu])
    K.op("dve", lambda e: e.tensor_scalar(out=cm[:], in0=cm[:], scalar1=-1.0, scalar2=1.0, op0=ALU.mult, op1=ALU.add), reads=[t_mu], writes=[t_mu])
    t_c = Trk()
    w0 = K.sb([128, 2, 384]); K.dma(w0[:], A["rw_w0"][l].partition_broadcast(128), writes=[t_c])
    a0 = K.sb([128, 2, 384]); K.dma(a0[:], A["rw_a0"][l].partition_broadcast(128), writes=[t_c])
    kk_ = K.sb([128, 384]); K.dma(kk_[:], A["rw_k_k"][l, :].partition_broadcast(128), writes=[t_c])
    ka_ = K.sb([128, 384]); K.dma(ka_[:], A["rw_k_a"][l, :].partition_broadcast(128), writes=[t_c])
    rk_ = K.sb([128, 384]); K.dma(rk_[:], A["rw_r_k"][l, :].partition_broadcast(128), writes=[t_c])
    W2 = K.sb([128, 384]); K.dma(W2[:], A["rw_w2"][l].rearrange("d r c -> (d r) c"), writes=[t_c])
    A2 = K.sb([128, 384]); K.dma(A2[:], A["rw_a2"][l].rearrange("d r c -> (d r) c"), writes=[t_c])
    G2 = K.sb([128, 384]); K.dma(G2[:], A["rw_g2"][l, 0:128, :], writes=[t_c])
    G2b = K.sb([32, 384]); K.dma(G2b[:], A["rw_g2"][l, 128:160, :], writes=[t_c])
    pc = [K.sb([128, RW]) for _ in range(2)]; t_pc = [Trk(), Trk()]
    pp = [K.sb([128, RW]) for _ in range(2)]; t_pp = [Trk(), Trk()]
    pn = [K.sb([128, RW]) for _ in range(2)]; t_pn = [Trk(), Trk()]
    xs = K.sb([128, RW]); t_xs = Trk()
    u1 = K.sb([128, RW]); t_u1 = Trk()
    u2 = K.sb([128, RW]); t_u2 = Trk()
    tw = K.sb([128, 416]); t_tw = Trk()
    lT = K.sb([128, 4, 128]); t_lT = Trk()
    pst = K.ps([128, 4, 128]); t_pst = Trk()
    plb = [K.ps([128, 512]) for _ in range(2)]; t_plb = [Trk(), Trk()]
    lwt = [K.sb([128, 384]) for _ in range(2)]; t_lwt = [Trk(), Trk()]
    npl = [0]
    stg = [K.sb([128, 11, 384]) for _ in range(2)]; t_stg = [Trk(), Trk()]
    ad = K.sb([128, 2, 384]); t_ad = Trk()
    tmp = K.sb([128, 384]); t_tmp = Trk()
    tmp2 = K.sb([128, 384]); t_tmp2 = Trk()
    ssq = K.sb([128, 6]); t_ssq = Trk()
    NEG = -float(np.exp(-0.5))

    def body(tt):
        c_ = pc[tt % 2]; tc = t_pc[tt % 2]; p_ = pp[tt % 2]; tp = t_pp[tt % 2]; n_ = pn[tt % 2]; tn = t_pn[tt % 2]
        r0 = tt * 128
        K.dma(c_[:], pscr[r0:r0 + 128, C_RW:C_RW + RW], reads=[t_p], writes=[tc])
        if tt in (0, NCTX_T):
            K.op("pool", lambda e: e.memset(p_[:], 0.0), writes=[tp])
            K.dma(p_[1:128, :], pscr[r0:r0 + 127, C_RW:C_RW + RW], reads=[t_p], writes=[tp])
        else:
            K.dma(p_[:], pscr[r0 - 1:r0 + 127, C_RW:C_RW + RW], reads=[t_p], writes=[tp])
        if tt in (NCTX_T - 1, NT - 1):
            K.op("pool", lambda e: e.memset(n_[:], 0.0), writes=[tn])
            K.dma(n_[0:127, :], pscr[r0 + 1:r0 + 128, C_RW:C_RW + RW], reads=[t_p], writes=[tn])
        else:
            K.dma(n_[:], pscr[r0 + 1:r0 + 129, C_RW:C_RW + RW], reads=[t_p], writes=[tn])
        K.op("dve", lambda e: e.tensor_tensor(out=xs[:], in0=c_[:], in1=cm[:], op=ALU.mult), reads=[tc, t_mu], writes=[t_xs])
        K.op("pool", lambda e: e.tensor_tensor(out=u1[:], in0=p_[:], in1=mu0[:], op=ALU.mult), reads=[tp, t_mu], writes=[t_u1])
        K.op("pool", lambda e: e.tensor_tensor(out=u2[:], in0=n_[:], in1=mu1[:], op=ALU.mult), reads=[tn, t_mu], writes=[t_u2])
        K.op("dve", lambda e: e.tensor_tensor(out=xs[:], in0=xs[:], in1=u1[:], op=ALU.add), reads=[t_xs, t_u1], writes=[t_xs])
        K.op("dve", lambda e: e.tensor_tensor(out=xs[:], in0=xs[:], in1=u2[:], op=ALU.add), reads=[t_xs, t_u2], writes=[t_xs])
        yield
        r = xs[:, 0:384]; k = xs[:, 384:768]; v = xs[:, 768:1152]
        K.op("act", lambda e: e.activation(out=tw[:, 0:128], in_=xs[:, 1152:1280], func=AF.Tanh), reads=[t_xs], writes=[t_tw])
        K.op("act", lambda e: e.copy(out=tw[:, 128:256], in_=xs[:, 1280:1408]), reads=[t_xs], writes=[t_tw])
        K.op("act", lambda e: e.activation(out=tw[:, 256:416], in_=xs[:, 1408:1568], func=AF.Sigmoid), reads=[t_xs], writes=[t_tw])
        for c in range(3):
            K.op("pe", lambda e, c=c: e.transpose(out=pst[:, c, :], in_=tw[:, c * 128:(c + 1) * 128], identity=identf[:]),
                 reads=[t_tw, t_idf], writes=[t_pst])
        K.op("pe", lambda e: e.transpose(out=pst[0:32, 3, :], in_=tw[:, 384:416], identity=identf[:]), reads=[t_tw, t_idf], writes=[t_pst])
        K.op("act", lambda e: e.copy(out=lT[:, 0:3, :], in_=pst[:, 0:3, :]), reads=[t_pst], writes=[t_lT])
        K.op("act", lambda e: e.copy(out=lT[0:32, 3, :], in_=pst[0:32, 3, :]), reads=[t_pst], writes=[t_lT])
        yield
        s_ = stg[tt % 2]; ts = t_stg[tt % 2]

        def nextpl():
            i_ = npl[0] % 2
            npl[0] += 1
            return plb[i_][:, 0:384], t_plb[i_]

        for d in range(2):
            p_w, t_w = nextpl()
            K.op("pe", lambda e, d=d, p_w=p_w: e.matmul(p_w, lhsT=lT[d * 64:(d + 1) * 64, 0, :], rhs=W2[d * 64:(d + 1) * 64, :], start=True, stop=True),
                 reads=[t_lT, t_c], writes=[t_w])
            K.op("dve", lambda e, d=d, p_w=p_w: e.tensor_tensor(out=lwt[d][:], in0=p_w, in1=w0[:, d, :], op=ALU.add), reads=[t_w, t_c], writes=[t_lwt[d]])
            K.op("act", lambda e, d=d: e.activation(out=lwt[d][:], in_=lwt[d][:], func=AF.Sigmoid), reads=[t_lwt[d]], writes=[t_lwt[d]])
            K.op("act", lambda e, d=d: e.mul(out=s_[:, 5 + 3 * d, :], in_=lwt[d][:], mul=NEG), reads=[t_lwt[d]], writes=[ts])
            p_a, t_a_ = nextpl()
            K.op("pe", lambda e, d=d, p_a=p_a: e.matmul(p_a, lhsT=lT[d * 64:(d + 1) * 64, 1, :], rhs=A2[d * 64:(d + 1) * 64, :], start=True, stop=True),
                 reads=[t_lT, t_c], writes=[t_a_])
            K.op("dve", lambda e, d=d, p_a=p_a: e.tensor_tensor(out=ad[:, d, :], in0=p_a, in1=a0[:, d, :], op=ALU.add), reads=[t_a_, t_c], writes=[t_ad])
            K.op("act", lambda e, d=d: e.activation(out=ad[:, d, :], in_=ad[:, d, :], func=AF.Sigmoid), reads=[t_ad], writes=[t_ad])
        p_g, t_g_ = nextpl()
        K.op("pe", lambda e: e.matmul(p_g, lhsT=lT[:, 2, :], rhs=G2[:], start=True, stop=False), reads=[t_lT, t_c], writes=[t_g_])
        K.op("pe", lambda e: e.matmul(p_g, lhsT=lT[0:32, 3, :], rhs=G2b[:], start=False, stop=True), reads=[t_lT, t_c], writes=[t_g_])
        K.op("act", lambda e: e.copy(out=s_[:, 3, :], in_=p_g), reads=[t_g_], writes=[ts])
        K.op("act", lambda e: e.copy(out=s_[:, 0, :], in_=r), reads=[t_xs], writes=[ts])
        K.op("act", lambda e: e.copy(out=s_[:, 1, :], in_=v), reads=[t_xs], writes=[ts])
        yield
        K.op("dve", lambda e: e.tensor_tensor(out=tmp[:], in0=k, in1=kk_[:], op=ALU.mult), reads=[t_xs, t_c], writes=[t_tmp])
        K.op("dve", lambda e: e.tensor_tensor(out=tmp2[:], in0=tmp[:], in1=tmp[:], op=ALU.mult), reads=[t_tmp], writes=[t_tmp2])
        K.op("dve", lambda e: e.tensor_reduce(out=ssq[:], in_=tmp2[:].rearrange("p (h n) -> p h n", h=6), axis=AX.X, op=ALU.add),
             reads=[t_tmp2], writes=[t_ssq])
        K.op("act", lambda e: e.activation(out=ssq[:], in_=ssq[:], func=AF.Sqrt, scale=1.0, bias=1e-12), reads=[t_ssq], writes=[t_ssq])
        K.op("dve", lambda e: e.reciprocal(out=ssq[:], in_=ssq[:]), reads=[t_ssq], writes=[t_ssq])
        K.op("dve", lambda e: e.tensor_tensor(out=s_[:, 2, :].rearrange("p (h n) -> p h n", h=6), in0=tmp[:].rearrange("p (h n) -> p h n", h=6),
                                              in1=ssq[:].unsqueeze(2).broadcast_to([128, 6, 64]), op=ALU.mult),
             reads=[t_tmp, t_ssq], writes=[ts])
        yield
        for d in range(2):
            K.op("dve", lambda e, d=d: e.scalar_tensor_tensor(out=tmp2[:], in0=ad[:, d, :], scalar=1.0, in1=ka_[:], op0=ALU.subtract, op1=ALU.mult),
                 reads=[t_ad, t_c], writes=[t_tmp2])
            K.op("dve", lambda e, d=d: e.scalar_tensor_tensor(out=s_[:, 6 + 3 * d, :], in0=tmp2[:], scalar=1.0, in1=k, op0=ALU.add, op1=ALU.mult),
                 reads=[t_tmp2, t_xs], writes=[ts])
            K.op("dve", lambda e, d=d: e.tensor_tensor(out=s_[:, 7 + 3 * d, :], in0=s_[:, 2, :], in1=ad[:, d, :], op=ALU.mult), reads=[ts, t_ad], writes=[ts])
            yield
        K.op("dve", lambda e: e.tensor_tensor(out=tmp[:], in0=s_[:, 6, :], in1=s_[:, 9, :], op=ALU.add), reads=[ts], writes=[t_tmp])
        K.op("dve", lambda e: e.tensor_tensor(out=tmp[:], in0=tmp[:], in1=rk_[:], op=ALU.mult), reads=[t_tmp, t_c], writes=[t_tmp])
        K.op("dve", lambda e: e.tensor_tensor(out=tmp[:], in0=tmp[:], in1=r, op=ALU.mult), reads=[t_tmp, t_xs], writes=[t_tmp])
        K.op("dve", lambda e: e.tensor_reduce(out=ssq[:], in_=tmp[:].rearrange("p (h n) -> p h n", h=6), axis=AX.X, op=ALU.add),
             reads=[t_tmp], writes=[t_ssq])
        K.op("dve", lambda e: e.tensor_tensor(out=s_[:, 4, :].rearrange("p (h n) -> p h n", h=6), in0=v.rearrange("p (h n) -> p h n", h=6),
                                              in1=ssq[:].unsqueeze(2).broadcast_to([128, 6, 64]), op=ALU.mult),
             reads=[t_xs, t_ssq], writes=[ts])
        K.dma(rwA[r0:r0 + 128, :, :], s_[:], reads=[ts], writes=[t_rwA])

    yield "rw_prep_%d" % l
    for tt in range(NT):
        yield from body(tt)
        yield


RW_FAST = [True]


class Banks:
    def __init__(self, K, n_rot=7):
        nb = (n_rot + 2) // 2
        self.t = [K.ps([128, 1024]) for _ in range(nb)]
        self.trk = [Trk() for _ in range(2 * nb)]
        self.i1 = 0
        self.i2 = 0
        self.n_rot = n_rot

    def bank(self, b):
        return self.t[b // 2][:, (b % 2) * 512:(b % 2) * 512 + 512], self.trk[b]

    def single(self):
        b = self.i1 % self.n_rot
        self.i1 += 1
        return self.bank(b)

    def double(self):
        p = self.i2 % 3
        self.i2 += 1
        return self.t[p], [self.trk[2 * p], self.trk[2 * p + 1]]


F32R_ = mybir.dt.float32r


def rw_consts(K, A, l):
    identf, t_idf = make_ident(K, F32)
    t_c = Trk()
    ones3 = K.sb([128, 3, 128])
    K.op("pool", lambda e: e.memset(ones3[:], 1.0), writes=[t_c])
    m_inc3 = K.sb([128, 3, 128]); m_str3 = K.sb([128, 3, 128]); id3 = K.sb([128, 3, 128]); Jm = K.sb([128, 128])
    blk = K.sb([128, 128]); cind = K.sb([128, 2])
    pat3 = [[0, 3], [1, 128]]
    K.op("pool", lambda e: e.affine_select(out=m_inc3[:], in_=ones3[:], pattern=pat3, compare_op=ALU.is_ge, fill=0.0, base=0, channel_multiplier=-1), reads=[t_c], writes=[t_c])
    K.op("pool", lambda e: e.affine_select(out=m_str3[:], in_=ones3[:], pattern=pat3, compare_op=ALU.is_ge, fill=0.0, base=-1, channel_multiplier=-1), reads=[t_c], writes=[t_c])
    K.op("pool", lambda e: e.affine_select(out=id3[:], in_=ones3[:], pattern=pat3, compare_op=ALU.is_equal, fill=0.0, base=0, channel_multiplier=-1), reads=[t_c], writes=[t_c])
    K.op("pool", lambda e: e.affine_select(out=Jm[:], in_=ones3[:, 0, :], pattern=[[1, 128]], compare_op=ALU.is_equal, fill=0.0, base=-127, channel_multiplier=1), reads=[t_c], writes=[t_c])
    K.op("pool", lambda e: e.memset(m_inc3[0:64, :, 64:128], 0.0), reads=[t_c], writes=[t_c])
    K.op("pool", lambda e: e.memset(m_str3[0:64, :, 64:128], 0.0), reads=[t_c], writes=[t_c])
    K.op("pool", lambda e: e.memset(blk[:], 0.0), writes=[t_c])
    K.op("pool", lambda e: e.memset(blk[0:64, 0:64], 1.0), reads=[t_c], writes=[t_c])
    K.op("pool", lambda e: e.memset(blk[64:128, 64:128], 1.0), reads=[t_c], writes=[t_c])
    K.op("pool", lambda e: e.memset(cind[:], 0.0), writes=[t_c])
    K.op("pool", lambda e: e.memset(cind[0:64, 0:1], 1.0), reads=[t_c], writes=[t_c])
    K.op("pool", lambda e: e.memset(cind[64:128, 1:2], 1.0), reads=[t_c], writes=[t_c])
    tri = m_inc3[:, 0, :]

    return dict(identf=identf, t_idf=t_idf, t_c=t_c, m_inc3=m_inc3, m_str3=m_str3, id3=id3, Jm=Jm, blk=blk, cind=cind, tri=tri)


def gen_rw_scan(K, A, l, C, d, rwA, t_rwA, ydst, t_yd, n_rot=3):
    identf = C["identf"]; t_idf = C["t_idf"]; t_c = C["t_c"]; m_inc3 = C["m_inc3"]; m_str3 = C["m_str3"]; id3 = C["id3"]
    Jm = C["Jm"]; blk = C["blk"]; cind = C["cind"]; tri = C["tri"]
    B = Banks(K, n_rot=n_rot)
    psY1, t_psY1 = B.bank(n_rot)
    a_in = [K.sb([128, 6, 384])]; t_a = [Trk()]
    if d == 1:
        af = K.sb([128, 6, 384]); t_af = Trk()
    names = ["cum", "d0", "d1", "e0", "em", "ep", "eh", "At", "Bt", "Kt", "Rt", "Bh", "Kh", "X1", "Z", "U", "Y1", "Y", "Vr"]
    S_ = {n: K.sb([128, 384]) for n in names}; T_ = {n: Trk() for n in names}
    fT = {n: K.sb([64, 6, 128]) for n in ["AtT", "BtT", "KtT", "RtT", "WmT"]}; t_fT = {n: Trk() for n in fT}
    gr = {n: K.sb([128, 6, 128]) for n in ["N", "LakT", "RbT", "RkT"]}; t_gr = {n: Trk() for n in gr}
    Nk = [K.sb([128, 6, 128]) for _ in range(2)]; t_Nk = [Trk(), Trk()]
    NkT = [K.sb([128, 6, 128]) for _ in range(2)]; t_NkT = [Trk(), Trk()]
    TT = K.sb([128, 6, 128]); t_TT = Trk()
    gamT = K.sb([64, 6, 2]); t_gam = Trk()
    M = K.sb([64, 6, 64]); t_M = Trk()
    Mt = K.sb([64, 6, 64]); t_Mt = Trk()
    cnt = [0]

    def RD(ap, r):
        return ap.bitcast(F32R_) if (r and RW_FAST[0]) else ap

    def ev(out, in_, reads, writes, r=False):
        out = RD(out, r)
        cnt[0] += 1
        if cnt[0] % 2:
            K.op("act", lambda e: e.copy(out=out, in_=in_), reads=reads, writes=writes)
        else:
            K.op("dve", lambda e: e.tensor_copy(out=out, in_=in_), reads=reads, writes=writes)

    def mm(out, lhsT, rhs, reads, writes, start=True, stop=True, fast=None):
        if fast is None:
            fast = RW_FAST[0]
        if fast and out.start_partition() == 0 and lhsT.start_partition() == 0:
            lhsT = lhsT.bitcast(mybir.dt.float32r); rhs = rhs.bitcast(mybir.dt.float32r)
        K.op("pe", lambda e: e.matmul(out, lhsT=lhsT, rhs=rhs, start=start, stop=stop), reads=reads, writes=writes)

    def tt_op(eng, out, in0, in1, op, reads, writes, r=False):
        out = RD(out, r)
        K.op(eng, lambda e: e.tensor_tensor(out=out, in0=in0, in1=in1, op=op), reads=reads, writes=writes)

    def h3(ap):
        return ap.rearrange("p (h n) -> p h n", n=128)

    def tile(tt, d, idx):
        r0 = tt * 128
        a_ = a_in[0]; ta = t_a[0]
        K.dma(a_[:, 0:3, :], rwA[r0:r0 + 128, 0:3, :], reads=[t_rwA], writes=[ta])
        K.dma(a_[:, 3:6, :], rwA[r0:r0 + 128, 5 + 3 * d:8 + 3 * d, :], reads=[t_rwA], writes=[ta])
        if d == 1:
            for i in range(6):
                pb, tb = B.single()
                mm(pb[:, 0:384], Jm[:], a_[:, i, :], [t_c, ta], [tb], fast=False)
                ev(af[:, i, :], pb[:, 0:384], [tb], [t_af])
            X = af; tX = t_af
        else:
            X = a_; tX = ta
        R_, V_, KK_, LW_, KD_, BD_ = [X[:, i, :] for i in range(6)]
        K.op("act", lambda e: e.copy(out=RD(S_["Vr"][:], True), in_=V_), reads=[tX], writes=[T_["Vr"]])
        Vh = lambda h: S_["Vr"][:, h * 64:(h + 1) * 64]
        pcum, tcum = B.single(); ptot, ttot = B.single(); pgT, tgT = B.single()
        mm(pcum[:, 0:384], tri, LW_, [t_c, tX], [tcum], fast=False)
        mm(ptot[:, 0:384], blk[:], LW_, [t_c, tX], [ttot], fast=False)
        gview = pgT[0:64, 0:12].rearrange("p (h c) -> p h c", c=2)
        for h in range(6):
            mm(gview[:, h, :], X[:, 3, h * 64:(h + 1) * 64], cind[:], [tX, t_c], [tgT], fast=False)
        K.op("act", lambda e: e.activation(out=gamT[:], in_=gview, func=AF.Exp), reads=[tgT], writes=[t_gam])
        K.op("act", lambda e: e.copy(out=S_["cum"][:], in_=pcum[:, 0:384]), reads=[tcum], writes=[T_["cum"]])
        tt_op("dve", S_["d0"][:], S_["cum"][:], LW_, ALU.subtract, [T_["cum"], tX], [T_["d0"]])
        tt_op("dve", S_["d1"][:], ptot[:, 0:384], S_["cum"][:], ALU.subtract, [ttot, T_["cum"]], [T_["d1"]])
        K.op("act", lambda e: e.activation(out=S_["e0"][:], in_=S_["d0"][:], func=AF.Exp), reads=[T_["d0"]], writes=[T_["e0"]])
        K.op("act", lambda e: e.activation(out=S_["em"][:], in_=S_["cum"][:], func=AF.Exp, scale=-1.0), reads=[T_["cum"]], writes=[T_["em"]])
        K.op("act", lambda e: e.activation(out=S_["ep"][:], in_=S_["cum"][:], func=AF.Exp), reads=[T_["cum"]], writes=[T_["ep"]])
        K.op("act", lambda e: e.activation(out=S_["eh"][:], in_=S_["d1"][:], func=AF.Exp), reads=[T_["d1"]], writes=[T_["eh"]])
        yield
        K.op("dve", lambda e: e.scalar_tensor_tensor(out=RD(S_["At"][:], True), in0=KK_, scalar=-1.0, in1=S_["e0"][:], op0=ALU.mult, op1=ALU.mult),
             reads=[tX, T_["e0"]], writes=[T_["At"]])
        tt_op("pool", S_["Bt"][:], BD_, S_["em"][:], ALU.mult, [tX, T_["em"]], [T_["Bt"]], r=True)
        tt_op("dve", S_["Kt"][:], KD_, S_["em"][:], ALU.mult, [tX, T_["em"]], [T_["Kt"]], r=True)
        tt_op("pool", S_["Rt"][:], R_, S_["ep"][:], ALU.mult, [tX, T_["ep"]], [T_["Rt"]], r=True)
        tt_op("dve", S_["Bh"][:], BD_, S_["eh"][:], ALU.mult, [tX, T_["eh"]], [T_["Bh"]], r=True)
        tt_op("pool", S_["Kh"][:], KD_, S_["eh"][:], ALU.mult, [tX, T_["eh"]], [T_["Kh"]], r=True)
        yield
        for src, dst in [("At", "AtT"), ("Bt", "BtT"), ("Kt", "KtT"), ("Rt", "RtT")]:
            for g in range(2):
                pb, tb = B.single()
                v = pb[0:64, 0:384].rearrange("p (h n) -> p h n", n=128)
                for j in range(3):
                    h = g * 3 + j
                    K.op("pe", lambda e, h=h, j=j, v=v, src=src: e.transpose(out=v[:, j, :], in_=S_[src][:, h * 64:(h + 1) * 64], identity=identf[:]),
                         reads=[T_[src], t_idf], writes=[tb])
                ev(fT[dst][:, g * 3:g * 3 + 3, :], v, [tb], [t_fT[dst]], r=True)
                yield
        for nm, a, b, msk in [("N", "BtT", "AtT", m_str3), ("LakT", "KtT", "AtT", m_str3), ("RbT", "BtT", "RtT", m_inc3), ("RkT", "KtT", "RtT", m_inc3)]:
            for g in range(2):
                pb, tb = B.single()
                v = h3(pb[:, 0:384])
                for j in range(3):
                    h = g * 3 + j
                    mm(v[:, j, :], fT[a][:, h, :], fT[b][:, h, :], [t_fT[a], t_fT[b]], [tb])
                tt_op("dve", gr[nm][:, g * 3:g * 3 + 3, :], v, msk[:], ALU.mult, [tb, t_c], [t_gr[nm]], r=True)
                yield
        cur = 0
        K.op("pool", lambda e: e.tensor_copy(out=RD(Nk[0][:], True), in_=gr["N"][:]), reads=[t_gr["N"]], writes=[t_Nk[0]])
        for g in range(2):
            pb, tb = B.single()
            v = h3(pb[:, 0:384])
            for j in range(3):
                h = g * 3 + j
                K.op("pe", lambda e, h=h, j=j, v=v: e.transpose(out=v[:, j, :], in_=gr["N"][:, h, :], identity=identf[:]),
                     reads=[t_gr["N"], t_idf], writes=[tb])
            ev(NkT[0][:, g * 3:g * 3 + 3, :], v, [tb], [t_NkT[0]], r=True)
            tt_op("dve", TT[:, g * 3:g * 3 + 3, :], gr["N"][:, g * 3:g * 3 + 3, :], id3[:], ALU.add, [t_gr["N"], t_c], [t_TT], r=True)
            yield
        for lev in range(5):
            nx = 1 - cur
            last = lev == 4
            for g in range(2):
                pb, tb = B.single()
                vb = h3(pb[:, 0:384])
                for j in range(3):
                    h = g * 3 + j
                    mm(vb[:, j, :], Nk[cur][:, h, :], NkT[cur][:, h, :], [t_NkT[cur], t_Nk[cur]], [tb])
                ev(NkT[nx][:, g * 3:g * 3 + 3, :], vb, [tb], [t_NkT[nx]], r=True)
                yield
                if not last:
                    pa, ta_ = B.single()
                    va = h3(pa[:, 0:384])
                    for j in range(3):
                        h = g * 3 + j
                        mm(va[:, j, :], NkT[cur][:, h, :], Nk[cur][:, h, :], [t_NkT[cur], t_Nk[cur]], [ta_])
                    ev(Nk[nx][:, g * 3:g * 3 + 3, :], va, [ta_], [t_Nk[nx]], r=True)
                    yield
            for g in range(2):
                pc_, tc_ = B.single()
                vc = h3(pc_[:, 0:384])
                for j in range(3):
                    h = g * 3 + j
                    mm(vc[:, j, :], NkT[nx][:, h, :], TT[:, h, :], [t_NkT[nx], t_TT], [tc_])
                tt_op("dve", TT[:, g * 3:g * 3 + 3, :], vc, TT[:, g * 3:g * 3 + 3, :], ALU.add, [tc_, t_TT], [t_TT], r=True)
                yield
            cur = nx
        pb, tb = B.single()
        for h in range(6):
            mm(pb[:, h * 64:(h + 1) * 64], gr["LakT"][:, h, :], Vh(h), [t_gr["LakT"], T_["Vr"]], [tb])
        ev(S_["X1"][:], pb[:, 0:384], [tb], [T_["X1"]], r=True)
        yield
        pb, tb = B.single()
        for h in range(6):
            mm(pb[:, h * 64:(h + 1) * 64], TT[:, h, :], S_["X1"][:, h * 64:(h + 1) * 64], [t_TT, T_["X1"]], [tb])
        ev(S_["Z"][:], pb[:, 0:384], [tb], [T_["Z"]])
        yield
        for g in range(2):
            pb, tb = B.single()
            v = pb[0:64, 0:384].rearrange("p (h n) -> p h n", n=128)
            for j in range(3):
                h = g * 3 + j
                mm(v[:, j, :], S_["At"][:, h * 64:(h + 1) * 64], TT[:, h, :], [T_["At"], t_TT], [tb])
            ev(fT["WmT"][:, g * 3:g * 3 + 3, :], v, [tb], [t_fT["WmT"]], r=True)
            yield
        for c in range(2):
            cs = slice(c * 64, (c + 1) * 64)
            pu, tu = B.single()
            for h in range(6):
                mm(pu[cs, h * 64:(h + 1) * 64], fT["WmT"][:, h, cs], M[:, h, :], [t_fT["WmT"], t_M], [tu])
                mm(psY1[cs, h * 64:(h + 1) * 64], fT["RtT"][:, h, cs], M[:, h, :], [t_fT["RtT"], t_M], [t_psY1])
            tt_op("dve", S_["U"][cs, :], pu[cs, 0:384], S_["Z"][cs, :], ALU.add, [tu, T_["Z"]], [T_["U"]], r=True)
            yield
            pm_, tm_ = B.single()
            mv = pm_[0:64, 0:384].rearrange("p (h n) -> p h n", n=64)
            for h in range(6):
                mm(mv[:, h, :], S_["Bh"][cs, h * 64:(h + 1) * 64], S_["U"][cs, h * 64:(h + 1) * 64], [T_["Bh"], T_["U"]], [tm_], start=True, stop=False)
                mm(mv[:, h, :], S_["Kh"][cs, h * 64:(h + 1) * 64], S_["Vr"][cs, h * 64:(h + 1) * 64], [T_["Kh"], T_["Vr"]], [tm_], start=False, stop=True)
            tt_op("dve", Mt[:], M[:], gamT[:, :, c:c + 1].broadcast_to([64, 6, 64]), ALU.mult, [t_M, t_gam], [t_Mt])
            tt_op("dve", M[:], mv, Mt[:], ALU.add, [tm_, t_Mt], [t_M], r=True)
            yield
        py, ty = B.single()
        for h in range(6):
            mm(py[:, h * 64:(h + 1) * 64], gr["RbT"][:, h, :], S_["U"][:, h * 64:(h + 1) * 64], [t_gr["RbT"], T_["U"]], [ty], start=True, stop=False)
            mm(py[:, h * 64:(h + 1) * 64], gr["RkT"][:, h, :], Vh(h), [t_gr["RkT"], T_["Vr"]], [ty], start=False, stop=True)
        K.op("act", lambda e: e.copy(out=S_["Y1"][:], in_=psY1[:, 0:384]), reads=[t_psY1], writes=[T_["Y1"]])
        tt_op("dve", S_["Y"][:], py[:, 0:384], S_["Y1"][:], ALU.add, [ty, T_["Y1"]], [T_["Y"]])
        yield
        if d == 0:
            K.dma(ydst[r0:r0 + 128, :], S_["Y"][:], reads=[T_["Y"]], writes=[t_yd])
        else:
            pb, tb = B.single()
            mm(pb[:, 0:384], Jm[:], S_["Y"][:], [t_c, T_["Y"]], [tb], fast=False)
            ev(S_["Y1"][:], pb[:, 0:384], [tb], [T_["Y1"]])
            K.dma(ydst[r0:r0 + 128, :], S_["Y1"][:], reads=[T_["Y1"]], writes=[t_yd])

    yield "rw_scan%d_%d" % (d, l)
    K.op("pool", lambda e: e.memset(M[:], 0.0), reads=[t_M], writes=[t_M])
    order = list(range(NT)) if d == 0 else [1, 0] + list(range(NT - 1, NCTX_T - 1, -1))
    for idx, tt in enumerate(order):
        yield from tile(tt, d, idx)
        yield


def gen_rw_readout(K, A, l, rwA, t_rwA, y0, t_y0, y1, t_y1, mixT, t_mix):
    identb, t_idb = make_ident(K, BF16)
    t_c = Trk()
    lng = K.sb([128, 384]); lnb = K.sb([128, 384])
    K.dma(lng[:], A["rw_ln_g"][l, :].partition_broadcast(128), writes=[t_c])
    K.dma(lnb[:], A["rw_ln_b"][l, :].partition_broadcast(128), writes=[t_c])
    NBUF = 2
    Y0 = [K.sb([128, 384]) for _ in range(NBUF)]; tY0 = [Trk() for _ in range(NBUF)]
    Y1_ = [K.sb([128, 384]) for _ in range(NBUF)]; tY1 = [Trk() for _ in range(NBUF)]
    GB = [K.sb([128, 2, 384]) for _ in range(NBUF)]; tGB = [Trk() for _ in range(NBUF)]
    W1 = [K.sb([128, 384]) for _ in range(NBUF)]; tW1 = [Trk() for _ in range(NBUF)]
    W2 = [K.sb([128, 384]) for _ in range(NBUF)]; tW2 = [Trk() for _ in range(NBUF)]
    ST = [K.sb([128, 6]) for _ in range(NBUF)]; tST = [Trk() for _ in range(NBUF)]
    OB = [K.sb([128, 384], BF16) for _ in range(NBUF)]; tOB = [Trk() for _ in range(NBUF)]
    OBT = [K.sb([128, 3, 128], BF16) for _ in range(NBUF)]; tOBT = [Trk() for _ in range(NBUF)]
    PS = [K.ps([128, 3, 128], BF16) for _ in range(2)]; tPS = [Trk(), Trk()]

    def tt_op(eng, out, in0, in1, op, reads, writes):
        K.op(eng, lambda e: e.tensor_tensor(out=out, in0=in0, in1=in1, op=op), reads=reads, writes=writes)

    def body(tt):
        n = tt % NBUF
        r0 = tt * 128
        y = Y0[n]; ty_ = tY0[n]; gb = GB[n]; t_gb = tGB[n]; w1 = W1[n]; w2 = W2[n]; st6 = ST[n]; t_st6 = tST[n]
        ob = OB[n]; t_ob = tOB[n]; obT = OBT[n]; t_obT = tOBT[n]
        T_ = {"w1": tW1[n], "w2": tW2[n]}
        K.dma(y[:], y0[r0:r0 + 128, :], reads=[t_y0], writes=[ty_])
        K.dma(Y1_[n][:], y1[r0:r0 + 128, :], reads=[t_y1], writes=[tY1[n]])
        K.dma(gb[:], rwA[r0:r0 + 128, 3:5, :], reads=[t_rwA], writes=[t_gb])
        tt_op("pool", y[:], y[:], Y1_[n][:], ALU.add, [ty_, tY1[n]], [ty_])
        y3 = y[:].rearrange("p (h n) -> p h n", h=6)
        w13 = w1[:].rearrange("p (h n) -> p h n", h=6)
        w23 = w2[:].rearrange("p (h n) -> p h n", h=6)
        K.op("dve", lambda e: e.tensor_reduce(out=st6[:], in_=y3, axis=AX.X, op=ALU.add), reads=[ty_], writes=[t_st6])
        K.op("dve", lambda e: e.tensor_scalar(out=st6[:], in0=st6[:], scalar1=-1.0 / 64, scalar2=None, op0=ALU.mult), reads=[t_st6], writes=[t_st6])
        tt_op("dve", w13, y3, st6[:].unsqueeze(2).broadcast_to([128, 6, 64]), ALU.add, [ty_, t_st6], [T_["w1"]])
        tt_op("pool", w2[:], w1[:], w1[:], ALU.mult, [T_["w1"]], [T_["w2"]])
        K.op("dve", lambda e: e.tensor_reduce(out=st6[:], in_=w23, axis=AX.X, op=ALU.add), reads=[T_["w2"]], writes=[t_st6])
        K.op("act", lambda e: e.activation(out=st6[:], in_=st6[:], func=AF.Sqrt, scale=1.0 / 64, bias=64e-5), reads=[t_st6], writes=[t_st6])
        K.op("dve", lambda e: e.reciprocal(out=st6[:], in_=st6[:]), reads=[t_st6], writes=[t_st6])
        tt_op("dve", w13, w13, st6[:].unsqueeze(2).broadcast_to([128, 6, 64]), ALU.mult, [T_["w1"], t_st6], [T_["w1"]])
        tt_op("pool", w1[:], w1[:], lng[:], ALU.mult, [T_["w1"], t_c], [T_["w1"]])
        tt_op("pool", w1[:], w1[:], lnb[:], ALU.add, [T_["w1"], t_c], [T_["w1"]])
        tt_op("dve", w1[:], w1[:], gb[:, 1, :], ALU.add, [T_["w1"], t_gb], [T_["w1"]])
        tt_op("dve", ob[:], w1[:], gb[:, 0, :], ALU.mult, [T_["w1"], t_gb], [t_ob])
        vb = PS[tt % 2]; tb = tPS[tt % 2]
        for c in range(3):
            K.op("pe", lambda e, c=c: e.transpose(out=vb[:, c, :], in_=ob[:, c * 128:(c + 1) * 128], identity=identb[:]), reads=[t_ob, t_idb], writes=[tb])
        K.op("act", lambda e: e.copy(out=obT[:], in_=vb[:]), reads=[tb], writes=[t_obT])
        K.dma(mixT[256:640, r0:r0 + 128].rearrange("(c p) n -> p c n", p=128), obT[:], reads=[t_obT], writes=[t_mix])

    yield "rw_readout_%d" % l
    for tt in range(NT):
        body(tt)
        yield


def phase_wout(K, A, l, xs, t_xs, modscr, t_mod, mixT, t_mix, need_ctx):
    K.push()
    wb = K.sb([128, 8, D], BF16); t_wb = Trk()
    stg = [K.sb([128, 8, 512]) for _ in range(2)]; t_stg = [Trk(), Trk()]
    for i in range(2):
        K.dma(stg[i][:], A["w_out"][l, :, i * 512:(i + 1) * 512].rearrange("(k p) n -> p k n", p=128), writes=[t_stg[i]])
        K.op("pool", lambda e, i=i: e.tensor_copy(out=wb[:, :, i * 512:(i + 1) * 512], in_=stg[i][:]), reads=[t_stg[i]], writes=[t_wb])
    m2 = [K.sb([128, D]) for _ in range(2)]; t_m2 = Trk()
    for which in range(2):
        K.dma(m2[which][:], modscr[which, 2 * D:3 * D].partition_broadcast(128), reads=[t_mod], writes=[t_m2])
    mt = [K.sb([128, 8, 128], BF16) for _ in range(2)]; t_mt = [Trk(), Trk()]
    xt = [K.sb([128, D]) for _ in range(2)]; t_xt = [Trk(), Trk()]
    tmp = K.sb([128, D]); t_tmp = Trk()
    ps = [K.ps([128, 512]) for _ in range(4)]; t_ps = [Trk() for _ in range(4)]

    def body(tt, n):
        m_ = mt[n % 2]; tm = t_mt[n % 2]; x_ = xt[n % 2]; tx = t_xt[n % 2]
        K.dma(m_[:], mixT[:, tt * 128:(tt + 1) * 128].rearrange("(c p) n -> p c n", p=128), reads=[t_mix], writes=[tm])
        K.dma(x_[:], xs[tt * 128:(tt + 1) * 128, :], reads=[t_xs], writes=[tx])
        mm_ = m2[1 if tt < NCTX_T else 0]
        for hf in range(2):
            p_ = ps[(2 * n + hf) % 4]; tp = t_ps[(2 * n + hf) % 4]
            for k in range(8):
                K.op("pe", lambda e, k=k, p_=p_, hf=hf: e.matmul(p_[:], lhsT=m_[:, k, :], rhs=wb[:, k, hf * 512:(hf + 1) * 512],
                                                                 start=(k == 0), stop=(k == 7)), reads=[tm, t_wb], writes=[tp])
            K.op("dve", lambda e, p_=p_, hf=hf: e.tensor_tensor(out=tmp[:, hf * 512:(hf + 1) * 512], in0=p_[:], in1=mm_[:, hf * 512:(hf + 1) * 512], op=ALU.mult),
                 reads=[tp, t_m2], writes=[t_tmp])
        K.op("pool", lambda e: e.tensor_tensor(out=x_[:], in0=x_[:], in1=tmp[:], op=ALU.add), reads=[tx, t_tmp], writes=[tx])
        K.dma(xs[tt * 128:(tt + 1) * 128, :], x_[:], reads=[tx], writes=[t_xs])

    for n, tt in enumerate(range(0 if need_ctx else NCTX_T, NT)):
        body(tt, n)
    K.pop()


def gen_conv(K, A, l, ub, vb, t_ub, t_vb, part=0, nparts=1, NC_=6):
    cf = [K.sb([128, D]) for _ in range(NC_)]; t_cf = [Trk() for _ in range(NC_)]
    cb = [K.sb([128, D], BF16) for _ in range(NC_)]; t_cb = [Trk() for _ in range(NC_)]
    engs = ["act", "dve", "pool"]
    jobs = []
    for i in range(128):
        jobs.append((A["pe_ut"][l, i].rearrange("p c e -> p (c e)"), ub[i].rearrange("p c e -> p (c e)"), t_ub))
        jobs.append((A["pe_v"][l, i * 128:(i + 1) * 128, :], vb[i], t_vb))
    per = len(jobs) // nparts
    jobs = jobs[part * per:(part + 1) * per]
    AH = min(4, NC_ - 1)

    def cv_load(n):
        K.dma(cf[n % NC_][:], jobs[n][0], writes=[t_cf[n % NC_]])

    def job(n):
        b_ = n % NC_
        eng = engs[n % 3]
        if eng == "act":
            K.op("act", lambda e: e.copy(out=cb[b_][:], in_=cf[b_][:]), reads=[t_cf[b_]], writes=[t_cb[b_]])
        else:
            K.op(eng, lambda e: e.tensor_copy(out=cb[b_][:], in_=cf[b_][:]), reads=[t_cf[b_]], writes=[t_cb[b_]])
        if n + AH < len(jobs):
            cv_load(n + AH)
        K.dma(jobs[n][1], cb[b_][:], reads=[t_cb[b_]], writes=[jobs[n][2]])

    yield "conv%d_%d" % (part, l)
    for n in range(AH):
        cv_load(n)
    for n in range(len(jobs)):
        job(n)
        if n % 2 == 1:
            yield


def phase_peer(K, A, l, xs, t_xs, modscr, t_mod, need_ctx, ubs, vbs, tconv):
    ub = ubs[l]; vb = vbs[l]; t_ub, t_vb = tconv
    K.push()
    identb, t_idb = make_ident(K, BF16)
    mods = load_mod(K, modscr, t_mod, A["norm_ffn_g"][l, :], 3, 4)
    m5 = [K.sb([128, D]) for _ in range(2)]; t_m5 = Trk()
    for which in range(2):
        K.dma(m5[which][:], modscr[which, 5 * D:6 * D].partition_broadcast(128), reads=[t_mod], writes=[t_m5])
    wq = K.sb([128, 8, 2048], BF16); t_wq = Trk()
    K.push()
    stg = [K.sb([128, 8, 512]) for _ in range(2)]; t_stg = [Trk(), Trk()]
    for i in range(4):
        K.dma(stg[i % 2][:], A["pe_wq"][l, :, i * 512:(i + 1) * 512].rearrange("(k p) n -> p k n", p=128), writes=[t_stg[i % 2]])
        K.op("pool", lambda e, i=i: e.tensor_copy(out=wq[:, :, i * 512:(i + 1) * 512], in_=stg[i % 2][:]), reads=[t_stg[i % 2]], writes=[t_wq])
    K.pop()
    keysT = K.sb([128, 16, 128]); t_keys = Trk()
    K.dma(keysT[:], A["keysT"][l].rearrange("c d k -> d c k"), writes=[t_keys])
    bk = [K.ps([128, 512]) for _ in range(8)]; t_bk = [Trk() for _ in range(8)]
    rot = [0]

    def rb():
        b = rot[0] % 4
        rot[0] += 1
        return bk[b], t_bk[b]

    xk = [K.sb([128, D]) for _ in range(2)]; t_xk = [Trk(), Trk()]
    scr = K.sb([128, D]); t_scr = Trk()
    ss = K.sb([128, 1]); t_ss = Trk()
    h = K.sb([128, D], BF16); t_h = Trk()
    hT = K.sb([128, 8, 256], BF16); t_hT = Trk()
    qT = K.sb([128, 16, 256]); t_qT = Trk()
    Ssb = [K.sb([128, 16, 128]) for _ in range(2)]; t_S = [Trk(), Trk()]
    s2pp = [K.sb([128, 8, 128]) for _ in range(2)]; t_s2 = [Trk(), Trk()]
    Dp = [K.sb([128, 8, 128], BF16) for _ in range(2)]; t_Dp = [Trk(), Trk()]
    top = K.sb([128, 8, 2, 16]); t_top = Trk()
    wk = K.sb([128, 256]); t_wk = Trk()
    cand = K.sb([128, 256]); t_cand = Trk()
    ctop = K.sb([128, 8, 16]); t_ctop = Trk()
    zs = K.sb([128, 8, 16]); t_zs = Trk()
    st8 = K.sb([128, 8]); t_st8 = Trk()
    NB = 4
    PSPL = 0
    ut = [K.sb([128, 8, 128], BF16) for _ in range(NB)]; t_ut = [Trk() for _ in range(NB)]
    vt = [K.sb([128, D], BF16) for _ in range(NB)]; t_vt = [Trk() for _ in range(NB)]
    ga = [K.sb([128, 256]) for _ in range(3)]; t_ga = [Trk() for _ in range(3)]
    EE = [[K.sb([128, 8, 128]) for _ in range(2)] for _ in range(2)]; t_EE = [[Trk(), Trk()], [Trk(), Trk()]]
    NG = 3
    E2 = [K.sb([128, 8, 128]) for _ in range(2)]; t_E2 = [Trk(), Trk()]
    e1 = [K.sb([128, 8, 128]) for _ in range(2)]; t_e1 = [Trk(), Trk()]
    GG = [[K.sb([128, 8, 128], BF16) for _ in range(3)] for _ in range(2)]; t_GG = [[Trk() for _ in range(3)] for _ in range(2)]
    AW = [K.sb([128, 256], BF16) for _ in range(2)]; t_AW = [Trk(), Trk()]
    tmpo = K.sb([128, D]); t_tmpo = Trk()
    NEGBIG = -1.0e30

    def block(t0, which):
        for j in range(2):
            tt = t0 + j
            K.dma(xk[j][:], xs[tt * 128:(tt + 1) * 128, :], reads=[t_xs], writes=[t_xk[j]])
            norm_mod_tile(K, xk[j][:], t_xk[j], mods[which], h, t_h, scr, t_scr, ss, t_ss)
            pb, tb = rb()
            pst = pb.bitcast(BF16).rearrange("p (c n) -> p c n", n=128)
            transpose_to(K, h, t_h, 8, identb, t_idb, pst, tb, hT[:, :, j * 128:(j + 1) * 128], t_hT)
        for cs in range(16):
            pb, tb = rb()
            for k in range(8):
                K.op("pe", lambda e, k=k, cs=cs, pb=pb: e.matmul(pb[:, 0:256], lhsT=wq[:, k, cs * 128:(cs + 1) * 128], rhs=hT[:, k, :],
                                                                 start=(k == 0), stop=(k == 7)), reads=[t_wq, t_hT], writes=[tb])
            if cs % 2:
                K.op("act", lambda e, cs=cs, pb=pb: e.copy(out=qT[:, cs, :], in_=pb[:, 0:256]), reads=[tb], writes=[t_qT])
            else:
                K.op("dve", lambda e, cs=cs, pb=pb: e.tensor_copy(out=qT[:, cs, :], in_=pb[:, 0:256]), reads=[tb], writes=[t_qT])
        for j in range(2):
            for q4 in range(4):
                pb, tb = rb()
                for u in range(4):
                    cs = q4 * 4 + u
                    K.op("pe", lambda e, cs=cs, u=u, pb=pb, j=j: e.matmul(pb[:, u * 128:(u + 1) * 128], lhsT=qT[:, cs, j * 128:(j + 1) * 128],
                                                                         rhs=keysT[:, cs, :], start=True, stop=True),
                         reads=[t_qT, t_keys], writes=[tb])
                K.op("act", lambda e, q4=q4, pb=pb, j=j: e.copy(out=Ssb[j][:, q4 * 4:(q4 + 1) * 4, :], in_=pb[:].rearrange("p (c n) -> p c n", n=128)),
                     reads=[tb], writes=[t_S[j]])
            for p in range(8):
                for side in range(2):
                    src = Ssb[j][:, 2 * p + side, :]
                    K.op("dve", lambda e, p=p, side=side, src=src: e.max(out=top[:, p, side, 0:8], in_=src), reads=[t_S[j]], writes=[t_top])
                    K.op("dve", lambda e, p=p, side=side, src=src: e.match_replace(out=wk[:, 0:128], in_to_replace=top[:, p, side, 0:8], in_values=src,
                                                                                  imm_value=NEGBIG), reads=[t_S[j], t_top], writes=[t_wk])
                    K.op("dve", lambda e, p=p, side=side: e.max(out=top[:, p, side, 8:16], in_=wk[:, 0:128]), reads=[t_wk], writes=[t_top])
                c3 = cand[:].rearrange("p (a b) -> p a b", a=16)
                K.op("pool", lambda e, p=p, c3=c3: e.tensor_tensor(out=c3, in0=top[:, p, 0, :].unsqueeze(2).broadcast_to([128, 16, 16]),
                                                                   in1=top[:, p, 1, :].unsqueeze(1).broadcast_to([128, 16, 16]), op=ALU.add),
                     reads=[t_top], writes=[t_cand])
                K.op("dve", lambda e, p=p: e.max(out=ctop[:, p, 0:8], in_=cand[:]), reads=[t_cand], writes=[t_ctop])
                K.op("dve", lambda e, p=p: e.match_replace(out=wk[:], in_to_replace=ctop[:, p, 0:8], in_values=cand[:], imm_value=NEGBIG),
                     reads=[t_cand, t_ctop], writes=[t_wk])
                K.op("dve", lambda e, p=p: e.max(out=ctop[:, p, 8:16], in_=wk[:]), reads=[t_wk], writes=[t_ctop])
            tau = ctop[:, :, 15:16]
            K.op("dve", lambda e: e.tensor_tensor(out=zs[:], in0=ctop[:], in1=tau.broadcast_to([128, 8, 16]), op=ALU.subtract),
                 reads=[t_ctop], writes=[t_zs])
            K.op("act", lambda e: e.activation(out=zs[:], in_=zs[:], func=AF.Exp), reads=[t_zs], writes=[t_zs])
            K.op("dve", lambda e: e.tensor_reduce(out=st8[:], in_=zs[:], axis=AX.X, op=ALU.add), reads=[t_zs], writes=[t_st8])
            K.op("dve", lambda e: e.reciprocal(out=st8[:], in_=st8[:]), reads=[t_st8], writes=[t_st8])
            S4 = Ssb[j][:].rearrange("p (h s) n -> p h s n", s=2)
            K.op("dve", lambda e, S4=S4, j=j: e.tensor_tensor(out=s2pp[j][:], in0=S4[:, :, 1, :], in1=tau.broadcast_to([128, 8, 128]), op=ALU.subtract),
                 reads=[t_S[j], t_ctop], writes=[t_s2[j]])
            m1 = top[:, :, 0, 0:1]
            K.op("dve", lambda e, j=j: e.tensor_tensor(out=s2pp[j][:], in0=s2pp[j][:], in1=m1.broadcast_to([128, 8, 128]), op=ALU.add),
                 reads=[t_s2[j], t_top], writes=[t_s2[j]])
            K.op("act", lambda e, j=j: e.activation(out=E2[j][:], in_=s2pp[j][:], func=AF.Exp, bias=1.0e-3), reads=[t_s2[j]], writes=[t_E2[j]])
            K.op("dve", lambda e, S4=S4, j=j: e.tensor_tensor(out=e1[j][:], in0=S4[:, :, 0, :], in1=m1.broadcast_to([128, 8, 128]), op=ALU.subtract),
                 reads=[t_S[j], t_top], writes=[t_e1[j]])
            K.op("act", lambda e, j=j: e.activation(out=e1[j][:], in_=e1[j][:], func=AF.Exp), reads=[t_e1[j]], writes=[t_e1[j]])
            K.op("dve", lambda e, j=j: e.tensor_tensor(out=Dp[j][:], in0=identb[:].unsqueeze(1).broadcast_to([128, 8, 128]),
                                                      in1=st8[:].unsqueeze(2).broadcast_to([128, 8, 128]), op=ALU.mult),
                 reads=[t_idb, t_st8], writes=[t_Dp[j]])
        S4 = [Ssb[j][:].rearrange("p (h s) n -> p h s n", s=2) for j in range(2)]

        def st_load(c):
            K.dma(ut[c % NB][:], ub[c], reads=[t_ub], writes=[t_ut[c % NB]])
            K.dma(vt[c % NB][:], vb[c], reads=[t_vb], writes=[t_vt[c % NB]])

        def st_gate(c):
            for j in range(2):
                E_ = EE[j][c % 2]; tE = t_EE[j][c % 2]; G_ = GG[j][c % NG]; tG = t_GG[j][c % NG]
                if j == 0:
                    K.op("pool", lambda e, E_=E_, j=j: e.tensor_tensor(out=E_[:], in0=E2[j][:], in1=e1[j][:, :, c:c + 1].broadcast_to([128, 8, 128]), op=ALU.mult),
                         reads=[t_E2[j], t_e1[j]], writes=[tE])
                else:
                    if PSPL > 0:
                        K.op("pool", lambda e, E_=E_, j=j: e.tensor_tensor(out=E_[:, 0:PSPL, :], in0=E2[j][:, 0:PSPL, :],
                                                                           in1=e1[j][:, 0:PSPL, c:c + 1].broadcast_to([128, PSPL, 128]), op=ALU.mult),
                             reads=[t_E2[j], t_e1[j]], writes=[tE])
                    for p in range(PSPL, 8):
                        K.op("act", lambda e, E_=E_, j=j, p=p: e.activation(out=E_[:, p, :], in_=E2[j][:, p, :], func=AF.Identity, scale=e1[j][:, p, c:c + 1]),
                             reads=[t_E2[j], t_e1[j]], writes=[tE])
                K.op("dve", lambda e, E_=E_, G_=G_: e.scalar_tensor_tensor(out=G_[:], in0=E_[:], scalar=1.0, in1=E_[:], op0=ALU.is_ge, op1=ALU.mult),
                     reads=[tE], writes=[tG])

        def st_A(c):
            pa, ta = bk[c % 2], t_bk[c % 2]
            u_ = ut[c % NB]
            for k in range(8):
                K.op("pe", lambda e, k=k: e.matmul(pa[:, 0:256], lhsT=u_[:, k, :], rhs=hT[:, k, :], start=(k == 0), stop=(k == 7)),
                     reads=[t_ut[c % NB], t_hT], writes=[ta])

        def st_gelu(c):
            pa, ta = bk[c % 2], t_bk[c % 2]
            g_ = ga[c % 3]
            K.op("act", lambda e: e.activation(out=g_[:], in_=pa[:, 0:256], func=AF.Gelu_apprx_tanh), reads=[ta], writes=[t_ga[c % 3]])

        def st_W(c):
            pw, tw = bk[2 + c % 2], t_bk[2 + c % 2]
            for j in range(2):
                G_ = GG[j][c % NG]; tG = t_GG[j][c % NG]
                for p in range(8):
                    K.op("pe", lambda e, p=p, j=j, G_=G_: e.matmul(pw[:, j * 128:(j + 1) * 128], lhsT=G_[:, p, :], rhs=Dp[j][:, p, :],
                                                                  start=(p == 0), stop=(p == 7)), reads=[tG, t_Dp[j]], writes=[tw])

        def st_AW(c):
            pw, tw = bk[2 + c % 2], t_bk[2 + c % 2]
            aw = AW[c % 2]; taw = t_AW[c % 2]; g_ = ga[c % 3]
            K.op("dve", lambda e: e.tensor_tensor(out=aw[:], in0=g_[:], in1=pw[:, 0:256], op=ALU.mult), reads=[t_ga[c % 3], tw], writes=[taw])

        def st_out(c):
            aw = AW[c % 2]; taw = t_AW[c % 2]; v_ = vt[c % NB]
            for j in range(2):
                for hf in range(2):
                    b = 4 + 2 * j + hf
                    K.op("pe", lambda e, j=j, hf=hf, b=b: e.matmul(bk[b][:], lhsT=aw[:, j * 128:(j + 1) * 128], rhs=v_[:, hf * 512:(hf + 1) * 512],
                                                                  start=(c == 0), stop=(c == 127)), reads=[taw, t_vt[c % NB]], writes=[t_bk[b]])

        for c in range(NB):
            st_load(c)
        for s_ in range(-3, 128):
            if s_ >= 0:
                st_AW(s_)
            if 0 <= s_ + 2 < 128:
                st_A(s_ + 2)
            if s_ >= 0:
                st_out(s_)
            if 0 <= s_ + 1 < 128:
                st_W(s_ + 1)
            if s_ + 3 < 128:
                st_gate(s_ + 3)
            if 0 <= s_ + 2 < 128:
                st_gelu(s_ + 2)
            if s_ >= 0 and s_ + NB < 128:
                st_load(s_ + NB)
        for j in range(2):
            tt = t0 + j
            for hf in range(2):
                b = 4 + 2 * j + hf
                K.op("dve", lambda e, b=b, hf=hf: e.tensor_tensor(out=tmpo[:, hf * 512:(hf + 1) * 512], in0=bk[b][:], in1=m5[which][:, hf * 512:(hf + 1) * 512], op=ALU.mult),
                     reads=[t_bk[b], t_m5], writes=[t_tmpo])
            K.op("dve", lambda e, j=j: e.tensor_tensor(out=xk[j][:], in0=xk[j][:], in1=tmpo[:], op=ALU.add), reads=[t_xk[j], t_tmpo], writes=[t_xk[j]])
            K.dma(xs[tt * 128:(tt + 1) * 128, :], xk[j][:], reads=[t_xk[j]], writes=[t_xs])

    for t0 in range(0 if need_ctx else NCTX_T, NT, 2):
        block(t0, 1 if t0 < NCTX_T else 0)
    K.pop()


IN_SPECS = {
    "xall": ([NTOK, D], F32), "cvec": ([128, 16], F32),
    "ada_w": ([2, D, 6144], F32), "ada_b": ([2, 6144], F32),
    "norm_mix_g": ([2, D], F32), "norm_ffn_g": ([2, D], F32),
    "w_in": ([2, D, IN_COLS], F32), "w_out": ([2, D, D], F32),
    "mla_q_norm_g": ([2, 256], F32), "mla_w_qb": ([2, 256, 384], F32), "mla_kv_norm_g": ([2, 128], F32),
    "mla_w_kvb": ([2, 128, 512], F32), "mla_qk_g": ([2, 2, 96], F32),
    "rw_mu": ([2, 2, 1568], F32), "rw_w0": ([2, 2, 384], F32), "rw_w2": ([2, 2, 64, 384], F32),
    "rw_a0": ([2, 2, 384], F32), "rw_a2": ([2, 2, 64, 384], F32), "rw_g2": ([2, 160, 384], F32),
    "rw_k_k": ([2, 384], F32), "rw_k_a": ([2, 384], F32), "rw_r_k": ([2, 384], F32),
    "rw_ln_g": ([2, 384], F32), "rw_ln_b": ([2, 384], F32), "gqa_qk_g": ([2, 2, 64], F32),
    "pe_wq": ([2, D, 2048], F32), "keysT": ([2, 16, 128, 128], F32),
    "pe_ut": ([2, 128, 128, 8, 128], F32), "pe_v": ([2, 16384, D], F32),
    "rope_m": ([2048, 32], F32), "rope_g": ([2048, 64], F32),
}


def build(layers=(0, 1), upto=None, dbg=False, scopes=False, same=True):
    nc = bass.Bass("TRN2", target_bir_lowering=False)
    A = {k: nc.dram_tensor(k, sh, dt, kind="ExternalInput").ap() for k, (sh, dt) in IN_SPECS.items()}
    skind = "ExternalOutput" if dbg else "Internal"
    out = nc.dram_tensor("out", [2048, D], F32, kind="ExternalOutput").ap()
    S = {}
    S["xs"] = nc.dram_tensor("xs", [NTOK, D], F32, kind=skind).ap()
    S["mod"] = nc.dram_tensor("modscr", [2, 6144], F32, kind=skind).ap()
    S["p"] = nc.dram_tensor("pscr", [NTOK, IN_COLS], F32, kind=skind).ap()
    S["rwA"] = nc.dram_tensor("rwA", [NTOK, 11, 384], F32, kind=skind).ap()
    S["yscr"] = nc.dram_tensor("yscr", [NTOK, 384], F32, kind=skind).ap()
    S["yscr1"] = nc.dram_tensor("yscr1", [NTOK, 384], F32, kind=skind).ap()
    S["mixT"] = nc.dram_tensor("mixT", [D, NTOK], BF16, kind=skind).ap()
    DS = {}
    for nm, sh in [("qtm", [96, 4, NTOK]), ("ktm", [96, 4, NTOK]), ("vdm", [NTOK, 4, 128]),
                   ("qtg", [64, 6, NTOK]), ("ktg", [64, 2, NTOK]), ("vdg", [NTOK, 2, 128])]:
        DS[nm] = nc.dram_tensor(nm, sh, BF16, kind="Internal").ap()
        DS["t_" + nm] = Trk()
    UB = [nc.dram_tensor("ub%d" % l, [128, 128, 8, 128], BF16, kind="Internal").ap() for l in range(2)]
    VB = [nc.dram_tensor("vb%d" % l, [128, 128, D], BF16, kind="Internal").ap() for l in range(2)]
    with ExitStack() as es:
        K = Kb(nc, es, same_eng_sync=same, scopes=scopes)
        t_out = Trk()
        T = {k: Trk() for k in S}
        K.push()
        stg = [K.sb([128, D]) for _ in range(2)]; t_stg = [Trk(), Trk()]
        for tt in range(NT):
            K.dma(stg[tt % 2][:], A["xall"][tt * 128:(tt + 1) * 128, :], writes=[t_stg[tt % 2]])
            K.dma(S["xs"][tt * 128:(tt + 1) * 128, :], stg[tt % 2][:], reads=[t_stg[tt % 2]], writes=[T["xs"]])
        K.pop()
        for l in layers:
            need_ctx = l < 1
            K.cur = "ada_%d" % l
            phase_ada(K, A, l, S["mod"], T["mod"])
            tconv = (Trk(), Trk())
            K.run_lanes([("proj_in", gen_proj_in(K, A, l, S["xs"], T["xs"], S["mod"], T["mod"], S["p"], T["p"]), 1.0),
                         ("conv", gen_conv(K, A, l, UB[l], VB[l], tconv[0], tconv[1], 0, 2), 3.6)])
            if upto == "projin":
                break

            def prep_lane():
                yield from gen_mla_prep(K, A, l, S["p"], T["p"], DS)
                yield from gen_gqa_prep(K, A, l, S["p"], T["p"], DS)

            K.run_lanes([("mla_prep", gen_mla_prep(K, A, l, S["p"], T["p"], DS), 1.0),
                         ("gqa_prep", gen_gqa_prep(K, A, l, S["p"], T["p"], DS), 0.7),
                         ("rw_prep", gen_rw_prep(K, A, l, S["p"], T["p"], S["rwA"], T["rwA"]), 1.0),
                         ("conv", gen_conv(K, A, l, UB[l], VB[l], tconv[0], tconv[1], 1, 2, NC_=2), 3.6)])
            if upto == "prep":
                break
            K.run_lanes([("attn_m", gen_attn(K, A, l, DS, S["mixT"], T["mixT"], need_ctx, "m"), 1.0),
                         ("attn_g", gen_attn(K, A, l, DS, S["mixT"], T["mixT"], need_ctx, "g"), 1.5)])
            K.push()
            RC = rw_consts(K, A, l)
            K.run_lanes([("rw_scan0", gen_rw_scan(K, A, l, RC, 0, S["rwA"], T["rwA"], S["yscr"], T["yscr"], n_rot=3), 1.0),
                         ("rw_scan1", gen_rw_scan(K, A, l, RC, 1, S["rwA"], T["rwA"], S["yscr1"], T["yscr1"], n_rot=3), 1.0)])
            K.pop()
            K.run_lanes([("rw_readout", gen_rw_readout(K, A, l, S["rwA"], T["rwA"], S["yscr"], T["yscr"], S["yscr1"], T["yscr1"], S["mixT"], T["mixT"]), 1.0)])
            if upto == "rwscan":
                break
            K.cur = "wout_%d" % l
            phase_wout(K, A, l, S["xs"], T["xs"], S["mod"], T["mod"], S["mixT"], T["mixT"], need_ctx)
            if upto == "wout":
                break
            K.cur = "peer_%d" % l
            phase_peer(K, A, l, S["xs"], T["xs"], S["mod"], T["mod"], need_ctx, UB, VB, tconv)
            if upto == "peer":
                break
        K.push()
        stg = [K.sb([128, D]) for _ in range(2)]; t_stg = [Trk(), Trk()]
        for tt in range(16):
            K.dma(stg[tt % 2][:], S["xs"][(tt + 2) * 128:(tt + 3) * 128, :], reads=[T["xs"]], writes=[t_stg[tt % 2]])
            K.dma(out[tt * 128:(tt + 1) * 128, :], stg[tt % 2][:], reads=[t_stg[tt % 2]], writes=[t_out])
        K.pop()
        K.emit([t_out] + list(T.values()))
    return nc


def rope_tables():
    n = np.arange(2048)
    row = (n // 64).astype(np.float32); col = (n % 64).astype(np.float32)
    tabs = []
    for rdim in (32, 64):
        q = rdim // 4
        inv = (10000.0 ** (-np.arange(q, dtype=np.float32) / q)).astype(np.float32)
        ang = np.concatenate([row[:, None] * inv, col[:, None] * inv], -1).astype(np.float32)
        tabs.append(np.concatenate([np.cos(ang), np.sin(ang)], -1).astype(np.float32))
    return tabs


def prep_inputs(inp, batches):
    f = lambda a: np.ascontiguousarray(np.asarray(a, dtype=np.float32))
    rm, rg = rope_tables()
    shared = {k: f(inp[k]) for k in ["ada_w", "ada_b", "norm_mix_g", "norm_ffn_g", "w_in", "w_out", "mla_q_norm_g", "mla_w_qb",
                                     "mla_kv_norm_g", "mla_w_kvb", "mla_qk_g", "rw_mu", "rw_w0", "rw_w2", "rw_a0", "rw_a2", "rw_g2",
                                     "rw_k_k", "rw_k_a", "rw_ln_g", "rw_ln_b", "gqa_qk_g", "pe_wq", "pe_v"]}
    shared["rw_r_k"] = f(inp["rw_r_k"]).reshape(2, 384)
    shared["keysT"] = f(np.transpose(f(inp["pe_keys"]).reshape(2, 16, 128, 128), (0, 1, 3, 2)))
    u = f(inp["pe_u"]).reshape(2, 128, 128, 8, 128)
    shared["pe_ut"] = f(np.transpose(u, (0, 1, 4, 3, 2)))
    shared["rope_m"] = rm; shared["rope_g"] = rg
    maps = []
    for b in batches:
        m = dict(shared)
        m["xall"] = f(np.concatenate([inp["ctx"][b], inp["x"][b]], 0))
        cv = np.stack([f(inp["c"][b]).reshape(8, 128), f(inp["c_ctx"]).reshape(8, 128)], -1)
        m["cvec"] = f(np.transpose(cv, (1, 0, 2)).reshape(128, 16))
        maps.append(m)
    return maps


def kernel(**inputs):
    from concourse.bass_utils import run_bass_kernel_spmd
    nc = build()
    maps = prep_inputs(inputs, list(range(8)))
    res = run_bass_kernel_spmd(nc, maps, core_ids=list(range(8)))
    return np.stack([np.asarray(r["out"], dtype=np.float32) for r in res.results], 0)
```

```python
from contextlib import ExitStack
import numpy as np
import concourse.bass as bass
import concourse.mybir as mybir

F32 = mybir.dt.float32
BF16 = mybir.dt.bfloat16
U32 = mybir.dt.uint32
AF = mybir.ActivationFunctionType
ALU = mybir.AluOpType
AX = mybir.AxisListType

COMPUTE = ("pe", "dve", "act", "pool")
NDS = 24


class Trk:
    __slots__ = ("w", "r")

    def __init__(self):
        self.w = None
        self.r = {}


class Kb:
    def __init__(self, nc, es, same_eng_sync=True, scopes=False):
        self.scopes = scopes
        self.nc = nc
        self.es = es
        self.same = ("dve", "act", "pool") if same_eng_sync is True else tuple(same_eng_sync or ())
        self.eng = {"pe": nc.tensor, "dve": nc.vector, "act": nc.scalar, "pool": nc.gpsimd, "sp": nc.sync}
        self.q = {e: [] for e in self.eng}
        self.cnt = {e: 0 for e in COMPUTE}
        self.sem = {e: es.enter_context(nc.semaphore("sem_" + e)) for e in COMPUTE}
        self.dsem = [es.enter_context(nc.semaphore("dsem%d" % i)) for i in range(NDS)]
        self.dcount = 0
        self.waited = {}
        self.ntens = 0
        self.pend = {e: [] for e in self.eng}
        self.stacks = []
        self.cur = "main"

    def push(self):
        st = ExitStack()
        st.__enter__()
        self.stacks.append(self.es)
        self.es = st

    def pop(self):
        self.barrier()
        self.es.__exit__(None, None, None)
        self.es = self.stacks.pop()

    def run_lanes(self, lanes):
        self.push()
        active = [[nm, g, w, 0.0] for nm, g, w in lanes]
        while active:
            active.sort(key=lambda a: a[3])
            a = active[0]
            self.cur = a[0]
            try:
                nm = next(a[1])
                if isinstance(nm, str):
                    a[0] = nm
                a[3] += 1.0 / a[2]
            except StopIteration:
                active.remove(a)
        self.pop()

    def barrier(self):
        for eng in self.eng:
            deps = [("e", E, self.cnt[E]) for E in COMPUTE if self.cnt[E] > 0 and E != eng]
            for k in range(NDS):
                if self.dcount > k:
                    deps.append(("d", k, 16 * ((self.dcount - 1 - k) // NDS + 1)))
            self.pend[eng].extend(self._filter(eng, deps))

    def sb(self, shape, dtype=F32, name=None):
        self.ntens += 1
        t = self.es.enter_context(self.nc.sbuf_tensor(name or ("sb%d" % self.ntens), list(shape), dtype))
        return t

    def ps(self, shape, dtype=F32, name=None):
        self.ntens += 1
        t = self.es.enter_context(self.nc.psum_tensor(name or ("ps%d" % self.ntens), list(shape), dtype))
        return t

    def _deps(self, reads, writes):
        deps = []
        for t in reads:
            if t.w is not None:
                deps.append(t.w)
        for t in writes:
            if t.w is not None:
                deps.append(t.w)
            deps.extend(t.r.values())
        return deps

    def _filter(self, eng, deps):
        waits = []
        for d in deps:
            if d[0] == "e":
                _, E, n = d
                if E == eng and E not in self.same:
                    continue
                key = (eng, E)
                if self.waited.get(key, 0) >= n:
                    continue
                self.waited[key] = n
                waits.append((self.sem[E], n))
            else:
                _, k, v = d
                key = (eng, "d%d" % k)
                if self.waited.get(key, 0) >= v:
                    continue
                self.waited[key] = v
                waits.append((self.dsem[k], v))
        return waits

    def _mark(self, tok, reads, writes):
        for t in reads:
            t.r[tok[1] if tok[0] == "e" else ("d%d" % tok[1])] = tok
        for t in writes:
            t.w = tok
            t.r = {}

    def op(self, eng, fn, reads=(), writes=()):
        deps = self._deps(reads, writes)
        waits = self.pend[eng] + self._filter(eng, deps)
        self.pend[eng] = []
        n = self.cnt[eng] + 1
        self.cnt[eng] = n
        self.q[eng].append((waits, fn, self.sem[eng], 1, self.cur))
        self._mark(("e", eng, n), reads, writes)

    def dma(self, out, in_, reads=(), writes=(), queue="sp", **kw):
        deps = self._deps(reads, writes)
        k = self.dcount % NDS
        v = 16 * (self.dcount // NDS + 1)
        self.dcount += 1
        if v > 16:
            deps.append(("d", k, v - 16))
        waits = self.pend[queue] + self._filter(queue, deps)
        self.pend[queue] = []
        self.q[queue].append((waits, lambda e: e.dma_start(out=out, in_=in_, **kw), self.dsem[k], 16, self.cur))
        self._mark(("d", k, v), reads, writes)

    def emit(self, final_tracks=()):
        deps = []
        for t in final_tracks:
            if t.w is not None:
                deps.append(t.w)
        fw = self._filter("sp", deps)
        nc = self.nc
        q = self.q
        with nc.Block() as block:
            def run(e, recs, extra=()):
                i = 0
                while i < len(recs):
                    sc = recs[i][4]
                    j = i
                    while j < len(recs) and recs[j][4] == sc:
                        j += 1
                    if self.scopes:
                        with nc.named_scope(sc):
                            for waits, fn, sem, inc, _ in recs[i:j]:
                                for s, v in waits:
                                    e.wait_ge(s, v)
                                fn(e).then_inc(sem, inc)
                    else:
                        for waits, fn, sem, inc, _ in recs[i:j]:
                            for s, v in waits:
                                e.wait_ge(s, v)
                            fn(e).then_inc(sem, inc)
                    i = j
                for s, v in extra:
                    e.wait_ge(s, v)

            pend = self.pend

            @block.sync
            def _(e):
                run(e, q["sp"], pend["sp"] + fw)

            @block.tensor
            def _(e):
                run(e, q["pe"], pend["pe"])

            @block.vector
            def _(e):
                run(e, q["dve"], pend["dve"])

            @block.scalar
            def _(e):
                run(e, q["act"], pend["act"])

            @block.gpsimd
            def _(e):
                run(e, q["pool"], pend["pool"])


D = 1024
NTOK = 2304
NT = 18
NCTX_T = 2
IN_COLS = 2624
C_MLA, C_RW, C_GQA = 0, 416, 1984
EPS = 1e-6


def make_ident(K, dtype):
    idf = K.sb([128, 128], F32)
    t = Trk()
    K.op("pool", lambda e: e.memset(idf[:], 0.0), writes=[t])
    K.op("pool", lambda e: e.affine_select(out=idf[:], in_=idf[:], pattern=[[-1, 128]], compare_op=ALU.not_equal,
                                           fill=1.0, base=0, channel_multiplier=1), reads=[t], writes=[t])
    if dtype == F32:
        return idf, t
    idb = K.sb([128, 128], dtype)
    t2 = Trk()
    K.op("dve", lambda e: e.tensor_copy(out=idb[:], in_=idf[:]), reads=[t], writes=[t2])
    return idb, t2


def bcast_load(K, dst, src_row, tr_src, tr_dst, parts=128):
    K.dma(dst, src_row.partition_broadcast(parts), reads=tr_src, writes=[tr_dst])


def phase_ada(K, A, l, modscr, t_mod):
    K.push()
    cv = K.sb([128, 16]); t_cv = Trk()
    K.dma(cv[:], A["cvec"], writes=[t_cv])
    sc = K.sb([128, 16]); t_sc = Trk()
    K.op("act", lambda e: e.activation(out=sc[:], in_=cv[:], func=AF.Silu), reads=[t_cv], writes=[t_sc])
    bias = K.sb([2, 6144]); t_b = Trk()
    K.dma(bias[:], A["ada_b"][l, :].partition_broadcast(2), writes=[t_b])
    res = K.sb([2, 6144]); t_res = Trk()
    wt = [K.sb([128, 8, 512]) for _ in range(2)]; t_w = [Trk(), Trk()]
    ps = [K.ps([2, 512]) for _ in range(2)]; t_ps = [Trk(), Trk()]
    for cb in range(12):
        w = wt[cb % 2]; tw = t_w[cb % 2]; p = ps[cb % 2]; tp = t_ps[cb % 2]
        K.dma(w[:], A["ada_w"][l, :, cb * 512:(cb + 1) * 512].rearrange("(k p) n -> p k n", p=128), writes=[tw])
        for k in range(8):
            K.op("pe", lambda e, k=k, w=w, p=p: e.matmul(p[:], lhsT=sc[:, 2 * k:2 * k + 2], rhs=w[:, k, :],
                                                       start=(k == 0), stop=(k == 7)), reads=[t_sc, tw], writes=[tp])
        K.op("dve", lambda e, p=p, cb=cb: e.tensor_tensor(out=res[:, cb * 512:(cb + 1) * 512], in0=p[:],
                                                          in1=bias[:, cb * 512:(cb + 1) * 512], op=ALU.add),
             reads=[tp, t_b], writes=[t_res])
    K.dma(modscr, res[:], reads=[t_res], writes=[t_mod])
    K.pop()


def load_mod(K, modscr, t_mod, g_row, i_shift, i_scale):
    out = []
    g = K.sb([128, D]); t_g = Trk()
    K.dma(g[:], g_row.partition_broadcast(128), writes=[t_g])
    for which in range(2):
        gm = K.sb([128, D]); sh = K.sb([128, D]); t1 = Trk(); t2 = Trk()
        K.dma(gm[:], modscr[which, i_scale * D:(i_scale + 1) * D].partition_broadcast(128), reads=[t_mod], writes=[t1])
        K.dma(sh[:], modscr[which, i_shift * D:(i_shift + 1) * D].partition_broadcast(128), reads=[t_mod], writes=[t2])
        K.op("pool", lambda e, gm=gm: e.scalar_tensor_tensor(out=gm[:], in0=gm[:], scalar=1.0, in1=g[:], op0=ALU.add, op1=ALU.mult),
             reads=[t1, t_g], writes=[t1]) if False else \
            K.op("dve", lambda e, gm=gm: e.scalar_tensor_tensor(out=gm[:], in0=gm[:], scalar=1.0, in1=g[:], op0=ALU.add, op1=ALU.mult),
                 reads=[t1, t_g], writes=[t1])
        out.append((gm, t1, sh, t2))
    return out


def norm_mod_tile(K, xt, t_x, mods, h, t_h, scr, t_scr, ss, t_ss):
    gm, t_gm, sh, t_sh = mods
    K.op("act", lambda e: e.activation(out=scr[:], in_=xt, func=AF.Square, accum_out=ss[:]), reads=[t_x], writes=[t_scr, t_ss])
    K.op("act", lambda e: e.activation(out=ss[:], in_=ss[:], func=AF.Sqrt, scale=1.0 / D, bias=EPS), reads=[t_ss], writes=[t_ss])
    K.op("dve", lambda e: e.reciprocal(out=ss[:], in_=ss[:]), reads=[t_ss], writes=[t_ss])
    K.op("dve", lambda e: e.scalar_tensor_tensor(out=scr[:], in0=xt, scalar=ss[:, 0:1], in1=gm[:], op0=ALU.mult, op1=ALU.mult),
         reads=[t_x, t_ss, t_gm], writes=[t_scr])
    K.op("dve", lambda e: e.tensor_tensor(out=h[:], in0=scr[:], in1=sh[:], op=ALU.add), reads=[t_scr, t_sh], writes=[t_h])


def transpose_to(K, src, t_src, nchunks, ident, t_id, pst, t_pst, dst, t_dst, eng="act"):
    for c in range(nchunks):
        K.op("pe", lambda e, c=c: e.transpose(out=pst[:, c, :], in_=src[:, c * 128:(c + 1) * 128], identity=ident[:]),
             reads=[t_src, t_id], writes=[t_pst])
    if eng == "act":
        K.op("act", lambda e: e.copy(out=dst, in_=pst[:, 0:nchunks, :]), reads=[t_pst], writes=[t_dst])
    else:
        K.op("dve", lambda e: e.tensor_copy(out=dst, in_=pst[:, 0:nchunks, :]), reads=[t_pst], writes=[t_dst])


def gen_proj_in(K, A, l, xs, t_xs, modscr, t_mod, pscr, t_p):
    ident, t_id = make_ident(K, BF16)
    mods = load_mod(K, modscr, t_mod, A["norm_mix_g"][l, :], 0, 1)
    wb = K.sb([128, 8, IN_COLS], BF16); t_wb = Trk()
    stg = [K.sb([128, 8, 512]) for _ in range(2)]; t_stg = [Trk(), Trk()]
    blocks = [(c0, min(512, IN_COLS - c0)) for c0 in range(0, IN_COLS, 512)]
    for i, (c0, n) in enumerate(blocks):
        s = stg[i % 2]; ts = t_stg[i % 2]
        K.dma(s[:, :, 0:n], A["w_in"][l, :, c0:c0 + n].rearrange("(k p) n -> p k n", p=128), writes=[ts])
        K.op("pool", lambda e, s=s, c0=c0, n=n: e.tensor_copy(out=wb[:, :, c0:c0 + n], in_=s[:, :, 0:n]), reads=[ts], writes=[t_wb])
    xt = [K.sb([128, D]) for _ in range(2)]; t_xt = [Trk(), Trk()]
    scr = K.sb([128, D]); t_scr = Trk()
    ss = K.sb([128, 1]); t_ss = Trk()
    h = K.sb([128, D], BF16); t_h = Trk()
    hT = [K.sb([128, 8, 128], BF16) for _ in range(2)]; t_hT = [Trk(), Trk()]
    pst = K.ps([128, 8, 128], BF16); t_pst = Trk()
    pp = [K.ps([128, 512]) for _ in range(3)]; t_pp = [Trk() for _ in range(3)]
    po = [K.sb([128, IN_COLS]) for _ in range(2)]; t_po = [Trk(), Trk()]
    nmm = [0]

    def body(tt):
        x_ = xt[tt % 2]; tx = t_xt[tt % 2]
        K.dma(x_[:], xs[tt * 128:(tt + 1) * 128, :], reads=[t_xs], writes=[tx])
        norm_mod_tile(K, x_[:], tx, mods[1 if tt < NCTX_T else 0], h, t_h, scr, t_scr, ss, t_ss)
        hT_ = hT[tt % 2]; thT = t_hT[tt % 2]
        transpose_to(K, h, t_h, 8, ident, t_id, pst, t_pst, hT_[:], thT)
        o = po[tt % 2]; to = t_po[tt % 2]
        for i, (c0, n) in enumerate(blocks):
            p_ = pp[nmm[0] % 3]; tp = t_pp[nmm[0] % 3]; nmm[0] += 1
            for k in range(8):
                K.op("pe", lambda e, k=k, p_=p_, c0=c0, n=n: e.matmul(p_[:, 0:n], lhsT=hT_[:, k, :], rhs=wb[:, k, c0:c0 + n], start=(k == 0), stop=(k == 7)),
                     reads=[thT, t_wb], writes=[tp])
            if i % 2 == 0:
                K.op("act", lambda e, p_=p_, c0=c0, n=n: e.copy(out=o[:, c0:c0 + n], in_=p_[:, 0:n]), reads=[tp], writes=[to])
            else:
                K.op("dve", lambda e, p_=p_, c0=c0, n=n: e.tensor_copy(out=o[:, c0:c0 + n], in_=p_[:, 0:n]), reads=[tp], writes=[to])
        K.dma(pscr[tt * 128:(tt + 1) * 128, :], o[:], reads=[to], writes=[t_p])

    yield "proj_in_%d" % l
    for tt in range(NT):
        body(tt)
        yield


def norm_rope_heads(K, src, t_src, H, dh, g, t_g, rope, t_rope, rdim, roff, out_bf, t_out, W):
    sq3 = W["sq"][:, 0:H * dh].rearrange("p (h d) -> p h d", h=H)
    qn = W["qn"][:, 0:H * dh].rearrange("p (h d) -> p h d", h=H)
    ssq = W["ssq"][:, 0:H]
    K.op("dve", lambda e: e.tensor_tensor(out=sq3, in0=src, in1=src, op=ALU.mult), reads=[t_src], writes=[W["t_sq"]])
    K.op("dve", lambda e: e.tensor_reduce(out=ssq, in_=sq3, axis=AX.X, op=ALU.add), reads=[W["t_sq"]], writes=[W["t_ssq"]])
    K.op("act", lambda e: e.activation(out=ssq, in_=ssq, func=AF.Sqrt, scale=1.0 / dh, bias=EPS), reads=[W["t_ssq"]], writes=[W["t_ssq"]])
    K.op("dve", lambda e: e.reciprocal(out=ssq, in_=ssq), reads=[W["t_ssq"]], writes=[W["t_ssq"]])
    K.op("dve", lambda e: e.tensor_tensor(out=qn, in0=src, in1=ssq.unsqueeze(2).broadcast_to([128, H, dh]), op=ALU.mult),
         reads=[t_src, W["t_ssq"]], writes=[W["t_qn"]])
    gb = g.unsqueeze(1).broadcast_to([128, H, dh])
    if rope is None:
        K.op("dve", lambda e: e.tensor_tensor(out=out_bf, in0=qn, in1=gb, op=ALU.mult), reads=[W["t_qn"], t_g], writes=[t_out])
        return
    K.op("dve", lambda e: e.tensor_tensor(out=qn, in0=qn, in1=gb, op=ALU.mult), reads=[W["t_qn"], t_g], writes=[W["t_qn"]])
    hf = rdim // 2
    xa = qn[:, :, roff:roff + hf]; xb = qn[:, :, roff + hf:roff + rdim]
    cos = rope[:, 0:hf].unsqueeze(1).broadcast_to([128, H, hf]); sin = rope[:, hf:rdim].unsqueeze(1).broadcast_to([128, H, hf])
    tm = [W["rt%d" % i][:, 0:H * hf].rearrange("p (h d) -> p h d", h=H) for i in range(4)]
    for i, (a_, b_) in enumerate([(xa, cos), (xb, sin), (xa, sin), (xb, cos)]):
        K.op("pool", lambda e, i=i, a_=a_, b_=b_: e.tensor_tensor(out=tm[i], in0=a_, in1=b_, op=ALU.mult),
             reads=[W["t_qn"], t_rope], writes=[W["t_rt%d" % i]])
    if roff > 0:
        K.op("act", lambda e: e.copy(out=out_bf[:, :, 0:roff], in_=qn[:, :, 0:roff]), reads=[W["t_qn"]], writes=[t_out])
    K.op("dve", lambda e: e.tensor_tensor(out=out_bf[:, :, roff:roff + hf], in0=tm[0], in1=tm[1], op=ALU.subtract),
         reads=[W["t_rt0"], W["t_rt1"]], writes=[t_out])
    K.op("dve", lambda e: e.tensor_tensor(out=out_bf[:, :, roff + hf:roff + rdim], in0=tm[2], in1=tm[3], op=ALU.add),
         reads=[W["t_rt2"], W["t_rt3"]], writes=[t_out])


def make_work(K, n):
    W = {}
    for nm, sh in [("sq", [128, n]), ("qn", [128, n]), ("ssq", [128, 8]), ("rt0", [128, n]), ("rt1", [128, n]), ("rt2", [128, n]), ("rt3", [128, n])]:
        W[nm] = K.sb(sh); W["t_" + nm] = Trk()
    return W


def attn_core(K, QT, t_QT, KT, t_KT, Vd, t_Vd, dq, heads, mixT, t_mix, need_ctx, R):
    SW, t_SW = R["SW"]
    ps_s, t_ps_s = R["ps_s"]
    PT, t_PT = R["PT"]; rdm, t_rdm = R["rdm"]; rds, t_rds = R["rds"]; osb, t_osb = R["osb"]
    blocks = [(256 + i * 512, 512, list(range(NT))) for i in range(4)]
    if need_ctx:
        blocks.append((0, 256, [0, 1]))
    st = R["cnt"]

    def unit(qh, kh, vs, row0, q0, nq, kts):
        half = (row0 % 128) // 64
        hs = slice(half * 64, half * 64 + 64)
        ds_ = slice((1 - half) * 64, (1 - half) * 64 + 64)
        po, tpo = R["ps_o"]; pw, tpw = R["ps_d"]
        for j, kt in enumerate(kts):
            ns = st[0]; st[0] += 1
            pS = ps_s[ns % 2]; tS = t_ps_s[ns % 2]; pt_ = PT[ns % 3]; tpt = t_PT[ns % 3]
            K.op("pe", lambda e, pS=pS, kt=kt: e.matmul(pS[:, 0:nq], lhsT=KT[0:dq, kh, kt * 128:(kt + 1) * 128], rhs=QT[0:dq, qh, q0:q0 + nq], start=True, stop=True),
                 reads=[t_KT, t_QT], writes=[tS])
            K.op("act", lambda e, pS=pS, pt_=pt_: e.activation(out=pt_[:, 0:nq], in_=pS[:, 0:nq], func=AF.Exp), reads=[tS], writes=[tpt])
            K.op("pe", lambda e, kt=kt, pt_=pt_, j=j: e.matmul(po[:, 0:nq], lhsT=Vd[:, kt, vs, :], rhs=pt_[:, 0:nq], start=(j == 0), stop=(j == len(kts) - 1)),
                 reads=[t_Vd, tpt], writes=[tpo])
            if j % 2 == 1:
                yield
        nb = st[1]; st[1] += 1
        ob = osb[nb % 2]; tob = t_osb[nb % 2]
        K.op("dve", lambda e: e.reciprocal(out=rdm[ds_, 0:nq], in_=po[ds_, 0:nq]), reads=[tpo], writes=[t_rdm])
        K.op("pe", lambda e: e.matmul(pw[:, 0:nq], lhsT=SW[:], rhs=rdm[:, 0:nq], start=True, stop=True), reads=[t_SW, t_rdm], writes=[tpw])
        K.op("act", lambda e: e.copy(out=rds[hs, 0:nq], in_=pw[hs, 0:nq]), reads=[tpw], writes=[t_rds])
        K.op("dve", lambda e: e.tensor_tensor(out=ob[hs, 0:nq], in0=po[hs, 0:nq], in1=rds[hs, 0:nq], op=ALU.mult), reads=[tpo, t_rds], writes=[tob])
        K.dma(mixT[row0:row0 + 64, q0:q0 + nq], ob[hs, 0:nq], reads=[tob], writes=[t_mix])

    for (qh, kh, vs, row0) in heads:
        for (q0, nq, kts) in blocks:
            yield from unit(qh, kh, vs, row0, q0, nq, kts)
            yield


def gen_attn(K, A, l, DS, mixT, t_mix, need_ctx, which):
    dq, nqh, nkh = (96, 4, 4) if which == "m" else (64, 6, 2)
    QT = K.sb([dq, nqh, NTOK], BF16); t_QT = Trk()
    KT = K.sb([dq, nkh, NTOK], BF16); t_KT = Trk()
    Vd = K.sb([128, NT, 4, 128], BF16); t_Vd = Trk()
    t_SW = Trk()
    o_ = K.sb([128, 128]); sa = K.sb([128, 128]); SW = K.sb([128, 128])
    K.op("pool", lambda e: e.memset(o_[:], 1.0), writes=[t_SW])
    K.op("pool", lambda e: e.affine_select(out=sa[:], in_=o_[:], pattern=[[1, 128]], compare_op=ALU.is_equal, fill=0.0, base=-64, channel_multiplier=-1), reads=[t_SW], writes=[t_SW])
    K.op("pool", lambda e: e.affine_select(out=SW[:], in_=o_[:], pattern=[[1, 128]], compare_op=ALU.is_equal, fill=0.0, base=64, channel_multiplier=-1), reads=[t_SW], writes=[t_SW])
    K.op("dve", lambda e: e.tensor_tensor(out=SW[:], in0=SW[:], in1=sa[:], op=ALU.add), reads=[t_SW], writes=[t_SW])
    rdm = K.sb([128, 512]); t_rdm = Trk()
    K.op("pool", lambda e: e.memset(rdm[:], 0.0), writes=[t_rdm])
    R = {"SW": (SW, t_SW),
         "ps_s": ([K.ps([128, 512]) for _ in range(2)], [Trk(), Trk()]),
         "ps_o": (K.ps([128, 512]), Trk()), "ps_d": (K.ps([128, 512]), Trk()),
         "PT": ([K.sb([128, 512], BF16) for _ in range(3)], [Trk() for _ in range(3)]),
         "rdm": (rdm, t_rdm), "rds": (K.sb([128, 512]), Trk()),
         "osb": ([K.sb([128, 512], BF16) for _ in range(2)], [Trk(), Trk()]), "cnt": [0, 0]}
    sfx = "m" if which == "m" else "g"
    yield ("attn_mla_%d" if which == "m" else "attn_gqa_%d") % l
    K.dma(QT[:], DS["qt" + sfx], reads=[DS["t_qt" + sfx]], writes=[t_QT])
    K.dma(KT[:], DS["kt" + sfx], reads=[DS["t_kt" + sfx]], writes=[t_KT])
    K.dma(Vd[:], DS["vd" + sfx].rearrange("(t p) h d -> p t h d", p=128), reads=[DS["t_vd" + sfx]], writes=[t_Vd])
    yield
    if which == "m":
        heads = [(h, h, h, h * 64) for h in range(4)]
    else:
        heads = [(g, g // 3, 2 * (g // 3) + (g % 2), 640 + g * 64) for g in range(6)]
    yield from attn_core(K, QT, t_QT, KT, t_KT, Vd, t_Vd, dq, heads, mixT, t_mix, need_ctx, R)


def gen_mla_prep(K, A, l, pscr, t_p, DS):
    qkT = [K.sb([96, 2, 4, 128], BF16) for _ in range(2)]; t_qkT = [Trk(), Trk()]
    Vt = [K.sb([128, 4, 128], BF16) for _ in range(2)]; t_Vt = [Trk(), Trk()]
    for i_ in range(2):
        K.op("pool", lambda e, i_=i_: e.memset(Vt[i_][:], 1.0), writes=[t_Vt[i_]])
    ident, t_id = make_ident(K, BF16)
    W = make_work(K, 384)
    gq = K.sb([128, 256]); t_gq = Trk(); bcast_load(K, gq[:], A["mla_q_norm_g"][l, :], [], t_gq)
    gkv = K.sb([128, 128]); t_gkv = Trk(); bcast_load(K, gkv[:], A["mla_kv_norm_g"][l, :], [], t_gkv)
    gqk = K.sb([128, 2, 96]); t_gqk = Trk()
    K.dma(gqk[:], A["mla_qk_g"][l].partition_broadcast(128), writes=[t_gqk])
    K.op("dve", lambda e: e.tensor_scalar(out=gqk[:, 0, :], in0=gqk[:, 0, :], scalar1=96.0 ** -0.5, scalar2=None, op0=ALU.mult),
         reads=[t_gqk], writes=[t_gqk])
    wq_f = K.sb([128, 2, 384]); t_wqf = Trk()
    K.dma(wq_f[:], A["mla_w_qb"][l].rearrange("(k p) n -> p k n", p=128), writes=[t_wqf])
    wq = K.sb([128, 2, 384], BF16); t_wq = Trk()
    K.op("dve", lambda e: e.tensor_copy(out=wq[:], in_=wq_f[:]), reads=[t_wqf], writes=[t_wq])
    wkv_f = K.sb([128, 512]); t_wkvf = Trk()
    K.dma(wkv_f[:], A["mla_w_kvb"][l], writes=[t_wkvf])
    wkv = K.sb([128, 512], BF16); t_wkv = Trk()
    K.op("dve", lambda e: e.tensor_copy(out=wkv[:], in_=wkv_f[:]), reads=[t_wkvf], writes=[t_wkv])
    pm = [K.sb([128, 416]) for _ in range(2)]; t_pm = [Trk(), Trk()]
    rp = [K.sb([128, 32]) for _ in range(2)]; t_rp = [Trk(), Trk()]
    junk = K.sb([128, 256]); t_junk = Trk()
    ss = K.sb([128, 2]); t_ss = Trk()
    cn = K.sb([128, 384], BF16); t_cn = Trk()
    cT = K.sb([128, 3, 128], BF16); t_cT = Trk()
    pst = K.ps([128, 3, 128], BF16); t_pst = Trk()
    pq = K.ps([128, 384]); t_pq = Trk()
    pkv = K.ps([128, 512]); t_pkv = Trk()
    qs = K.sb([128, 4, 96]); t_qs = Trk()
    ks = K.sb([128, 4, 96]); t_ks = Trk()
    qb = K.sb([128, 4, 96], BF16); t_qb = Trk()
    kb = K.sb([128, 4, 96], BF16); t_kb = Trk()
    ptqk = K.ps([96, 2, 4, 128], BF16); t_ptq = Trk(); t_ptk = t_ptq
    ptq = ptqk[:, 0]; ptk = ptqk[:, 1]
    def body(tt):
        p_ = pm[tt % 2]; tp = t_pm[tt % 2]
        K.dma(p_[:], pscr[tt * 128:(tt + 1) * 128, C_MLA:C_MLA + 416], reads=[t_p], writes=[tp])
        rope = None; trp = None
        if tt >= NCTX_T:
            rope = rp[tt % 2]; trp = t_rp[tt % 2]
            K.dma(rope[:], A["rope_m"][(tt - 2) * 128:(tt - 1) * 128, :], writes=[trp])
        for j, (c0, n, g_, tg) in enumerate([(0, 256, gq, t_gq), (256, 128, gkv, t_gkv)]):
            K.op("act", lambda e, c0=c0, n=n, j=j: e.activation(out=junk[:, 0:n], in_=p_[:, c0:c0 + n], func=AF.Square, accum_out=ss[:, j:j + 1]),
                 reads=[tp], writes=[t_junk, t_ss])
            K.op("act", lambda e, n=n, j=j: e.activation(out=ss[:, j:j + 1], in_=ss[:, j:j + 1], func=AF.Sqrt, scale=1.0 / n, bias=EPS),
                 reads=[t_ss], writes=[t_ss])
            K.op("dve", lambda e, j=j: e.reciprocal(out=ss[:, j:j + 1], in_=ss[:, j:j + 1]), reads=[t_ss], writes=[t_ss])
            K.op("dve", lambda e, c0=c0, n=n, j=j, g_=g_: e.scalar_tensor_tensor(out=cn[:, c0:c0 + n], in0=p_[:, c0:c0 + n], scalar=ss[:, j:j + 1],
                                                                                in1=g_[:], op0=ALU.mult, op1=ALU.mult),
                 reads=[tp, t_ss, tg], writes=[t_cn])
        transpose_to(K, cn, t_cn, 3, ident, t_id, pst, t_pst, cT[:], t_cT)
        yield
        for k in range(2):
            K.op("pe", lambda e, k=k: e.matmul(pq[:], lhsT=cT[:, k, :], rhs=wq[:, k, :], start=(k == 0), stop=(k == 1)),
                 reads=[t_cT, t_wq], writes=[t_pq])
        K.op("pe", lambda e: e.matmul(pkv[:], lhsT=cT[:, 2, :], rhs=wkv[:], start=True, stop=True), reads=[t_cT, t_wkv], writes=[t_pkv])
        yield
        K.op("act", lambda e: e.copy(out=qs[:], in_=pq[:].rearrange("p (h d) -> p h d", h=4)), reads=[t_pq], writes=[t_qs])
        kv3 = pkv[:].rearrange("p (h d) -> p h d", h=4)
        K.op("act", lambda e: e.copy(out=ks[:, :, 0:64], in_=kv3[:, :, 0:64]), reads=[t_pkv], writes=[t_ks])
        K.op("pool", lambda e: e.tensor_copy(out=ks[:, :, 64:96], in_=p_[:, 384:416].unsqueeze(1).broadcast_to([128, 4, 32])),
             reads=[tp], writes=[t_ks])
        Vt_ = Vt[tt % 2]; tVt = t_Vt[tt % 2]
        for h_ in range(4):
            c_ = (h_ % 2) * 64
            if h_ < 2:
                K.op("act", lambda e, h_=h_, c_=c_: e.copy(out=Vt_[:, h_, c_:c_ + 64], in_=kv3[:, h_, 64:128]), reads=[t_pkv], writes=[tVt])
            else:
                K.op("dve", lambda e, h_=h_, c_=c_: e.tensor_copy(out=Vt_[:, h_, c_:c_ + 64], in_=kv3[:, h_, 64:128]), reads=[t_pkv], writes=[tVt])
        K.dma(DS["vdm"][tt * 128:(tt + 1) * 128], Vt_[:], reads=[tVt], writes=[DS["t_vdm"]])
        yield
        norm_rope_heads(K, qs[:], t_qs, 4, 96, gqk[:, 0, :], t_gqk, rope, trp, 32, 64, qb[:], t_qb, W)
        yield
        norm_rope_heads(K, ks[:], t_ks, 4, 96, gqk[:, 1, :], t_gqk, rope, trp, 32, 64, kb[:], t_kb, W)
        yield
        qk_ = qkT[tt % 2]; tqk = t_qkT[tt % 2]
        for i_, (src, ts, pt_) in enumerate([(qb, t_qb, ptq), (kb, t_kb, ptk)]):
            for h in range(4):
                K.op("pe", lambda e, h=h, src=src, pt_=pt_: e.transpose(out=pt_[:, h, :], in_=src[:, h, :], identity=ident[:]),
                     reads=[ts, t_id], writes=[t_ptq])
        K.op("act", lambda e: e.copy(out=qk_[:], in_=ptqk[:]), reads=[t_ptq], writes=[tqk])
        K.dma(DS["qtm"][:, :, tt * 128:(tt + 1) * 128], qk_[:, 0], reads=[tqk], writes=[DS["t_qtm"]])
        K.dma(DS["ktm"][:, :, tt * 128:(tt + 1) * 128], qk_[:, 1], reads=[tqk], writes=[DS["t_ktm"]])
    yield "mla_prep_%d" % l
    for tt in range(NT):
        yield from body(tt)
        yield


def gen_gqa_prep(K, A, l, pscr, t_p, DS):
    qkT = [K.sb([64, 8, 128], BF16) for _ in range(2)]; t_qkT = [Trk(), Trk()]
    Vt = [K.sb([128, 4, 128], BF16) for _ in range(2)]; t_Vt = [Trk(), Trk()]
    for i_ in range(2):
        K.op("pool", lambda e, i_=i_: e.memset(Vt[i_][:], 1.0), writes=[t_Vt[i_]])
    ident, t_id = make_ident(K, BF16)
    W = make_work(K, 384)
    gqk = K.sb([128, 2, 64]); t_gqk = Trk()
    K.dma(gqk[:], A["gqa_qk_g"][l].partition_broadcast(128), writes=[t_gqk])
    K.op("dve", lambda e: e.tensor_scalar(out=gqk[:, 0, :], in0=gqk[:, 0, :], scalar1=64.0 ** -0.5, scalar2=None, op0=ALU.mult),
         reads=[t_gqk], writes=[t_gqk])
    pg = [K.sb([128, 640]) for _ in range(2)]; t_pg = [Trk(), Trk()]
    rp = [K.sb([128, 64]) for _ in range(2)]; t_rp = [Trk(), Trk()]
    qb = K.sb([128, 6, 64], BF16); t_qb = Trk()
    kb = K.sb([128, 2, 64], BF16); t_kb = Trk()
    ptqk = K.ps([64, 8, 128], BF16); t_ptq = Trk(); t_ptk = t_ptq
    ptq = ptqk[:, 0:6]; ptk = ptqk[:, 6:8]
    def body(tt):
        p_ = pg[tt % 2]; tp = t_pg[tt % 2]
        K.dma(p_[:], pscr[tt * 128:(tt + 1) * 128, C_GQA:C_GQA + 640], reads=[t_p], writes=[tp])
        rope = None; trp = None
        if tt >= NCTX_T:
            rope = rp[tt % 2]; trp = t_rp[tt % 2]
            K.dma(rope[:], A["rope_g"][(tt - 2) * 128:(tt - 1) * 128, :], writes=[trp])
        q3 = p_[:, 0:384].rearrange("p (h d) -> p h d", h=6)
        k3 = p_[:, 384:512].rearrange("p (h d) -> p h d", h=2)
        v3 = p_[:, 512:640].rearrange("p (h d) -> p h d", h=2)
        Vt_ = Vt[tt % 2]; tVt = t_Vt[tt % 2]
        for kvh in range(2):
            K.op("act", lambda e, kvh=kvh: e.copy(out=Vt_[:, 2 * kvh, 0:64], in_=v3[:, kvh, :]), reads=[tp], writes=[tVt])
            K.op("dve", lambda e, kvh=kvh: e.tensor_copy(out=Vt_[:, 2 * kvh + 1, 64:128], in_=v3[:, kvh, :]), reads=[tp], writes=[tVt])
        K.dma(DS["vdg"][tt * 128:(tt + 1) * 128], Vt_[:], reads=[tVt], writes=[DS["t_vdg"]])
        yield
        norm_rope_heads(K, q3, tp, 6, 64, gqk[:, 0, :], t_gqk, rope, trp, 64, 0, qb[:], t_qb, W)
        yield
        norm_rope_heads(K, k3, tp, 2, 64, gqk[:, 1, :], t_gqk, rope, trp, 64, 0, kb[:], t_kb, W)
        yield
        qk_ = qkT[tt % 2]; tqk = t_qkT[tt % 2]
        for (src, ts, pt_, H) in [(qb, t_qb, ptq, 6), (kb, t_kb, ptk, 2)]:
            for h in range(H):
                K.op("pe", lambda e, h=h, src=src, pt_=pt_: e.transpose(out=pt_[:, h, :], in_=src[:, h, :], identity=ident[:]),
                     reads=[ts, t_id], writes=[t_ptq])
        K.op("act", lambda e: e.copy(out=qk_[:], in_=ptqk[:]), reads=[t_ptq], writes=[tqk])
        K.dma(DS["qtg"][:, :, tt * 128:(tt + 1) * 128], qk_[:, 0:6], reads=[tqk], writes=[DS["t_qtg"]])
        K.dma(DS["ktg"][:, :, tt * 128:(tt + 1) * 128], qk_[:, 6:8], reads=[tqk], writes=[DS["t_ktg"]])
    yield "gqa_prep_%d" % l
    for tt in range(NT):
        yield from body(tt)
        yield


def gen_rw_prep(K, A, l, pscr, t_p, rwA, t_rwA):
    identf, t_idf = make_ident(K, F32)
    RW = 1568
    mu0 = K.sb([128, RW]); mu1 = K.sb([128, RW]); cm = K.sb([128, RW]); t_mu = Trk()
    K.dma(mu0[:], A["rw_mu"][l, 0, :].partition_broadcast(128), writes=[t_mu])
    K.dma(mu1[:], A["rw_mu"][l, 1, :].partition_broadcast(128), writes=[t_mu])
    K.op("dve", lambda e: e.tensor_tensor(out=cm[:], in0=mu0[:], in1=mu1[:], op=ALU.add), reads=[t_mu], writes=[t_mu])
    K.op("dve", lambda e: e.tensor_scalar(out=cm[:], in0=cm[:], scalar1=-1.0, scalar2=1.0, op0=ALU.mult, op1=ALU.add), reads=[t_mu], writes=[t_mu])
    t_c = Trk()
    w0 = K.sb([128, 2, 384]); K.dma(w0[:], A["rw_w0"][l].partition_broadcast(128), writes=[t_c])
    a0 = K.sb([128, 2, 384]); K.dma(a0[:], A["rw_a0"][l].partition_broadcast(128), writes=[t_c])
    kk_ = K.sb([128, 384]); K.dma(kk_[:], A["rw_k_k"][l, :].partition_broadcast(128), writes=[t_c])
    ka_ = K.sb([128, 384]); K.dma(ka_[:], A["rw_k_a"][l, :].partition_broadcast(128), writes=[t_c])
    rk_ = K.sb([128, 384]); K.dma(rk_[:], A["rw_r_k"][l, :].partition_broadcast(128), writes=[t_c])
    W2 = K.sb([128, 384]); K.dma(W2[:], A["rw_w2"][l].rearrange("d r c -> (d r) c"), writes=[t_c])
    A2 = K.sb([128, 384]); K.dma(A2[:], A["rw_a2"][l].rearrange("d r c -> (d r) c"), writes=[t_c])
    G2 = K.sb([128, 384]); K.dma(G2[:], A["rw_g2"][l, 0:128, :], writes=[t_c])
    G2b = K.sb([32, 384]); K.dma(G2b[:], A["rw_g2"][l, 128:160, :], writes=[t_c])
    pc = [K.sb([128, RW]) for _ in range(2)]; t_pc = [Trk(), Trk()]
    pp = [K.sb([128, RW]) for _ in range(2)]; t_pp = [Trk(), Trk()]
    pn = [K.sb([128, RW]) for _ in range(2)]; t_pn = [Trk(), Trk()]
    xs = K.sb([128, RW]); t_xs = Trk()
    u1 = K.sb([128, RW]); t_u1 = Trk()
    u2 = K.sb([128, RW]); t_u2 = Trk()
    tw = K.sb([128, 416]); t_tw = Trk()
    lT = K.sb([128, 4, 128]); t_lT = Trk()
    pst = K.ps([128, 4, 128]); t_pst = Trk()
    plb = [K.ps([128, 512]) for _ in range(2)]; t_plb = [Trk(), Trk()]
    lwt = [K.sb([128, 384]) for _ in range(2)]; t_lwt = [Trk(), Trk()]
    npl = [0]
    stg = [K.sb([128, 11, 384]) for _ in range(2)]; t_stg = [Trk(), Trk()]
    ad = K.sb([128, 2, 384]); t_ad = Trk()
    tmp = K.sb([128, 384]); t_tmp = Trk()
    tmp2 = K.sb([128, 384]); t_tmp2 = Trk()
    ssq = K.sb([128, 6]); t_ssq = Trk()
    NEG = -float(np.exp(-0.5))

    def body(tt):
        c_ = pc[tt % 2]; tc = t_pc[tt % 2]; p_ = pp[tt % 2]; tp = t_pp[tt % 2]; n_ = pn[tt % 2]; tn = t_pn[tt % 2]
        r0 = tt * 128
        K.dma(c_[:], pscr[r0:r0 + 128, C_RW:C_RW + RW], reads=[t_p], writes=[tc])
        if tt in (0, NCTX_T):
            K.op("pool", lambda e: e.memset(p_[:], 0.0), writes=[tp])
            K.dma(p_[1:128, :], pscr[r0:r0 + 127, C_RW:C_RW + RW], reads=[t_p], writes=[tp])
        else:
            K.dma(p_[:], pscr[r0 - 1:r0 + 127, C_RW:C_RW + RW], reads=[t_p], writes=[tp])
        if tt in (NCTX_T - 1, NT - 1):
            K.op("pool", lambda e: e.memset(n_[:], 0.0), writes=[tn])
            K.dma(n_[0:127, :], pscr[r0 + 1:r0 + 128, C_RW:C_RW + RW], reads=[t_p], writes=[tn])
        else:
            K.dma(n_[:], pscr[r0 + 1:r0 + 129, C_RW:C_RW + RW], reads=[t_p], writes=[tn])
        K.op("dve", lambda e: e.tensor_tensor(out=xs[:], in0=c_[:], in1=cm[:], op=ALU.mult), reads=[tc, t_mu], writes=[t_xs])
        K.op("pool", lambda e: e.tensor_tensor(out=u1[:], in0=p_[:], in1=mu0[:], op=ALU.mult), reads=[tp, t_mu], writes=[t_u1])
        K.op("pool", lambda e: e.tensor_tensor(out=u2[:], in0=n_[:], in1=mu1[:], op=ALU.mult), reads=[tn, t_mu], writes=[t_u2])
        K.op("dve", lambda e: e.tensor_tensor(out=xs[:], in0=xs[:], in1=u1[:], op=ALU.add), reads=[t_xs, t_u1], writes=[t_xs])
        K.op("dve", lambda e: e.tensor_tensor(out=xs[:], in0=xs[:], in1=u2[:], op=ALU.add), reads=[t_xs, t_u2], writes=[t_xs])
        yield
        r = xs[:, 0:384]; k = xs[:, 384:768]; v = xs[:, 768:1152]
        K.op("act", lambda e: e.activation(out=tw[:, 0:128], in_=xs[:, 1152:1280], func=AF.Tanh), reads=[t_xs], writes=[t_tw])
        K.op("act", lambda e: e.copy(out=tw[:, 128:256], in_=xs[:, 1280:1408]), reads=[t_xs], writes=[t_tw])
        K.op("act", lambda e: e.activation(out=tw[:, 256:416], in_=xs[:, 1408:1568], func=AF.Sigmoid), reads=[t_xs], writes=[t_tw])
        for c in range(3):
            K.op("pe", lambda e, c=c: e.transpose(out=pst[:, c, :], in_=tw[:, c * 128:(c + 1) * 128], identity=identf[:]),
                 reads=[t_tw, t_idf], writes=[t_pst])
        K.op("pe", lambda e: e.transpose(out=pst[0:32, 3, :], in_=tw[:, 384:416], identity=identf[:]), reads=[t_tw, t_idf], writes=[t_pst])
        K.op("act", lambda e: e.copy(out=lT[:, 0:3, :], in_=pst[:, 0:3, :]), reads=[t_pst], writes=[t_lT])
        K.op("act", lambda e: e.copy(out=lT[0:32, 3, :], in_=pst[0:32, 3, :]), reads=[t_pst], writes=[t_lT])
        yield
        s_ = stg[tt % 2]; ts = t_stg[tt % 2]

        def nextpl():
            i_ = npl[0] % 2
            npl[0] += 1
            return plb[i_][:, 0:384], t_plb[i_]

        for d in range(2):
            p_w, t_w = nextpl()
            K.op("pe", lambda e, d=d, p_w=p_w: e.matmul(p_w, lhsT=lT[d * 64:(d + 1) * 64, 0, :], rhs=W2[d * 64:(d + 1) * 64, :], start=True, stop=True),
                 reads=[t_lT, t_c], writes=[t_w])
            K.op("dve", lambda e, d=d, p_w=p_w: e.tensor_tensor(out=lwt[d][:], in0=p_w, in1=w0[:, d, :], op=ALU.add), reads=[t_w, t_c], writes=[t_lwt[d]])
            K.op("act", lambda e, d=d: e.activation(out=lwt[d][:], in_=lwt[d][:], func=AF.Sigmoid), reads=[t_lwt[d]], writes=[t_lwt[d]])
            K.op("act", lambda e, d=d: e.mul(out=s_[:, 5 + 3 * d, :], in_=lwt[d][:], mul=NEG), reads=[t_lwt[d]], writes=[ts])
            p_a, t_a_ = nextpl()
            K.op("pe", lambda e, d=d, p_a=p_a: e.matmul(p_a, lhsT=lT[d * 64:(d + 1) * 64, 1, :], rhs=A2[d * 64:(d + 1) * 64, :], start=True, stop=True),
                 reads=[t_lT, t_c], writes=[t_a_])
            K.op("dve", lambda e, d=d, p_a=p_a: e.tensor_tensor(out=ad[:, d, :], in0=p_a, in1=a0[:, d, :], op=ALU.add), reads=[t_a_, t_c], writes=[t_ad])
            K.op("act", lambda e, d=d: e.activation(out=ad[:, d, :], in_=ad[:, d, :], func=AF.Sigmoid), reads=[t_ad], writes=[t_ad])
        p_g, t_g_ = nextpl()
        K.op("pe", lambda e: e.matmul(p_g, lhsT=lT[:, 2, :], rhs=G2[:], start=True, stop=False), reads=[t_lT, t_c], writes=[t_g_])
        K.op("pe", lambda e: e.matmul(p_g, lhsT=lT[0:32, 3, :], rhs=G2b[:], start=False, stop=True), reads=[t_lT, t_c], writes=[t_g_])
        K.op("act", lambda e: e.copy(out=s_[:, 3, :], in_=p_g), reads=[t_g_], writes=[ts])
        K.op("act", lambda e: e.copy(out=s_[:, 0, :], in_=r), reads=[t_xs], writes=[ts])
        K.op("act", lambda e: e.copy(out=s_[:, 1, :], in_=v), reads=[t_xs], writes=[ts])
        yield
        K.op("dve", lambda e: e.tensor_tensor(out=tmp[:], in0=k, in1=kk_[:], op=ALU.mult), reads=[t_xs, t_c], writes=[t_tmp])
        K.op("dve", lambda e: e.tensor_tensor(out=tmp2[:], in0=tmp[:], in1=tmp[:], op=ALU.mult), reads=[t_tmp], writes=[t_tmp2])
        K.op("dve", lambda e: e.tensor_reduce(out=ssq[:], in_=tmp2[:].rearrange("p (h n) -> p h n", h=6), axis=AX.X, op=ALU.add),
             reads=[t_tmp2], writes=[t_ssq])
        K.op("act", lambda e: e.activation(out=ssq[:], in_=ssq[:], func=AF.Sqrt, scale=1.0, bias=1e-12), reads=[t_ssq], writes=[t_ssq])
        K.op("dve", lambda e: e.reciprocal(out=ssq[:], in_=ssq[:]), reads=[t_ssq], writes=[t_ssq])
        K.op("dve", lambda e: e.tensor_tensor(out=s_[:, 2, :].rearrange("p (h n) -> p h n", h=6), in0=tmp[:].rearrange("p (h n) -> p h n", h=6),
                                              in1=ssq[:].unsqueeze(2).broadcast_to([128, 6, 64]), op=ALU.mult),
             reads=[t_tmp, t_ssq], writes=[ts])
        yield
        for d in range(2):
            K.op("dve", lambda e, d=d: e.scalar_tensor_tensor(out=tmp2[:], in0=ad[:, d, :], scalar=1.0, in1=ka_[:], op0=ALU.subtract, op1=ALU.mult),
                 reads=[t_ad, t_c], writes=[t_tmp2])
            K.op("dve", lambda e, d=d: e.scalar_tensor_tensor(out=s_[:, 6 + 3 * d, :], in0=tmp2[:], scalar=1.0, in1=k, op0=ALU.add, op1=ALU.mult),
                 reads=[t_tmp2, t_xs], writes=[ts])
            K.op("dve", lambda e, d=d: e.tensor_tensor(out=s_[:, 7 + 3 * d, :], in0=s_[:, 2, :], in1=ad[:, d, :], op=ALU.mult), reads=[ts, t_ad], writes=[ts])
            yield
        K.op("dve", lambda e: e.tensor_tensor(out=tmp[:], in0=s_[:, 6, :], in1=s_[:, 9, :], op=ALU.add), reads=[ts], writes=[t_tmp])
        K.op("dve", lambda e: e.tensor_tensor(out=tmp[:], in0=tmp[:], in1=rk_[:], op=ALU.mult), reads=[t_tmp, t_c], writes=[t_tmp])
        K.op("dve", lambda e: e.tensor_tensor(out=tmp[:], in0=tmp[:], in1=r, op=ALU.mult), reads=[t_tmp, t_xs], writes=[t_tmp])
        K.op("dve", lambda e: e.tensor_reduce(out=ssq[:], in_=tmp[:].rearrange("p (h n) -> p h n", h=6), axis=AX.X, op=ALU.add),
             reads=[t_tmp], writes=[t_ssq])
        K.op("dve", lambda e: e.tensor_tensor(out=s_[:, 4, :].rearrange("p (h n) -> p h n", h=6), in0=v.rearrange("p (h n) -> p h n", h=6),
                                              in1=ssq[:].unsqueeze(2).broadcast_to([128, 6, 64]), op=ALU.mult),
             reads=[t_xs, t_ssq], writes=[ts])
        K.dma(rwA[r0:r0 + 128, :, :], s_[:], reads=[ts], writes=[t_rwA])

    yield "rw_prep_%d" % l
    for tt in range(NT):
        yield from body(tt)
        yield


RW_FAST = [True]


class Banks:
    def __init__(self, K, n_rot=7):
        nb = (n_rot + 2) // 2
        self.t = [K.ps([128, 1024]) for _ in range(nb)]
        self.trk = [Trk() for _ in range(2 * nb)]
        self.i1 = 0
        self.i2 = 0
        self.n_rot = n_rot

    def bank(self, b):
        return self.t[b // 2][:, (b % 2) * 512:(b % 2) * 512 + 512], self.trk[b]

    def single(self):
        b = self.i1 % self.n_rot
        self.i1 += 1
        return self.bank(b)

    def double(self):
        p = self.i2 % 3
        self.i2 += 1
        return self.t[p], [self.trk[2 * p], self.trk[2 * p + 1]]


F32R_ = mybir.dt.float32r


def rw_consts(K, A, l):
    identf, t_idf = make_ident(K, F32)
    t_c = Trk()
    ones3 = K.sb([128, 3, 128])
    K.op("pool", lambda e: e.memset(ones3[:], 1.0), writes=[t_c])
    m_inc3 = K.sb([128, 3, 128]); m_str3 = K.sb([128, 3, 128]); id3 = K.sb([128, 3, 128]); Jm = K.sb([128, 128])
    blk = K.sb([128, 128]); cind = K.sb([128, 2])
    pat3 = [[0, 3], [1, 128]]
    K.op("pool", lambda e: e.affine_select(out=m_inc3[:], in_=ones3[:], pattern=pat3, compare_op=ALU.is_ge, fill=0.0, base=0, channel_multiplier=-1), reads=[t_c], writes=[t_c])
    K.op("pool", lambda e: e.affine_select(out=m_str3[:], in_=ones3[:], pattern=pat3, compare_op=ALU.is_ge, fill=0.0, base=-1, channel_multiplier=-1), reads=[t_c], writes=[t_c])
    K.op("pool", lambda e: e.affine_select(out=id3[:], in_=ones3[:], pattern=pat3, compare_op=ALU.is_equal, fill=0.0, base=0, channel_multiplier=-1), reads=[t_c], writes=[t_c])
    K.op("pool", lambda e: e.affine_select(out=Jm[:], in_=ones3[:, 0, :], pattern=[[1, 128]], compare_op=ALU.is_equal, fill=0.0, base=-127, channel_multiplier=1), reads=[t_c], writes=[t_c])
    K.op("pool", lambda e: e.memset(m_inc3[0:64, :, 64:128], 0.0), reads=[t_c], writes=[t_c])
    K.op("pool", lambda e: e.memset(m_str3[0:64, :, 64:128], 0.0), reads=[t_c], writes=[t_c])
    K.op("pool", lambda e: e.memset(blk[:], 0.0), writes=[t_c])
    K.op("pool", lambda e: e.memset(blk[0:64, 0:64], 1.0), reads=[t_c], writes=[t_c])
    K.op("pool", lambda e: e.memset(blk[64:128, 64:128], 1.0), reads=[t_c], writes=[t_c])
    K.op("pool", lambda e: e.memset(cind[:], 0.0), writes=[t_c])
    K.op("pool", lambda e: e.memset(cind[0:64, 0:1], 1.0), reads=[t_c], writes=[t_c])
    K.op("pool", lambda e: e.memset(cind[64:128, 1:2], 1.0), reads=[t_c], writes=[t_c])
    tri = m_inc3[:, 0, :]

    return dict(identf=identf, t_idf=t_idf, t_c=t_c, m_inc3=m_inc3, m_str3=m_str3, id3=id3, Jm=Jm, blk=blk, cind=cind, tri=tri)


def gen_rw_scan(K, A, l, C, d, rwA, t_rwA, ydst, t_yd, n_rot=3):
    identf = C["identf"]; t_idf = C["t_idf"]; t_c = C["t_c"]; m_inc3 = C["m_inc3"]; m_str3 = C["m_str3"]; id3 = C["id3"]
    Jm = C["Jm"]; blk = C["blk"]; cind = C["cind"]; tri = C["tri"]
    B = Banks(K, n_rot=n_rot)
    psY1, t_psY1 = B.bank(n_rot)
    a_in = [K.sb([128, 6, 384])]; t_a = [Trk()]
    if d == 1:
        af = K.sb([128, 6, 384]); t_af = Trk()
    names = ["cum", "d0", "d1", "e0", "em", "ep", "eh", "At", "Bt", "Kt", "Rt", "Bh", "Kh", "X1", "Z", "U", "Y1", "Y", "Vr"]
    S_ = {n: K.sb([128, 384]) for n in names}; T_ = {n: Trk() for n in names}
    fT = {n: K.sb([64, 6, 128]) for n in ["AtT", "BtT", "KtT", "RtT", "WmT"]}; t_fT = {n: Trk() for n in fT}
    gr = {n: K.sb([128, 6, 128]) for n in ["N", "LakT", "RbT", "RkT"]}; t_gr = {n: Trk() for n in gr}
    Nk = [K.sb([128, 6, 128]) for _ in range(2)]; t_Nk = [Trk(), Trk()]
    NkT = [K.sb([128, 6, 128]) for _ in range(2)]; t_NkT = [Trk(), Trk()]
    TT = K.sb([128, 6, 128]); t_TT = Trk()
    gamT = K.sb([64, 6, 2]); t_gam = Trk()
    M = K.sb([64, 6, 64]); t_M = Trk()
    Mt = K.sb([64, 6, 64]); t_Mt = Trk()
    cnt = [0]

    def RD(ap, r):
        return ap.bitcast(F32R_) if (r and RW_FAST[0]) else ap

    def ev(out, in_, reads, writes, r=False):
        out = RD(out, r)
        cnt[0] += 1
        if cnt[0] % 2:
            K.op("act", lambda e: e.copy(out=out, in_=in_), reads=reads, writes=writes)
        else:
            K.op("dve", lambda e: e.tensor_copy(out=out, in_=in_), reads=reads, writes=writes)

    def mm(out, lhsT, rhs, reads, writes, start=True, stop=True, fast=None):
        if fast is None:
            fast = RW_FAST[0]
        if fast and out.start_partition() == 0 and lhsT.start_partition() == 0:
            lhsT = lhsT.bitcast(mybir.dt.float32r); rhs = rhs.bitcast(mybir.dt.float32r)
        K.op("pe", lambda e: e.matmul(out, lhsT=lhsT, rhs=rhs, start=start, stop=stop), reads=reads, writes=writes)

    def tt_op(eng, out, in0, in1, op, reads, writes, r=False):
        out = RD(out, r)
        K.op(eng, lambda e: e.tensor_tensor(out=out, in0=in0, in1=in1, op=op), reads=reads, writes=writes)

    def h3(ap):
        return ap.rearrange("p (h n) -> p h n", n=128)

    def tile(tt, d, idx):
        r0 = tt * 128
        a_ = a_in[0]; ta = t_a[0]
        K.dma(a_[:, 0:3, :], rwA[r0:r0 + 128, 0:3, :], reads=[t_rwA], writes=[ta])
        K.dma(a_[:, 3:6, :], rwA[r0:r0 + 128, 5 + 3 * d:8 + 3 * d, :], reads=[t_rwA], writes=[ta])
        if d == 1:
            for i in range(6):
                pb, tb = B.single()
                mm(pb[:, 0:384], Jm[:], a_[:, i, :], [t_c, ta], [tb], fast=False)
                ev(af[:, i, :], pb[:, 0:384], [tb], [t_af])
            X = af; tX = t_af
        else:
            X = a_; tX = ta
        R_, V_, KK_, LW_, KD_, BD_ = [X[:, i, :] for i in range(6)]
        K.op("act", lambda e: e.copy(out=RD(S_["Vr"][:], True), in_=V_), reads=[tX], writes=[T_["Vr"]])
        Vh = lambda h: S_["Vr"][:, h * 64:(h + 1) * 64]
        pcum, tcum = B.single(); ptot, ttot = B.single(); pgT, tgT = B.single()
        mm(pcum[:, 0:384], tri, LW_, [t_c, tX], [tcum], fast=False)
        mm(ptot[:, 0:384], blk[:], LW_, [t_c, tX], [ttot], fast=False)
        gview = pgT[0:64, 0:12].rearrange("p (h c) -> p h c", c=2)
        for h in range(6):
            mm(gview[:, h, :], X[:, 3, h * 64:(h + 1) * 64], cind[:], [tX, t_c], [tgT], fast=False)
        K.op("act", lambda e: e.activation(out=gamT[:], in_=gview, func=AF.Exp), reads=[tgT], writes=[t_gam])
        K.op("act", lambda e: e.copy(out=S_["cum"][:], in_=pcum[:, 0:384]), reads=[tcum], writes=[T_["cum"]])
        tt_op("dve", S_["d0"][:], S_["cum"][:], LW_, ALU.subtract, [T_["cum"], tX], [T_["d0"]])
        tt_op("dve", S_["d1"][:], ptot[:, 0:384], S_["cum"][:], ALU.subtract, [ttot, T_["cum"]], [T_["d1"]])
        K.op("act", lambda e: e.activation(out=S_["e0"][:], in_=S_["d0"][:], func=AF.Exp), reads=[T_["d0"]], writes=[T_["e0"]])
        K.op("act", lambda e: e.activation(out=S_["em"][:], in_=S_["cum"][:], func=AF.Exp, scale=-1.0), reads=[T_["cum"]], writes=[T_["em"]])
        K.op("act", lambda e: e.activation(out=S_["ep"][:], in_=S_["cum"][:], func=AF.Exp), reads=[T_["cum"]], writes=[T_["ep"]])
        K.op("act", lambda e: e.activation(out=S_["eh"][:], in_=S_["d1"][:], func=AF.Exp), reads=[T_["d1"]], writes=[T_["eh"]])
        yield
        K.op("dve", lambda e: e.scalar_tensor_tensor(out=RD(S_["At"][:], True), in0=KK_, scalar=-1.0, in1=S_["e0"][:], op0=ALU.mult, op1=ALU.mult),
             reads=[tX, T_["e0"]], writes=[T_["At"]])
        tt_op("pool", S_["Bt"][:], BD_, S_["em"][:], ALU.mult, [tX, T_["em"]], [T_["Bt"]], r=True)
        tt_op("dve", S_["Kt"][:], KD_, S_["em"][:], ALU.mult, [tX, T_["em"]], [T_["Kt"]], r=True)
        tt_op("pool", S_["Rt"][:], R_, S_["ep"][:], ALU.mult, [tX, T_["ep"]], [T_["Rt"]], r=True)
        tt_op("dve", S_["Bh"][:], BD_, S_["eh"][:], ALU.mult, [tX, T_["eh"]], [T_["Bh"]], r=True)
        tt_op("pool", S_["Kh"][:], KD_, S_["eh"][:], ALU.mult, [tX, T_["eh"]], [T_["Kh"]], r=True)
        yield
        for src, dst in [("At", "AtT"), ("Bt", "BtT"), ("Kt", "KtT"), ("Rt", "RtT")]:
            for g in range(2):
                pb, tb = B.single()
                v = pb[0:64, 0:384].rearrange("p (h n) -> p h n", n=128)
                for j in range(3):
                    h = g * 3 + j
                    K.op("pe", lambda e, h=h, j=j, v=v, src=src: e.transpose(out=v[:, j, :], in_=S_[src][:, h * 64:(h + 1) * 64], identity=identf[:]),
                         reads=[T_[src], t_idf], writes=[tb])
                ev(fT[dst][:, g * 3:g * 3 + 3, :], v, [tb], [t_fT[dst]], r=True)
                yield
        for nm, a, b, msk in [("N", "BtT", "AtT", m_str3), ("LakT", "KtT", "AtT", m_str3), ("RbT", "BtT", "RtT", m_inc3), ("RkT", "KtT", "RtT", m_inc3)]:
            for g in range(2):
                pb, tb = B.single()
                v = h3(pb[:, 0:384])
                for j in range(3):
                    h = g * 3 + j
                    mm(v[:, j, :], fT[a][:, h, :], fT[b][:, h, :], [t_fT[a], t_fT[b]], [tb])
                tt_op("dve", gr[nm][:, g * 3:g * 3 + 3, :], v, msk[:], ALU.mult, [tb, t_c], [t_gr[nm]], r=True)
                yield
        cur = 0
        K.op("pool", lambda e: e.tensor_copy(out=RD(Nk[0][:], True), in_=gr["N"][:]), reads=[t_gr["N"]], writes=[t_Nk[0]])
        for g in range(2):
            pb, tb = B.single()
            v = h3(pb[:, 0:384])
            for j in range(3):
                h = g * 3 + j
                K.op("pe", lambda e, h=h, j=j, v=v: e.transpose(out=v[:, j, :], in_=gr["N"][:, h, :], identity=identf[:]),
                     reads=[t_gr["N"], t_idf], writes=[tb])
            ev(NkT[0][:, g * 3:g * 3 + 3, :], v, [tb], [t_NkT[0]], r=True)
            tt_op("dve", TT[:, g * 3:g * 3 + 3, :], gr["N"][:, g * 3:g * 3 + 3, :], id3[:], ALU.add, [t_gr["N"], t_c], [t_TT], r=True)
            yield
        for lev in range(5):
            nx = 1 - cur
            last = lev == 4
            for g in range(2):
                pb, tb = B.single()
                vb = h3(pb[:, 0:384])
                for j in range(3):
                    h = g * 3 + j
                    mm(vb[:, j, :], Nk[cur][:, h, :], NkT[cur][:, h, :], [t_NkT[cur], t_Nk[cur]], [tb])
                ev(NkT[nx][:, g * 3:g * 3 + 3, :], vb, [tb], [t_NkT[nx]], r=True)
                yield
                if not last:
                    pa, ta_ = B.single()
                    va = h3(pa[:, 0:384])
                    for j in range(3):
                        h = g * 3 + j
                        mm(va[:, j, :], NkT[cur][:, h, :], Nk[cur][:, h, :], [t_NkT[cur], t_Nk[cur]], [ta_])
                    ev(Nk[nx][:, g * 3:g * 3 + 3, :], va, [ta_], [t_Nk[nx]], r=True)
                    yield
            for g in range(2):
                pc_, tc_ = B.single()
                vc = h3(pc_[:, 0:384])
                for j in range(3):
                    h = g * 3 + j
                    mm(vc[:, j, :], NkT[nx][:, h, :], TT[:, h, :], [t_NkT[nx], t_TT], [tc_])
                tt_op("dve", TT[:, g * 3:g * 3 + 3, :], vc, TT[:, g * 3:g * 3 + 3, :], ALU.add, [tc_, t_TT], [t_TT], r=True)
                yield
            cur = nx
        pb, tb = B.single()
        for h in range(6):
            mm(pb[:, h * 64:(h + 1) * 64], gr["LakT"][:, h, :], Vh(h), [t_gr["LakT"], T_["Vr"]], [tb])
        ev(S_["X1"][:], pb[:, 0:384], [tb], [T_["X1"]], r=True)
        yield
        pb, tb = B.single()
        for h in range(6):
            mm(pb[:, h * 64:(h + 1) * 64], TT[:, h, :], S_["X1"][:, h * 64:(h + 1) * 64], [t_TT, T_["X1"]], [tb])
        ev(S_["Z"][:], pb[:, 0:384], [tb], [T_["Z"]])
        yield
        for g in range(2):
            pb, tb = B.single()
            v = pb[0:64, 0:384].rearrange("p (h n) -> p h n", n=128)
            for j in range(3):
                h = g * 3 + j
                mm(v[:, j, :], S_["At"][:, h * 64:(h + 1) * 64], TT[:, h, :], [T_["At"], t_TT], [tb])
            ev(fT["WmT"][:, g * 3:g * 3 + 3, :], v, [tb], [t_fT["WmT"]], r=True)
            yield
        for c in range(2):
            cs = slice(c * 64, (c + 1) * 64)
            pu, tu = B.single()
            for h in range(6):
                mm(pu[cs, h * 64:(h + 1) * 64], fT["WmT"][:, h, cs], M[:, h, :], [t_fT["WmT"], t_M], [tu])
                mm(psY1[cs, h * 64:(h + 1) * 64], fT["RtT"][:, h, cs], M[:, h, :], [t_fT["RtT"], t_M], [t_psY1])
            tt_op("dve", S_["U"][cs, :], pu[cs, 0:384], S_["Z"][cs, :], ALU.add, [tu, T_["Z"]], [T_["U"]], r=True)
            yield
            pm_, tm_ = B.single()
            mv = pm_[0:64, 0:384].rearrange("p (h n) -> p h n", n=64)
            for h in range(6):
                mm(mv[:, h, :], S_["Bh"][cs, h * 64:(h + 1) * 64], S_["U"][cs, h * 64:(h + 1) * 64], [T_["Bh"], T_["U"]], [tm_], start=True, stop=False)
                mm(mv[:, h, :], S_["Kh"][cs, h * 64:(h + 1) * 64], S_["Vr"][cs, h * 64:(h + 1) * 64], [T_["Kh"], T_["Vr"]], [tm_], start=False, stop=True)
            tt_op("dve", Mt[:], M[:], gamT[:, :, c:c + 1].broadcast_to([64, 6, 64]), ALU.mult, [t_M, t_gam], [t_Mt])
            tt_op("dve", M[:], mv, Mt[:], ALU.add, [tm_, t_Mt], [t_M], r=True)
            yield
        py, ty = B.single()
        for h in range(6):
            mm(py[:, h * 64:(h + 1) * 64], gr["RbT"][:, h, :], S_["U"][:, h * 64:(h + 1) * 64], [t_gr["RbT"], T_["U"]], [ty], start=True, stop=False)
            mm(py[:, h * 64:(h + 1) * 64], gr["RkT"][:, h, :], Vh(h), [t_gr["RkT"], T_["Vr"]], [ty], start=False, stop=True)
        K.op("act", lambda e: e.copy(out=S_["Y1"][:], in_=psY1[:, 0:384]), reads=[t_psY1], writes=[T_["Y1"]])
        tt_op("dve", S_["Y"][:], py[:, 0:384], S_["Y1"][:], ALU.add, [ty, T_["Y1"]], [T_["Y"]])
        yield
        if d == 0:
            K.dma(ydst[r0:r0 + 128, :], S_["Y"][:], reads=[T_["Y"]], writes=[t_yd])
        else:
            pb, tb = B.single()
            mm(pb[:, 0:384], Jm[:], S_["Y"][:], [t_c, T_["Y"]], [tb], fast=False)
            ev(S_["Y1"][:], pb[:, 0:384], [tb], [T_["Y1"]])
            K.dma(ydst[r0:r0 + 128, :], S_["Y1"][:], reads=[T_["Y1"]], writes=[t_yd])

    yield "rw_scan%d_%d" % (d, l)
    K.op("pool", lambda e: e.memset(M[:], 0.0), reads=[t_M], writes=[t_M])
    order = list(range(NT)) if d == 0 else [1, 0] + list(range(NT - 1, NCTX_T - 1, -1))
    for idx, tt in enumerate(order):
        yield from tile(tt, d, idx)
        yield


def gen_rw_readout(K, A, l, rwA, t_rwA, y0, t_y0, y1, t_y1, mixT, t_mix):
    identb, t_idb = make_ident(K, BF16)
    t_c = Trk()
    lng = K.sb([128, 384]); lnb = K.sb([128, 384])
    K.dma(lng[:], A["rw_ln_g"][l, :].partition_broadcast(128), writes=[t_c])
    K.dma(lnb[:], A["rw_ln_b"][l, :].partition_broadcast(128), writes=[t_c])
    NBUF = 2
    Y0 = [K.sb([128, 384]) for _ in range(NBUF)]; tY0 = [Trk() for _ in range(NBUF)]
    Y1_ = [K.sb([128, 384]) for _ in range(NBUF)]; tY1 = [Trk() for _ in range(NBUF)]
    GB = [K.sb([128, 2, 384]) for _ in range(NBUF)]; tGB = [Trk() for _ in range(NBUF)]
    W1 = [K.sb([128, 384]) for _ in range(NBUF)]; tW1 = [Trk() for _ in range(NBUF)]
    W2 = [K.sb([128, 384]) for _ in range(NBUF)]; tW2 = [Trk() for _ in range(NBUF)]
    ST = [K.sb([128, 6]) for _ in range(NBUF)]; tST = [Trk() for _ in range(NBUF)]
    OB = [K.sb([128, 384], BF16) for _ in range(NBUF)]; tOB = [Trk() for _ in range(NBUF)]
    OBT = [K.sb([128, 3, 128], BF16) for _ in range(NBUF)]; tOBT = [Trk() for _ in range(NBUF)]
    PS = [K.ps([128, 3, 128], BF16) for _ in range(2)]; tPS = [Trk(), Trk()]

    def tt_op(eng, out, in0, in1, op, reads, writes):
        K.op(eng, lambda e: e.tensor_tensor(out=out, in0=in0, in1=in1, op=op), reads=reads, writes=writes)

    def body(tt):
        n = tt % NBUF
        r0 = tt * 128
        y = Y0[n]; ty_ = tY0[n]; gb = GB[n]; t_gb = tGB[n]; w1 = W1[n]; w2 = W2[n]; st6 = ST[n]; t_st6 = tST[n]
        ob = OB[n]; t_ob = tOB[n]; obT = OBT[n]; t_obT = tOBT[n]
        T_ = {"w1": tW1[n], "w2": tW2[n]}
        K.dma(y[:], y0[r0:r0 + 128, :], reads=[t_y0], writes=[ty_])
        K.dma(Y1_[n][:], y1[r0:r0 + 128, :], reads=[t_y1], writes=[tY1[n]])
        K.dma(gb[:], rwA[r0:r0 + 128, 3:5, :], reads=[t_rwA], writes=[t_gb])
        tt_op("pool", y[:], y[:], Y1_[n][:], ALU.add, [ty_, tY1[n]], [ty_])
        y3 = y[:].rearrange("p (h n) -> p h n", h=6)
        w13 = w1[:].rearrange("p (h n) -> p h n", h=6)
        w23 = w2[:].rearrange("p (h n) -> p h n", h=6)
        K.op("dve", lambda e: e.tensor_reduce(out=st6[:], in_=y3, axis=AX.X, op=ALU.add), reads=[ty_], writes=[t_st6])
        K.op("dve", lambda e: e.tensor_scalar(out=st6[:], in0=st6[:], scalar1=-1.0 / 64, scalar2=None, op0=ALU.mult), reads=[t_st6], writes=[t_st6])
        tt_op("dve", w13, y3, st6[:].unsqueeze(2).broadcast_to([128, 6, 64]), ALU.add, [ty_, t_st6], [T_["w1"]])
        tt_op("pool", w2[:], w1[:], w1[:], ALU.mult, [T_["w1"]], [T_["w2"]])
        K.op("dve", lambda e: e.tensor_reduce(out=st6[:], in_=w23, axis=AX.X, op=ALU.add), reads=[T_["w2"]], writes=[t_st6])
        K.op("act", lambda e: e.activation(out=st6[:], in_=st6[:], func=AF.Sqrt, scale=1.0 / 64, bias=64e-5), reads=[t_st6], writes=[t_st6])
        K.op("dve", lambda e: e.reciprocal(out=st6[:], in_=st6[:]), reads=[t_st6], writes=[t_st6])
        tt_op("dve", w13, w13, st6[:].unsqueeze(2).broadcast_to([128, 6, 64]), ALU.mult, [T_["w1"], t_st6], [T_["w1"]])
        tt_op("pool", w1[:], w1[:], lng[:], ALU.mult, [T_["w1"], t_c], [T_["w1"]])
        tt_op("pool", w1[:], w1[:], lnb[:], ALU.add, [T_["w1"], t_c], [T_["w1"]])
        tt_op("dve", w1[:], w1[:], gb[:, 1, :], ALU.add, [T_["w1"], t_gb], [T_["w1"]])
        tt_op("dve", ob[:], w1[:], gb[:, 0, :], ALU.mult, [T_["w1"], t_gb], [t_ob])
        vb = PS[tt % 2]; tb = tPS[tt % 2]
        for c in range(3):
            K.op("pe", lambda e, c=c: e.transpose(out=vb[:, c, :], in_=ob[:, c * 128:(c + 1) * 128], identity=identb[:]), reads=[t_ob, t_idb], writes=[tb])
        K.op("act", lambda e: e.copy(out=obT[:], in_=vb[:]), reads=[tb], writes=[t_obT])
        K.dma(mixT[256:640, r0:r0 + 128].rearrange("(c p) n -> p c n", p=128), obT[:], reads=[t_obT], writes=[t_mix])

    yield "rw_readout_%d" % l
    for tt in range(NT):
        body(tt)
        yield


def phase_wout(K, A, l, xs, t_xs, modscr, t_mod, mixT, t_mix, need_ctx):
    K.push()
    wb = K.sb([128, 8, D], BF16); t_wb = Trk()
    stg = [K.sb([128, 8, 512]) for _ in range(2)]; t_stg = [Trk(), Trk()]
    for i in range(2):
        K.dma(stg[i][:], A["w_out"][l, :, i * 512:(i + 1) * 512].rearrange("(k p) n -> p k n", p=128), writes=[t_stg[i]])
        K.op("pool", lambda e, i=i: e.tensor_copy(out=wb[:, :, i * 512:(i + 1) * 512], in_=stg[i][:]), reads=[t_stg[i]], writes=[t_wb])
    m2 = [K.sb([128, D]) for _ in range(2)]; t_m2 = Trk()
    for which in range(2):
        K.dma(m2[which][:], modscr[which, 2 * D:3 * D].partition_broadcast(128), reads=[t_mod], writes=[t_m2])
    mt = [K.sb([128, 8, 128], BF16) for _ in range(2)]; t_mt = [Trk(), Trk()]
    xt = [K.sb([128, D]) for _ in range(2)]; t_xt = [Trk(), Trk()]
    tmp = K.sb([128, D]); t_tmp = Trk()
    ps = [K.ps([128, 512]) for _ in range(4)]; t_ps = [Trk() for _ in range(4)]

    def body(tt, n):
        m_ = mt[n % 2]; tm = t_mt[n % 2]; x_ = xt[n % 2]; tx = t_xt[n % 2]
        K.dma(m_[:], mixT[:, tt * 128:(tt + 1) * 128].rearrange("(c p) n -> p c n", p=128), reads=[t_mix], writes=[tm])
        K.dma(x_[:], xs[tt * 128:(tt + 1) * 128, :], reads=[t_xs], writes=[tx])
        mm_ = m2[1 if tt < NCTX_T else 0]
        for hf in range(2):
            p_ = ps[(2 * n + hf) % 4]; tp = t_ps[(2 * n + hf) % 4]
            for k in range(8):
                K.op("pe", lambda e, k=k, p_=p_, hf=hf: e.matmul(p_[:], lhsT=m_[:, k, :], rhs=wb[:, k, hf * 512:(hf + 1) * 512],
                                                                 start=(k == 0), stop=(k == 7)), reads=[tm, t_wb], writes=[tp])
            K.op("dve", lambda e, p_=p_, hf=hf: e.tensor_tensor(out=tmp[:, hf * 512:(hf + 1) * 512], in0=p_[:], in1=mm_[:, hf * 512:(hf + 1) * 512], op=ALU.mult),
                 reads=[tp, t_m2], writes=[t_tmp])
        K.op("pool", lambda e: e.tensor_tensor(out=x_[:], in0=x_[:], in1=tmp[:], op=ALU.add), reads=[tx, t_tmp], writes=[tx])
        K.dma(xs[tt * 128:(tt + 1) * 128, :], x_[:], reads=[tx], writes=[t_xs])

    for n, tt in enumerate(range(0 if need_ctx else NCTX_T, NT)):
        body(tt, n)
    K.pop()


def gen_conv(K, A, l, ub, vb, t_ub, t_vb, part=0, nparts=1, NC_=6):
    cf = [K.sb([128, D]) for _ in range(NC_)]; t_cf = [Trk() for _ in range(NC_)]
    cb = [K.sb([128, D], BF16) for _ in range(NC_)]; t_cb = [Trk() for _ in range(NC_)]
    engs = ["act", "dve", "pool"]
    jobs = []
    for i in range(128):
        jobs.append((A["pe_ut"][l, i].rearrange("p c e -> p (c e)"), ub[i].rearrange("p c e -> p (c e)"), t_ub))
        jobs.append((A["pe_v"][l, i * 128:(i + 1) * 128, :], vb[i], t_vb))
    per = len(jobs) // nparts
    jobs = jobs[part * per:(part + 1) * per]
    AH = min(4, NC_ - 1)

    def cv_load(n):
        K.dma(cf[n % NC_][:], jobs[n][0], writes=[t_cf[n % NC_]])

    def job(n):
        b_ = n % NC_
        eng = engs[n % 3]
        if eng == "act":
            K.op("act", lambda e: e.copy(out=cb[b_][:], in_=cf[b_][:]), reads=[t_cf[b_]], writes=[t_cb[b_]])
        else:
            K.op(eng, lambda e: e.tensor_copy(out=cb[b_][:], in_=cf[b_][:]), reads=[t_cf[b_]], writes=[t_cb[b_]])
        if n + AH < len(jobs):
            cv_load(n + AH)
        K.dma(jobs[n][1], cb[b_][:], reads=[t_cb[b_]], writes=[jobs[n][2]])

    yield "conv%d_%d" % (part, l)
    for n in range(AH):
        cv_load(n)
    for n in range(len(jobs)):
        job(n)
        if n % 2 == 1:
            yield


def phase_peer(K, A, l, xs, t_xs, modscr, t_mod, need_ctx, ubs, vbs, tconv):
    ub = ubs[l]; vb = vbs[l]; t_ub, t_vb = tconv
    K.push()
    identb, t_idb = make_ident(K, BF16)
    mods = load_mod(K, modscr, t_mod, A["norm_ffn_g"][l, :], 3, 4)
    m5 = [K.sb([128, D]) for _ in range(2)]; t_m5 = Trk()
    for which in range(2):
        K.dma(m5[which][:], modscr[which, 5 * D:6 * D].partition_broadcast(128), reads=[t_mod], writes=[t_m5])
    wq = K.sb([128, 8, 2048], BF16); t_wq = Trk()
    K.push()
    stg = [K.sb([128, 8, 512]) for _ in range(2)]; t_stg = [Trk(), Trk()]
    for i in range(4):
        K.dma(stg[i % 2][:], A["pe_wq"][l, :, i * 512:(i + 1) * 512].rearrange("(k p) n -> p k n", p=128), writes=[t_stg[i % 2]])
        K.op("pool", lambda e, i=i: e.tensor_copy(out=wq[:, :, i * 512:(i + 1) * 512], in_=stg[i % 2][:]), reads=[t_stg[i % 2]], writes=[t_wq])
    K.pop()
    keysT = K.sb([128, 16, 128]); t_keys = Trk()
    K.dma(keysT[:], A["keysT"][l].rearrange("c d k -> d c k"), writes=[t_keys])
    bk = [K.ps([128, 512]) for _ in range(8)]; t_bk = [Trk() for _ in range(8)]
    rot = [0]

    def rb():
        b = rot[0] % 4
        rot[0] += 1
        return bk[b], t_bk[b]

    xk = [K.sb([128, D]) for _ in range(2)]; t_xk = [Trk(), Trk()]
    scr = K.sb([128, D]); t_scr = Trk()
    ss = K.sb([128, 1]); t_ss = Trk()
    h = K.sb([128, D], BF16); t_h = Trk()
    hT = K.sb([128, 8, 256], BF16); t_hT = Trk()
    qT = K.sb([128, 16, 256]); t_qT = Trk()
    Ssb = [K.sb([128, 16, 128]) for _ in range(2)]; t_S = [Trk(), Trk()]
    s2pp = [K.sb([128, 8, 128]) for _ in range(2)]; t_s2 = [Trk(), Trk()]
    Dp = [K.sb([128, 8, 128], BF16) for _ in range(2)]; t_Dp = [Trk(), Trk()]
    top = K.sb([128, 8, 2, 16]); t_top = Trk()
    wk = K.sb([128, 256]); t_wk = Trk()
    cand = K.sb([128, 256]); t_cand = Trk()
    ctop = K.sb([128, 8, 16]); t_ctop = Trk()
    zs = K.sb([128, 8, 16]); t_zs = Trk()
    st8 = K.sb([128, 8]); t_st8 = Trk()
    NB = 4
    PSPL = 0
    ut = [K.sb([128, 8, 128], BF16) for _ in range(NB)]; t_ut = [Trk() for _ in range(NB)]
    vt = [K.sb([128, D], BF16) for _ in range(NB)]; t_vt = [Trk() for _ in range(NB)]
    ga = [K.sb([128, 256]) for _ in range(3)]; t_ga = [Trk() for _ in range(3)]
    EE = [[K.sb([128, 8, 128]) for _ in range(2)] for _ in range(2)]; t_EE = [[Trk(), Trk()], [Trk(), Trk()]]
    E2 = [K.sb([128, 8, 128]) for _ in range(2)]; t_E2 = [Trk(), Trk()]
    e1 = [K.sb([128, 8, 128]) for _ in range(2)]; t_e1 = [Trk(), Trk()]
    GG = [[K.sb([128, 8, 128], BF16) for _ in range(2)] for _ in range(2)]; t_GG = [[Trk(), Trk()], [Trk(), Trk()]]
    AW = [K.sb([128, 256], BF16) for _ in range(2)]; t_AW = [Trk(), Trk()]
    tmpo = K.sb([128, D]); t_tmpo = Trk()
    NEGBIG = -1.0e30

    def block(t0, which):
        for j in range(2):
            tt = t0 + j
            K.dma(xk[j][:], xs[tt * 128:(tt + 1) * 128, :], reads=[t_xs], writes=[t_xk[j]])
            norm_mod_tile(K, xk[j][:], t_xk[j], mods[which], h, t_h, scr, t_scr, ss, t_ss)
            pb, tb = rb()
            pst = pb.bitcast(BF16).rearrange("p (c n) -> p c n", n=128)
            transpose_to(K, h, t_h, 8, identb, t_idb, pst, tb, hT[:, :, j * 128:(j + 1) * 128], t_hT)
        for cs in range(16):
            pb, tb = rb()
            for k in range(8):
                K.op("pe", lambda e, k=k, cs=cs, pb=pb: e.matmul(pb[:, 0:256], lhsT=wq[:, k, cs * 128:(cs + 1) * 128], rhs=hT[:, k, :],
                                                                 start=(k == 0), stop=(k == 7)), reads=[t_wq, t_hT], writes=[tb])
            if cs % 2:
                K.op("act", lambda e, cs=cs, pb=pb: e.copy(out=qT[:, cs, :], in_=pb[:, 0:256]), reads=[tb], writes=[t_qT])
            else:
                K.op("dve", lambda e, cs=cs, pb=pb: e.tensor_copy(out=qT[:, cs, :], in_=pb[:, 0:256]), reads=[tb], writes=[t_qT])
        for j in range(2):
            for q4 in range(4):
                pb, tb = rb()
                for u in range(4):
                    cs = q4 * 4 + u
                    K.op("pe", lambda e, cs=cs, u=u, pb=pb, j=j: e.matmul(pb[:, u * 128:(u + 1) * 128], lhsT=qT[:, cs, j * 128:(j + 1) * 128],
                                                                         rhs=keysT[:, cs, :], start=True, stop=True),
                         reads=[t_qT, t_keys], writes=[tb])
                K.op("act", lambda e, q4=q4, pb=pb, j=j: e.copy(out=Ssb[j][:, q4 * 4:(q4 + 1) * 4, :], in_=pb[:].rearrange("p (c n) -> p c n", n=128)),
                     reads=[tb], writes=[t_S[j]])
            for p in range(8):
                for side in range(2):
                    src = Ssb[j][:, 2 * p + side, :]
                    K.op("dve", lambda e, p=p, side=side, src=src: e.max(out=top[:, p, side, 0:8], in_=src), reads=[t_S[j]], writes=[t_top])
                    K.op("dve", lambda e, p=p, side=side, src=src: e.match_replace(out=wk[:, 0:128], in_to_replace=top[:, p, side, 0:8], in_values=src,
                                                                                  imm_value=NEGBIG), reads=[t_S[j], t_top], writes=[t_wk])
                    K.op("dve", lambda e, p=p, side=side: e.max(out=top[:, p, side, 8:16], in_=wk[:, 0:128]), reads=[t_wk], writes=[t_top])
                c3 = cand[:].rearrange("p (a b) -> p a b", a=16)
                K.op("pool", lambda e, p=p, c3=c3: e.tensor_tensor(out=c3, in0=top[:, p, 0, :].unsqueeze(2).broadcast_to([128, 16, 16]),
                                                                   in1=top[:, p, 1, :].unsqueeze(1).broadcast_to([128, 16, 16]), op=ALU.add),
                     reads=[t_top], writes=[t_cand])
                K.op("dve", lambda e, p=p: e.max(out=ctop[:, p, 0:8], in_=cand[:]), reads=[t_cand], writes=[t_ctop])
                K.op("dve", lambda e, p=p: e.match_replace(out=wk[:], in_to_replace=ctop[:, p, 0:8], in_values=cand[:], imm_value=NEGBIG),
                     reads=[t_cand, t_ctop], writes=[t_wk])
                K.op("dve", lambda e, p=p: e.max(out=ctop[:, p, 8:16], in_=wk[:]), reads=[t_wk], writes=[t_ctop])
            tau = ctop[:, :, 15:16]
            K.op("dve", lambda e: e.tensor_tensor(out=zs[:], in0=ctop[:], in1=tau.broadcast_to([128, 8, 16]), op=ALU.subtract),
                 reads=[t_ctop], writes=[t_zs])
            K.op("act", lambda e: e.activation(out=zs[:], in_=zs[:], func=AF.Exp), reads=[t_zs], writes=[t_zs])
            K.op("dve", lambda e: e.tensor_reduce(out=st8[:], in_=zs[:], axis=AX.X, op=ALU.add), reads=[t_zs], writes=[t_st8])
            K.op("dve", lambda e: e.reciprocal(out=st8[:], in_=st8[:]), reads=[t_st8], writes=[t_st8])
            S4 = Ssb[j][:].rearrange("p (h s) n -> p h s n", s=2)
            K.op("dve", lambda e, S4=S4, j=j: e.tensor_tensor(out=s2pp[j][:], in0=S4[:, :, 1, :], in1=tau.broadcast_to([128, 8, 128]), op=ALU.subtract),
                 reads=[t_S[j], t_ctop], writes=[t_s2[j]])
            m1 = top[:, :, 0, 0:1]
            K.op("dve", lambda e, j=j: e.tensor_tensor(out=s2pp[j][:], in0=s2pp[j][:], in1=m1.broadcast_to([128, 8, 128]), op=ALU.add),
                 reads=[t_s2[j], t_top], writes=[t_s2[j]])
            K.op("act", lambda e, j=j: e.activation(out=E2[j][:], in_=s2pp[j][:], func=AF.Exp, bias=1.0e-3), reads=[t_s2[j]], writes=[t_E2[j]])
            K.op("dve", lambda e, S4=S4, j=j: e.tensor_tensor(out=e1[j][:], in0=S4[:, :, 0, :], in1=m1.broadcast_to([128, 8, 128]), op=ALU.subtract),
                 reads=[t_S[j], t_top], writes=[t_e1[j]])
            K.op("act", lambda e, j=j: e.activation(out=e1[j][:], in_=e1[j][:], func=AF.Exp), reads=[t_e1[j]], writes=[t_e1[j]])
            K.op("dve", lambda e, j=j: e.tensor_tensor(out=Dp[j][:], in0=identb[:].unsqueeze(1).broadcast_to([128, 8, 128]),
                                                      in1=st8[:].unsqueeze(2).broadcast_to([128, 8, 128]), op=ALU.mult),
                 reads=[t_idb, t_st8], writes=[t_Dp[j]])
        S4 = [Ssb[j][:].rearrange("p (h s) n -> p h s n", s=2) for j in range(2)]

        def st_load(c):
            K.dma(ut[c % NB][:], ub[c], reads=[t_ub], writes=[t_ut[c % NB]])
            K.dma(vt[c % NB][:], vb[c], reads=[t_vb], writes=[t_vt[c % NB]])

        def st_gate(c):
            for j in range(2):
                E_ = EE[j][c % 2]; tE = t_EE[j][c % 2]; G_ = GG[j][c % 2]; tG = t_GG[j][c % 2]
                if j == 0:
                    K.op("pool", lambda e, E_=E_, j=j: e.tensor_tensor(out=E_[:], in0=E2[j][:], in1=e1[j][:, :, c:c + 1].broadcast_to([128, 8, 128]), op=ALU.mult),
                         reads=[t_E2[j], t_e1[j]], writes=[tE])
                else:
                    if PSPL > 0:
                        K.op("pool", lambda e, E_=E_, j=j: e.tensor_tensor(out=E_[:, 0:PSPL, :], in0=E2[j][:, 0:PSPL, :],
                                                                           in1=e1[j][:, 0:PSPL, c:c + 1].broadcast_to([128, PSPL, 128]), op=ALU.mult),
                             reads=[t_E2[j], t_e1[j]], writes=[tE])
                    for p in range(PSPL, 8):
                        K.op("act", lambda e, E_=E_, j=j, p=p: e.activation(out=E_[:, p, :], in_=E2[j][:, p, :], func=AF.Identity, scale=e1[j][:, p, c:c + 1]),
                             reads=[t_E2[j], t_e1[j]], writes=[tE])
                K.op("dve", lambda e, E_=E_, G_=G_: e.scalar_tensor_tensor(out=G_[:], in0=E_[:], scalar=1.0, in1=E_[:], op0=ALU.is_ge, op1=ALU.mult),
                     reads=[tE], writes=[tG])

        def st_A(c):
            pa, ta = bk[c % 2], t_bk[c % 2]
            u_ = ut[c % NB]
            for k in range(8):
                K.op("pe", lambda e, k=k: e.matmul(pa[:, 0:256], lhsT=u_[:, k, :], rhs=hT[:, k, :], start=(k == 0), stop=(k == 7)),
                     reads=[t_ut[c % NB], t_hT], writes=[ta])

        def st_gelu(c):
            pa, ta = bk[c % 2], t_bk[c % 2]
            g_ = ga[c % 3]
            K.op("act", lambda e: e.activation(out=g_[:], in_=pa[:, 0:256], func=AF.Gelu_apprx_tanh), reads=[ta], writes=[t_ga[c % 3]])

        def st_W(c):
            pw, tw = bk[2 + c % 2], t_bk[2 + c % 2]
            for j in range(2):
                G_ = GG[j][c % 2]; tG = t_GG[j][c % 2]
                for p in range(8):
                    K.op("pe", lambda e, p=p, j=j, G_=G_: e.matmul(pw[:, j * 128:(j + 1) * 128], lhsT=G_[:, p, :], rhs=Dp[j][:, p, :],
                                                                  start=(p == 0), stop=(p == 7)), reads=[tG, t_Dp[j]], writes=[tw])

        def st_AW(c):
            pw, tw = bk[2 + c % 2], t_bk[2 + c % 2]
            aw = AW[c % 2]; taw = t_AW[c % 2]; g_ = ga[c % 3]
            K.op("dve", lambda e: e.tensor_tensor(out=aw[:], in0=g_[:], in1=pw[:, 0:256], op=ALU.mult), reads=[t_ga[c % 3], tw], writes=[taw])

        def st_out(c):
            aw = AW[c % 2]; taw = t_AW[c % 2]; v_ = vt[c % NB]
            for j in range(2):
                for hf in range(2):
                    b = 4 + 2 * j + hf
                    K.op("pe", lambda e, j=j, hf=hf, b=b: e.matmul(bk[b][:], lhsT=aw[:, j * 128:(j + 1) * 128], rhs=v_[:, hf * 512:(hf + 1) * 512],
                                                                  start=(c == 0), stop=(c == 127)), reads=[taw, t_vt[c % NB]], writes=[t_bk[b]])

        for c in range(NB):
            st_load(c)
        for s_ in range(-2, 128):
            if s_ >= 0:
                st_AW(s_)
            if s_ + 2 < 128:
                st_A(s_ + 2)
            if s_ >= 0:
                st_out(s_)
            if 0 <= s_ + 1 < 128:
                st_W(s_ + 1)
            if s_ + 2 < 128:
                st_gate(s_ + 2)
                st_gelu(s_ + 2)
            if s_ >= 0 and s_ + NB < 128:
                st_load(s_ + NB)
        for j in range(2):
            tt = t0 + j
            for hf in range(2):
                b = 4 + 2 * j + hf
                K.op("dve", lambda e, b=b, hf=hf: e.tensor_tensor(out=tmpo[:, hf * 512:(hf + 1) * 512], in0=bk[b][:], in1=m5[which][:, hf * 512:(hf + 1) * 512], op=ALU.mult),
                     reads=[t_bk[b], t_m5], writes=[t_tmpo])
            K.op("dve", lambda e, j=j: e.tensor_tensor(out=xk[j][:], in0=xk[j][:], in1=tmpo[:], op=ALU.add), reads=[t_xk[j], t_tmpo], writes=[t_xk[j]])
            K.dma(xs[tt * 128:(tt + 1) * 128, :], xk[j][:], reads=[t_xk[j]], writes=[t_xs])

    for t0 in range(0 if need_ctx else NCTX_T, NT, 2):
        block(t0, 1 if t0 < NCTX_T else 0)
    K.pop()


IN_SPECS = {
    "xall": ([NTOK, D], F32), "cvec": ([128, 16], F32),
    "ada_w": ([2, D, 6144], F32), "ada_b": ([2, 6144], F32),
    "norm_mix_g": ([2, D], F32), "norm_ffn_g": ([2, D], F32),
    "w_in": ([2, D, IN_COLS], F32), "w_out": ([2, D, D], F32),
    "mla_q_norm_g": ([2, 256], F32), "mla_w_qb": ([2, 256, 384], F32), "mla_kv_norm_g": ([2, 128], F32),
    "mla_w_kvb": ([2, 128, 512], F32), "mla_qk_g": ([2, 2, 96], F32),
    "rw_mu": ([2, 2, 1568], F32), "rw_w0": ([2, 2, 384], F32), "rw_w2": ([2, 2, 64, 384], F32),
    "rw_a0": ([2, 2, 384], F32), "rw_a2": ([2, 2, 64, 384], F32), "rw_g2": ([2, 160, 384], F32),
    "rw_k_k": ([2, 384], F32), "rw_k_a": ([2, 384], F32), "rw_r_k": ([2, 384], F32),
    "rw_ln_g": ([2, 384], F32), "rw_ln_b": ([2, 384], F32), "gqa_qk_g": ([2, 2, 64], F32),
    "pe_wq": ([2, D, 2048], F32), "keysT": ([2, 16, 128, 128], F32),
    "pe_ut": ([2, 128, 128, 8, 128], F32), "pe_v": ([2, 16384, D], F32),
    "rope_m": ([2048, 32], F32), "rope_g": ([2048, 64], F32),
}


def build(layers=(0, 1), upto=None, dbg=False, scopes=False, same=True):
    nc = bass.Bass("TRN2", target_bir_lowering=False)
    A = {k: nc.dram_tensor(k, sh, dt, kind="ExternalInput").ap() for k, (sh, dt) in IN_SPECS.items()}
    skind = "ExternalOutput" if dbg else "Internal"
    out = nc.dram_tensor("out", [2048, D], F32, kind="ExternalOutput").ap()
    S = {}
    S["xs"] = nc.dram_tensor("xs", [NTOK, D], F32, kind=skind).ap()
    S["mod"] = nc.dram_tensor("modscr", [2, 6144], F32, kind=skind).ap()
    S["p"] = nc.dram_tensor("pscr", [NTOK, IN_COLS], F32, kind=skind).ap()
    S["rwA"] = nc.dram_tensor("rwA", [NTOK, 11, 384], F32, kind=skind).ap()
    S["yscr"] = nc.dram_tensor("yscr", [NTOK, 384], F32, kind=skind).ap()
    S["yscr1"] = nc.dram_tensor("yscr1", [NTOK, 384], F32, kind=skind).ap()
    S["mixT"] = nc.dram_tensor("mixT", [D, NTOK], BF16, kind=skind).ap()
    DS = {}
    for nm, sh in [("qtm", [96, 4, NTOK]), ("ktm", [96, 4, NTOK]), ("vdm", [NTOK, 4, 128]),
                   ("qtg", [64, 6, NTOK]), ("ktg", [64, 2, NTOK]), ("vdg", [NTOK, 4, 128])]:
        DS[nm] = nc.dram_tensor(nm, sh, BF16, kind="Internal").ap()
        DS["t_" + nm] = Trk()
    UB = [nc.dram_tensor("ub%d" % l, [128, 128, 8, 128], BF16, kind="Internal").ap() for l in range(2)]
    VB = [nc.dram_tensor("vb%d" % l, [128, 128, D], BF16, kind="Internal").ap() for l in range(2)]
    with ExitStack() as es:
        K = Kb(nc, es, same_eng_sync=same, scopes=scopes)
        t_out = Trk()
        T = {k: Trk() for k in S}
        K.push()
        stg = [K.sb([128, D]) for _ in range(2)]; t_stg = [Trk(), Trk()]
        for tt in range(NT):
            K.dma(stg[tt % 2][:], A["xall"][tt * 128:(tt + 1) * 128, :], writes=[t_stg[tt % 2]])
            K.dma(S["xs"][tt * 128:(tt + 1) * 128, :], stg[tt % 2][:], reads=[t_stg[tt % 2]], writes=[T["xs"]])
        K.pop()
        for l in layers:
            need_ctx = l < 1
            K.cur = "ada_%d" % l
            phase_ada(K, A, l, S["mod"], T["mod"])
            tconv = (Trk(), Trk())
            K.run_lanes([("proj_in", gen_proj_in(K, A, l, S["xs"], T["xs"], S["mod"], T["mod"], S["p"], T["p"]), 1.0),
                         ("conv", gen_conv(K, A, l, UB[l], VB[l], tconv[0], tconv[1], 0, 2), 3.6)])
            if upto == "projin":
                break

            def prep_lane():
                yield from gen_mla_prep(K, A, l, S["p"], T["p"], DS)
                yield from gen_gqa_prep(K, A, l, S["p"], T["p"], DS)

            K.run_lanes([("mla_prep", gen_mla_prep(K, A, l, S["p"], T["p"], DS), 1.0),
                         ("gqa_prep", gen_gqa_prep(K, A, l, S["p"], T["p"], DS), 0.7),
                         ("rw_prep", gen_rw_prep(K, A, l, S["p"], T["p"], S["rwA"], T["rwA"]), 1.0),
                         ("conv", gen_conv(K, A, l, UB[l], VB[l], tconv[0], tconv[1], 1, 2, NC_=2), 3.6)])
            if upto == "prep":
                break
            K.run_lanes([("attn_m", gen_attn(K, A, l, DS, S["mixT"], T["mixT"], need_ctx, "m"), 1.0),
                         ("attn_g", gen_attn(K, A, l, DS, S["mixT"], T["mixT"], need_ctx, "g"), 1.5)])
            K.push()
            RC = rw_consts(K, A, l)
            K.run_lanes([("rw_scan0", gen_rw_scan(K, A, l, RC, 0, S["rwA"], T["rwA"], S["yscr"], T["yscr"], n_rot=3), 1.0),
                         ("rw_scan1", gen_rw_scan(K, A, l, RC, 1, S["rwA"], T["rwA"], S["yscr1"], T["yscr1"], n_rot=3), 1.0)])
            K.pop()
            K.run_lanes([("rw_readout", gen_rw_readout(K, A, l, S["rwA"], T["rwA"], S["yscr"], T["yscr"], S["yscr1"], T["yscr1"], S["mixT"], T["mixT"]), 1.0)])
            if upto == "rwscan":
                break
            K.cur = "wout_%d" % l
            phase_wout(K, A, l, S["xs"], T["xs"], S["mod"], T["mod"], S["mixT"], T["mixT"], need_ctx)
            if upto == "wout":
                break
            K.cur = "peer_%d" % l
            phase_peer(K, A, l, S["xs"], T["xs"], S["mod"], T["mod"], need_ctx, UB, VB, tconv)
            if upto == "peer":
                break
        K.push()
        stg = [K.sb([128, D]) for _ in range(2)]; t_stg = [Trk(), Trk()]
        for tt in range(16):
            K.dma(stg[tt % 2][:], S["xs"][(tt + 2) * 128:(tt + 3) * 128, :], reads=[T["xs"]], writes=[t_stg[tt % 2]])
            K.dma(out[tt * 128:(tt + 1) * 128, :], stg[tt % 2][:], reads=[t_stg[tt % 2]], writes=[t_out])
        K.pop()
        K.emit([t_out] + list(T.values()))
    return nc


def rope_tables():
    n = np.arange(2048)
    row = (n // 64).astype(np.float32); col = (n % 64).astype(np.float32)
    tabs = []
    for rdim in (32, 64):
        q = rdim // 4
        inv = (10000.0 ** (-np.arange(q, dtype=np.float32) / q)).astype(np.float32)
        ang = np.concatenate([row[:, None] * inv, col[:, None] * inv], -1).astype(np.float32)
        tabs.append(np.concatenate([np.cos(ang), np.sin(ang)], -1).astype(np.float32))
    return tabs


def prep_inputs(inp, batches):
    f = lambda a: np.ascontiguousarray(np.asarray(a, dtype=np.float32))
    rm, rg = rope_tables()
    shared = {k: f(inp[k]) for k in ["ada_w", "ada_b", "norm_mix_g", "norm_ffn_g", "w_in", "w_out", "mla_q_norm_g", "mla_w_qb",
                                     "mla_kv_norm_g", "mla_w_kvb", "mla_qk_g", "rw_mu", "rw_w0", "rw_w2", "rw_a0", "rw_a2", "rw_g2",
                                     "rw_k_k", "rw_k_a", "rw_ln_g", "rw_ln_b", "gqa_qk_g", "pe_wq", "pe_v"]}
    shared["rw_r_k"] = f(inp["rw_r_k"]).reshape(2, 384)
    shared["keysT"] = f(np.transpose(f(inp["pe_keys"]).reshape(2, 16, 128, 128), (0, 1, 3, 2)))
    u = f(inp["pe_u"]).reshape(2, 128, 128, 8, 128)
    shared["pe_ut"] = f(np.transpose(u, (0, 1, 4, 3, 2)))
    shared["rope_m"] = rm; shared["rope_g"] = rg
    maps = []
    for b in batches:
        m = dict(shared)
        m["xall"] = f(np.concatenate([inp["ctx"][b], inp["x"][b]], 0))
        cv = np.stack([f(inp["c"][b]).reshape(8, 128), f(inp["c_ctx"]).reshape(8, 128)], -1)
        m["cvec"] = f(np.transpose(cv, (1, 0, 2)).reshape(128, 16))
        maps.append(m)
    return maps


def kernel(**inputs):
    from concourse.bass_utils import run_bass_kernel_spmd
    nc = build()
    maps = prep_inputs(inputs, list(range(8)))
    res = run_bass_kernel_spmd(nc, maps, core_ids=list(range(8)))
    return np.stack([np.asarray(r["out"], dtype=np.float32) for r in res.results], 0)
```
